# Optimizing a Trainium2 kernel written in Bass

```python
import jax, jax.numpy as jnp
from jax import lax
import numpy as np

D_MODEL = 1024
BATCH = 32
SEQ = 2048
DEPTH = 2

N_MIXERS = 2
N_CONV_LAYERS = (DEPTH + 1) // 2
N_RET_LAYERS = DEPTH // 2
CONV_WIDTH = 3
RET_HEADS = 4
RET_QK_DIM = D_MODEL // RET_HEADS
RET_V_DIM = 2 * RET_QK_DIM
RET_QK_WIDTH = RET_HEADS * RET_QK_DIM
RET_V_WIDTH = RET_HEADS * RET_V_DIM
RET_CHUNK = 128
ROPE_BASE = 10000.0
N_EXPERTS = 16
EC_CAPACITY_FACTOR = 2
EXPERT_FF = 2 * D_MODEL
PLE_DIM = 256
NORM_EPS = 1e-6
GN_EPS = 1e-5

kernel_name = "hybrid_conv_retention_ec_moe_encoder"


def rms_norm(x, g):
    xf = x.astype(jnp.float32)
    y = xf * lax.rsqrt(jnp.mean(xf * xf, axis=-1, keepdims=True) + NORM_EPS)
    return (y * g.astype(jnp.float32)).astype(x.dtype)


def short_conv_mixer(h, w_in, w_conv, b_conv, w_out):
    b_gate, c_gate, v = jnp.split(h @ w_in, 3, axis=-1)
    u = c_gate * v
    kern = w_conv[:, None, :].astype(u.dtype)
    pad = CONV_WIDTH // 2
    y = lax.conv_general_dilated(u, kern, window_strides=(1,), padding=((pad, pad),),
                                 dimension_numbers=('NWC', 'WIO', 'NWC'),
                                 feature_group_count=u.shape[-1]) + b_conv.astype(u.dtype)
    return (b_gate * y) @ w_out


def rotate(x, pos):
    half = x.shape[-1] // 2
    inv = ROPE_BASE ** (-jnp.arange(half, dtype=jnp.float32) / half)
    ang = pos.astype(jnp.float32)[:, None] * inv[None, :]
    cos = jnp.cos(ang)[:, None, :]
    sin = jnp.sin(ang)[:, None, :]
    x1, x2 = x[..., :half], x[..., half:]
    return jnp.concatenate([x1 * cos - x2 * sin, x1 * sin + x2 * cos], axis=-1)


def retention_one_direction(q, k, v, log_gamma, strict):
    b, h, s, dk = q.shape
    dv = v.shape[-1]
    L = RET_CHUNK
    n = s // L
    lg = log_gamma.astype(jnp.float32)
    idx = jnp.arange(L, dtype=jnp.float32)
    diff = idx[:, None] - idx[None, :]
    mask = (diff > 0) if strict else (diff >= 0)
    intra_decay = jnp.where(mask[None], jnp.exp(lg[:, None, None] * jnp.where(mask, diff, 0.0)[None]), 0.0)
    q_decay = jnp.exp(lg[:, None] * (idx[None, :] + 1.0))
    k_decay = jnp.exp(lg[:, None] * (L - 1.0 - idx[None, :]))
    chunk_decay = jnp.exp(lg * L)

    def to_chunks(t):
        return jnp.moveaxis(t.reshape(b, h, n, L, t.shape[-1]), 2, 0)

    def step(state, inp):
        qi, ki, vi = inp
        scores = jnp.einsum('bhid,bhjd->bhij', qi, ki) * intra_decay[None]
        intra = jnp.einsum('bhij,bhjv->bhiv', scores, vi)
        cross = jnp.einsum('bhld,bhdv->bhlv', qi * q_decay[None, :, :, None], state)
        state = state * chunk_decay[None, :, None, None] + jnp.einsum(
            'bhld,bhlv->bhdv', ki * k_decay[None, :, :, None], vi)
        return state, intra + cross

    init = jnp.zeros((b, h, dk, dv), jnp.float32)
    _, out = lax.scan(step, init, (to_chunks(q), to_chunks(k), to_chunks(v)))
    return jnp.moveaxis(out, 0, 2).reshape(b, h, s, dv)


def retention_mixer(h, w_in, log_decay, w_out):
    b, s, _ = h.shape
    proj = h @ w_in
    q, k, v, g = jnp.split(proj, [RET_QK_WIDTH, 2 * RET_QK_WIDTH, 2 * RET_QK_WIDTH + RET_V_WIDTH], axis=-1)
    pos = jnp.arange(s)
    q = rotate(q.reshape(b, s, RET_HEADS, RET_QK_DIM).astype(jnp.float32), pos)
    k = rotate(k.reshape(b, s, RET_HEADS, RET_QK_DIM).astype(jnp.float32), pos) * (RET_QK_DIM ** -0.5)
    v = v.reshape(b, s, RET_HEADS, RET_V_DIM).astype(jnp.float32)
    q, k, v = (jnp.transpose(t, (0, 2, 1, 3)) for t in (q, k, v))
    fwd = retention_one_direction(q, k, v, log_decay[0], strict=False)
    bwd = jnp.flip(retention_one_direction(jnp.flip(q, 2), jnp.flip(k, 2), jnp.flip(v, 2),
                                           log_decay[1], strict=True), 2)
    o = fwd + bwd
    mu = jnp.mean(o, axis=-1, keepdims=True)
    var = jnp.mean(jnp.square(o - mu), axis=-1, keepdims=True)
    o = (o - mu) * lax.rsqrt(var + GN_EPS)
    o = jnp.transpose(o, (0, 2, 1, 3)).reshape(b, s, RET_V_WIDTH).astype(h.dtype)
    return (jax.nn.silu(g) * o) @ w_out


def expert_choice_ffn(h, router_w, w_gate, w_up, w_down):
    b, s, _ = h.shape
    cap = EC_CAPACITY_FACTOR * s // N_EXPERTS
    logits = jnp.einsum('bsd,de->bse', h.astype(jnp.float32), router_w.astype(jnp.float32))
    affinity = jax.nn.softmax(logits, axis=-1)
    gates, tok = lax.top_k(jnp.swapaxes(affinity, 1, 2), cap)
    tok_e = jnp.swapaxes(tok, 0, 1)
    gates_e = jnp.swapaxes(gates, 0, 1)
    bidx = jnp.arange(b)[:, None]

    def run_expert(args):
        wg, wu, wd, t = args
        xe = h[bidx, t]
        return (jax.nn.silu(xe @ wg) * (xe @ wu)) @ wd

    y = lax.map(run_expert, (w_gate, w_up, w_down, tok_e))
    y = y * gates_e[..., None].astype(y.dtype)
    return jnp.zeros_like(h).at[jnp.arange(b)[None, :, None], tok_e].add(y)


def setup_inputs(seed: int = 0) -> dict:
    key = jax.random.key(seed)
    ks = jax.random.split(key, 20)
    f32 = jnp.float32
    D, E, F = D_MODEL, N_EXPERTS, EXPERT_FF

    def w(k, shape, fan_in):
        return jax.random.normal(k, shape, f32) * (fan_in ** -0.5)

    base_decay = jnp.log(1.0 - 2.0 ** (-5.0 - jnp.arange(RET_HEADS, dtype=f32)))
    ret_log_decay = base_decay[None, None, :] * (1.0 + 0.05 * jax.random.normal(ks[10], (N_RET_LAYERS, 2, RET_HEADS), f32))
    return {
        "x": jax.random.normal(ks[0], (BATCH, SEQ, D), f32),
        "p": jax.random.normal(ks[1], (DEPTH, BATCH, SEQ, PLE_DIM), f32),
        "norm_mix": 1.0 + 0.01 * jax.random.normal(ks[2], (DEPTH, D), f32),
        "norm_ffn": 1.0 + 0.01 * jax.random.normal(ks[3], (DEPTH, D), f32),
        "norm_ple": 1.0 + 0.01 * jax.random.normal(ks[4], (DEPTH, D), f32),
        "final_norm": 1.0 + 0.01 * jax.random.normal(ks[5], (D,), f32),
        "conv_w_in": w(ks[6], (N_CONV_LAYERS, D, 3 * D), D),
        "conv_w": w(ks[7], (N_CONV_LAYERS, CONV_WIDTH, D), CONV_WIDTH),
        "conv_b": 0.01 * jax.random.normal(ks[8], (N_CONV_LAYERS, D), f32),
        "conv_w_out": w(ks[9], (N_CONV_LAYERS, D, D), D),
        "ret_w_in": w(ks[11], (N_RET_LAYERS, D, 2 * RET_QK_WIDTH + 2 * RET_V_WIDTH), D),
        "ret_log_decay": ret_log_decay,
        "ret_w_out": w(ks[12], (N_RET_LAYERS, RET_V_WIDTH, D), RET_V_WIDTH),
        "router_w": w(ks[13], (DEPTH, D, E), D),
        "exp_w_gate": w(ks[14], (DEPTH, E, D, F), D),
        "exp_w_up": w(ks[15], (DEPTH, E, D, F), D),
        "exp_w_down": w(ks[16], (DEPTH, E, F, D), F),
        "ple_w_proj": w(ks[17], (DEPTH, PLE_DIM, D), PLE_DIM),
        "ple_w_gate": w(ks[18], (DEPTH, D, D), D),
    }


def reference(x, p, norm_mix, norm_ffn, norm_ple, final_norm, conv_w_in, conv_w, conv_b, conv_w_out,
              ret_w_in, ret_log_decay, ret_w_out, router_w, exp_w_gate, exp_w_up, exp_w_down,
              ple_w_proj, ple_w_gate):
    h = x
    for i in range(DEPTH):
        j = i // N_MIXERS
        hn = rms_norm(h, norm_mix[i])
        if i % N_MIXERS == 0:
            mix = short_conv_mixer(hn, conv_w_in[j], conv_w[j], conv_b[j], conv_w_out[j])
        else:
            mix = retention_mixer(hn, ret_w_in[j], ret_log_decay[j], ret_w_out[j])
        h = h + mix
        h = h + expert_choice_ffn(rms_norm(h, norm_ffn[i]), router_w[i], exp_w_gate[i], exp_w_up[i], exp_w_down[i])
        gate = jax.nn.sigmoid(rms_norm(h, norm_ple[i]) @ ple_w_gate[i])
        h = h + gate * (p[i].astype(h.dtype) @ ple_w_proj[i])
    return rms_norm(h, final_norm)
```

```python
import numpy as np
from contextlib import ExitStack
import concourse.bass as bass
import concourse.mybir as mybir
from concourse.bass_utils import run_bass_kernel_spmd

F32 = mybir.dt.float32
BF16 = mybir.dt.bfloat16
I32 = mybir.dt.int32
U32 = mybir.dt.uint32
AF = mybir.ActivationFunctionType
ALU = mybir.AluOpType
AX = mybir.AxisListType

ENGS = ["pe", "act", "dve", "pool", "sp"]
N_DMA_SEMS = 20
SEMS = {}


class Op:
    __slots__ = ("eng", "fn", "waits", "idx", "flag", "cnt", "dsem", "dval", "is_dma", "pre")

    def __init__(self, eng, fn, idx, is_dma):
        self.eng = eng
        self.fn = fn
        self.idx = idx
        self.is_dma = is_dma
        self.flag = False
        self.cnt = None
        self.waits = []
        self.dsem = None
        self.dval = None
        self.pre = None


class Phase:
    def __init__(self, nc, name):
        self.nc = nc
        self.name = name
        self.q = {e: [] for e in ENGS}
        self.last_w = {}
        self.readers = {}
        self.stack = ExitStack()
        self.dma_rr = 0
        self.dma_last = [None] * N_DMA_SEMS
        self.dma_cnt = list(SEMS["dcnt"])
        self.n_ops = 0

    def sb(self, name, shape, dt):
        return self.stack.enter_context(self.nc.sbuf_tensor(f"{self.name}_{name}", list(shape), dt))

    def ps(self, name, shape, dt=F32):
        return self.stack.enter_context(self.nc.psum_tensor(f"{self.name}_{name}", list(shape), dt))

    def op(self, eng, fn, r=(), w=(), dma=False, after=(), pe_acc=False):
        o = Op(eng, fn, len(self.q[eng]), dma)
        deps = []
        for k in r:
            lw = self.last_w.get(k)
            if lw is not None:
                deps.append(lw)
        for k in w:
            lw = self.last_w.get(k)
            if lw is not None:
                deps.append(lw)
            deps.extend(self.readers.get(k, ()))
        deps.extend(after)
        seen = set()
        for d in deps:
            if d is o or id(d) in seen:
                continue
            seen.add(id(d))
            if d.eng == "pe" and eng == "pe" and not d.is_dma:
                continue
            o.waits.append(d)
            if not d.is_dma:
                d.flag = True
        if dma:
            s = self.dma_rr % N_DMA_SEMS
            self.dma_rr += 1
            o.pre = self.dma_last[s]
            self.dma_cnt[s] += 1
            o.dsem = s
            o.dval = 16 * self.dma_cnt[s]
            self.dma_last[s] = o
        for k in w:
            self.last_w[k] = o
            self.readers[k] = []
        for k in r:
            if k not in w:
                self.readers.setdefault(k, []).append(o)
        self.q[eng].append(o)
        self.n_ops += 1
        return o

    def emit(self):
        nc = self.nc
        st = self.stack
        esem = SEMS["esem"]
        dsem = SEMS["dsem"]
        ebase = dict(SEMS["ebase"])
        dbase = [16 * c for c in SEMS["dcnt"]]
        final = {}
        for e in ENGS:
            comp = [o for o in self.q[e] if not o.is_dma]
            if comp:
                comp[-1].flag = True
            c = ebase[e]
            for o in self.q[e]:
                if not o.is_dma and o.flag:
                    c += 1
                    o.cnt = c
            final[e] = c
        dfinal = [16 * c for c in self.dma_cnt]
        SEMS["ebase"] = dict(final)
        SEMS["dcnt"] = list(self.dma_cnt)
        q = self.q

        def run(e, eng):
            waited_e = dict(ebase)
            waited_d = list(dbase)
            for o in q[e]:
                ws = list(o.waits)
                if o.pre is not None:
                    ws.append(o.pre)
                for d in ws:
                    if d.is_dma:
                        if waited_d[d.dsem] < d.dval:
                            eng.wait_ge(dsem[d.dsem], d.dval)
                            waited_d[d.dsem] = d.dval
                    else:
                        if waited_e[d.eng] < d.cnt:
                            eng.wait_ge(esem[d.eng], d.cnt)
                            waited_e[d.eng] = d.cnt
                ins = o.fn(eng)
                if o.is_dma:
                    ins.then_inc(dsem[o.dsem], 16)
                elif o.flag:
                    ins.then_inc(esem[e], 1)
            for x in ENGS:
                if final[x] > waited_e[x]:
                    eng.wait_ge(esem[x], final[x])
            for i in range(N_DMA_SEMS):
                if dfinal[i] > waited_d[i]:
                    eng.wait_ge(dsem[i], dfinal[i])

        with nc.Block() as block:
            @block.tensor
            def _(eng):
                run("pe", eng)

            @block.scalar
            def _(eng):
                run("act", eng)

            @block.vector
            def _(eng):
                run("dve", eng)

            @block.gpsimd
            def _(eng):
                run("pool", eng)

            @block.sync
            def _(eng):
                run("sp", eng)
        self.stack.close()


S = 2048
D = 1024
NT = 16
NE = 16
CAP = 256
FF = 2048
PLE = 256
NCORES = 8
import os as _os
CCUT = _os.environ.get('CCUT', '')


def _consts():
    half = 128
    inv = (10000.0 ** (-np.arange(half, dtype=np.float32) / np.float32(half))).astype(np.float32)
    pos = np.arange(S, dtype=np.float32)
    ang = (inv[:, None] * pos[None, :]).astype(np.float32)
    cs = np.stack([np.cos(ang.astype(np.float64)), np.sin(ang.astype(np.float64))], 1).astype(np.float32)
    j = np.arange(128, dtype=np.float32)[:, None]
    i = np.arange(128, dtype=np.float32)[None, :]
    dmat = np.stack([np.maximum(i - j, 0), (j <= i).astype(np.float32),
                     np.maximum(j - i, 0), (j > i).astype(np.float32)], 0).astype(np.float32)
    rows = np.stack([np.broadcast_to(i + 1, (128, 128)), np.broadcast_to(128 - i, (128, 128))], 0).astype(np.float32)
    cols = np.concatenate([127 - j, j, np.full((128, 1), 128.0, np.float32)], 1).astype(np.float32)
    seqoff = ((np.arange(64) // 16) * S).astype(np.float32)[:, None]
    return dict(ident=np.eye(128, dtype=np.float32), cs=np.ascontiguousarray(cs), dmat=dmat,
                rows=np.ascontiguousarray(rows), cols=np.ascontiguousarray(cols), seqoff=seqoff)


def build(nseq, stop_after=None, debug=False):
    nc = bass.Bass("TRN2", target_bir_lowering=False)
    T = nseq * S
    NTL = nseq * NT

    def din(name, shape, dt=F32):
        return nc.dram_tensor(name, list(shape), dt, kind="ExternalInput").ap()

    x = din("x", [T, D])
    p_in = din("p", [2, T, PLE])
    norm_mix = din("norm_mix", [2, D])
    norm_ffn = din("norm_ffn", [2, D])
    norm_ple = din("norm_ple", [2, D])
    final_norm = din("final_norm", [1, D])
    conv_w_in = din("conv_w_in", [D, 3 * D])
    conv_wb = din("conv_wb", [4, D])
    conv_w_out = din("conv_w_out", [D, D])
    ret_w_in = din("ret_w_in", [D, 6 * D])
    ret_ld = din("ret_ld", [1, 8])
    ret_w_out = din("ret_w_out", [2 * D, D])
    router_w = din("router_w", [2, D, NE])
    w_gate = din("exp_w_gate", [2, NE, D, FF])
    w_up = din("exp_w_up", [2, NE, D, FF])
    w_down = din("exp_w_down", [2, NE, FF, D])
    ple_proj = din("ple_w_proj", [2, PLE, D])
    ple_gate = din("ple_w_gate", [2, D, D])
    c_ident = din("ident", [128, 128])
    c_cs = din("cs", [128, 2, S])
    c_dmat = din("dmat", [4, 128, 128])
    c_rows = din("rows", [2, 128, 128])
    c_cols = din("cols", [128, 3])
    c_seqoff = din("seqoff", [64, 1])
    out = nc.dram_tensor("out", [T, D], F32, kind="ExternalOutput").ap()
    H = nc.dram_tensor("Hres", [T, D], F32, kind="ExternalOutput" if debug else "Internal").ap()
    XN = nc.dram_tensor("XNs", [T, D], BF16, kind="Internal").ap()
    SBD = nc.dram_tensor("SBD", [16, 128, 1024], BF16, kind="Internal").ap()

    gst = ExitStack()
    SEMS["esem"] = {e: gst.enter_context(nc.semaphore(f"s_{e}")) for e in ENGS}
    SEMS["dsem"] = [gst.enter_context(nc.semaphore(f"d{i}")) for i in range(N_DMA_SEMS)]
    SEMS["ebase"] = {e: 0 for e in ENGS}
    SEMS["dcnt"] = [0] * N_DMA_SEMS
    identf = gst.enter_context(nc.sbuf_tensor("g_identf", [128, 128], F32))
    identb = gst.enter_context(nc.sbuf_tensor("g_identb", [128, 128], BF16))
    aff_tok = gst.enter_context(nc.sbuf_tensor("g_aff", [128, NT, 64], F32))
    epsn = gst.enter_context(nc.sbuf_tensor("g_eps", [128, 2], F32))

    ph = Phase(nc, "I")
    ph.op("sp", lambda e: e.dma_start(out=identf[:], in_=c_ident), w=["idf"], dma=True)
    ph.op("dve", lambda e: e.tensor_copy(out=identb[:], in_=identf[:]), r=["idf"], w=["idb"])
    ph.op("dve", lambda e: e.memset(aff_tok[:], 0.0), w=["aff"])
    ph.op("dve", lambda e: e.memset(epsn[:, 0:1], 1e-6), w=["eps0"])
    ph.op("dve", lambda e: e.memset(epsn[:, 1:2], 1e-5), w=["eps1"])
    ph.emit()

    def rmsnorm(ph, hin, hkey, gb, gkey, outt, okey, junk, sst, tag):
        ph.op("act", lambda e: e.activation(out=junk[:], in_=hin, func=AF.Square, accum_out=sst[:, 0:1]),
              r=[hkey], w=[("ss", tag)])
        ph.op("act", lambda e: e.activation(out=sst[:, 1:2], in_=sst[:, 0:1], func=AF.Sqrt, bias=epsn[:, 0:1], scale=1.0 / D),
              r=[("ss", tag)], w=[("rs", tag)])
        ph.op("dve", lambda e: e.reciprocal(out=sst[:, 2:3], in_=sst[:, 1:2]), r=[("rs", tag)], w=[("ri", tag)])
        ph.op("dve", lambda e: e.scalar_tensor_tensor(out=outt, in0=hin, scalar=sst[:, 2:3], in1=gb[:], op0=ALU.mult, op1=ALU.mult),
              r=[hkey, ("ri", tag), gkey], w=[okey])

    def transpose8(ph, src, skey, ptb, pkey, dst, dkey, eng, n=8):
        for k in range(n):
            ph.op("pe", lambda e, k=k: e.transpose(out=ptb[:, k, :], in_=src[:, k * 128:(k + 1) * 128], identity=identb[:]),
                  r=[skey], w=[pkey])
        if eng == "act":
            ph.op("act", lambda e: e.activation(out=dst, in_=ptb[:, 0:n, :], func=AF.Copy), r=[pkey], w=[dkey])
        else:
            ph.op("dve", lambda e: e.tensor_copy(out=dst, in_=ptb[:, 0:n, :]), r=[pkey], w=[dkey])

    def phase_A(s):
        ph = Phase(nc, f"A{s}")
        xnT = ph.sb("xnT", [128, 8, S], BF16)
        zT = ph.sb("zT", [128, 8, S], BF16)
        wout = ph.sb("wout", [128, 8, D], BF16)
        win = [ph.sb(f"win{i}", [128, 3, 8, 128], BF16) for i in range(2)]
        gmix = ph.sb("gmix", [128, D], F32)
        cw4 = ph.sb("cw4", [4, D], F32)
        cwb = ph.sb("cwb", [128, 8, 4], F32)
        xt = [ph.sb(f"xt{i}", [128, D], F32) for i in range(2)]
        junk = ph.sb("junk", [128, D], BF16)
        sst = [ph.sb(f"sst{i}", [128, 4], F32) for i in range(2)]
        xn = [ph.sb(f"xn{i}", [128, D], BF16) for i in range(2)]
        u = [ph.sb(f"u{i}", [128, S + 2], F32) for i in range(2)]
        bsb = [ph.sb(f"bsb{i}", [128, S], F32) for i in range(2)]
        csb = [ph.sb(f"csb{i}", [128, 512], F32) for i in range(2)]
        yc = [ph.sb(f"yc{i}", [128, S], F32) for i in range(2)]
        hn = [ph.sb(f"hn{i}", [128, D], F32) for i in range(2)]
        ptb = [ph.ps(f"ptb{i}", [128, 8, 128], BF16) for i in range(2)]
        PP = [ph.ps(f"pp{i}", [128, 1024], F32) for i in range(3)]

        ph.op("sp", lambda e: e.dma_start(out=gmix[:], in_=norm_mix[0:1, :].partition_broadcast(128)), w=["gmix"], dma=True)
        ph.op("sp", lambda e: e.dma_start(out=cw4[:], in_=conv_wb), w=["cw4"], dma=True)
        ph.op("pool", lambda e: e.dma_start(out=wout[:], in_=conv_w_out.rearrange("(k p) n -> p k n", p=128)), w=["wout"], dma=True)
        cps = PP[0][:, 0:32].rearrange("p (j w) -> p j w", w=4)
        for j in range(8):
            ph.op("pe", lambda e, j=j: e.transpose(out=cps[:, j, :], in_=cw4[:, j * 128:(j + 1) * 128], identity=identf[0:4, 0:4]),
                  r=["cw4"], w=[("pp", 0, 0)])
        ph.op("dve", lambda e: e.tensor_copy(out=cwb[:], in_=cps), r=[("pp", 0, 0)], w=["cwb"])
        for i in range(2):
            ph.op("dve", lambda e, i=i: e.memset(u[i][:, 0:1], 0.0), w=[("u", i)])
            ph.op("dve", lambda e, i=i: e.memset(u[i][:, S + 1:S + 2], 0.0), w=[("u", i)])
        for i in range(NT):
            b = i % 2
            r0 = s * S + i * 128
            ph.op("sp", lambda e, b=b, r0=r0: e.dma_start(out=xt[b][:], in_=x[r0:r0 + 128, :]), w=[("xt", b)], dma=True)
            rmsnorm(ph, xt[b][:], ("xt", b), gmix, "gmix", xn[b][:], ("xn", b), junk, sst[b], b)
            transpose8(ph, xn[b], ("xn", b), ptb[b], ("ptb", b), xnT[:, :, i * 128:(i + 1) * 128], ("xnT", i), "act")
        xnT_keys = [("xnT", i) for i in range(NT)]
        w_in_v = conv_w_in.rearrange("(k p) (t n) -> p t k n", p=128, t=3)
        for j in range(8):
            jb = j % 2
            ph.op("pool", lambda e, j=j, jb=jb: e.dma_start(out=win[jb][:], in_=w_in_v[:, :, :, j * 128:(j + 1) * 128]),
                  w=[("win", jb)], dma=True)
            for tb in range(4):
                q = (j * 4 + tb) % 2
                bps = PP[q][:, 0:512]
                cps_ = PP[q][:, 512:1024]
                vps = PP[2][:, q * 512:(q + 1) * 512]
                tks = [("xnT", i) for i in range(tb * 4, tb * 4 + 4)]
                for t, (dst, key) in enumerate([(bps, ("pp", q, 0)), (cps_, ("pp", q, 1)), (vps, ("pp", 2, q))]):
                    for k in range(8):
                        ph.op("pe", lambda e, dst=dst, t=t, k=k, jb=jb, tb=tb: e.matmul(
                            dst, lhsT=win[jb][:, t, k, :], rhs=xnT[:, k, tb * 512:(tb + 1) * 512], start=(k == 0), stop=(k == 7)),
                            r=[("win", jb)] + tks, w=[key])
                ph.op("act", lambda e, q=q, cps_=cps_: e.activation(out=csb[q][:], in_=cps_, func=AF.Copy), r=[("pp", q, 1)], w=[("csb", q)])
                ph.op("act", lambda e, jb=jb, tb=tb, bps=bps: e.activation(out=bsb[jb][:, tb * 512:(tb + 1) * 512], in_=bps, func=AF.Copy),
                      r=[("pp", q, 0)], w=[("bsb", jb)])
                ph.op("dve", lambda e, jb=jb, tb=tb, q=q, vps=vps: e.tensor_tensor(
                    out=u[jb][:, 1 + tb * 512:1 + (tb + 1) * 512], in0=csb[q][:], in1=vps, op=ALU.mult),
                    r=[("csb", q), ("pp", 2, q)], w=[("u", jb)])
            ph.op("act", lambda e, j=j, jb=jb: e.activation(out=yc[jb][:], in_=u[jb][:, 1:S + 1], func=AF.Identity,
                                                            bias=cwb[:, j, 3:4], scale=cwb[:, j, 1:2]),
                  r=[("u", jb), "cwb"], w=[("yc", jb)])
            ph.op("dve", lambda e, j=j, jb=jb: e.scalar_tensor_tensor(out=yc[jb][:], in0=u[jb][:, 0:S], scalar=cwb[:, j, 0:1], in1=yc[jb][:],
                                                                     op0=ALU.mult, op1=ALU.add),
                  r=[("u", jb), "cwb", ("yc", jb)], w=[("yc", jb)])
            ph.op("dve", lambda e, j=j, jb=jb: e.scalar_tensor_tensor(out=yc[jb][:], in0=u[jb][:, 2:S + 2], scalar=cwb[:, j, 2:3], in1=yc[jb][:],
                                                                      op0=ALU.mult, op1=ALU.add),
                  r=[("u", jb), "cwb", ("yc", jb)], w=[("yc", jb)])
            ph.op("pool", lambda e, j=j, jb=jb: e.tensor_tensor(out=zT[:, j, :], in0=bsb[jb][:], in1=yc[jb][:], op=ALU.mult),
                  r=[("bsb", jb), ("yc", jb)], w=[("zT", j)])
        zkeys = [("zT", j) for j in range(8)]
        for i in range(NT):
            b = i % 2
            r0 = s * S + i * 128
            ph.op("sp", lambda e, b=b, r0=r0: e.dma_start(out=xt[b][:], in_=x[r0:r0 + 128, :]), w=[("xt", b)], dma=True)
            for nh in range(2):
                for k in range(8):
                    ph.op("pe", lambda e, b=b, nh=nh, k=k, i=i: e.matmul(
                        PP[b][:, nh * 512:(nh + 1) * 512], lhsT=zT[:, k, i * 128:(i + 1) * 128], rhs=wout[:, k, nh * 512:(nh + 1) * 512],
                        start=(k == 0), stop=(k == 7)), r=zkeys + ["wout"], w=[("pp", b, nh)])
                ph.op("dve", lambda e, b=b, nh=nh: e.tensor_tensor(out=hn[b][:, nh * 512:(nh + 1) * 512], in0=xt[b][:, nh * 512:(nh + 1) * 512],
                                                                  in1=PP[b][:, nh * 512:(nh + 1) * 512], op=ALU.add),
                      r=[("xt", b), ("pp", b, nh)], w=[("hn", b, nh)])
            ph.op("sp", lambda e, b=b, r0=r0: e.dma_start(out=H[r0:r0 + 128, :], in_=hn[b][:]), r=[("hn", b, 0), ("hn", b, 1)], w=[("H", r0)], dma=True)
        ph.emit()

    def phase_R(l):
        ph = Phase(nc, f"R{l}")
        gffn = ph.sb("gffn", [128, D], F32)
        wr = ph.sb("wr", [128, 8, NE], BF16)
        hn = [ph.sb(f"hn{i}", [128, D], F32) for i in range(2)]
        junk = ph.sb("junk", [128, D], BF16)
        sst = [ph.sb(f"sst{i}", [128, 4], F32) for i in range(2)]
        xn = [ph.sb(f"xn{i}", [128, D], BF16) for i in range(2)]
        xT = [ph.sb(f"xT{i}", [128, 8, 128], BF16) for i in range(2)]
        sm = [ph.sb(f"sm{i}", [128, 4], F32) for i in range(2)]
        ex = [ph.sb(f"ex{i}", [128, NE], F32) for i in range(2)]
        ptb = [ph.ps(f"ptb{i}", [128, 8, 128], BF16) for i in range(2)]
        PL = [ph.ps(f"pl{i}", [128, NE], F32) for i in range(2)]
        ph.op("sp", lambda e: e.dma_start(out=gffn[:], in_=norm_ffn[l:l + 1, :].partition_broadcast(128)), w=["gffn"], dma=True)
        ph.op("pool", lambda e: e.dma_start(out=wr[:], in_=router_w[l].rearrange("(k p) n -> p k n", p=128)), w=["wr"], dma=True)
        for ti in range(NTL):
            b = ti % 2
            s, i = divmod(ti, NT)
            r0 = ti * 128
            ph.op("sp", lambda e, b=b, r0=r0: e.dma_start(out=hn[b][:], in_=H[r0:r0 + 128, :]), w=[("hn", b)], dma=True)
            rmsnorm(ph, hn[b][:], ("hn", b), gffn, "gffn", xn[b][:], ("xn", b), junk, sst[b], b)
            ph.op("sp", lambda e, b=b, r0=r0: e.dma_start(out=XN[r0:r0 + 128, :], in_=xn[b][:]), r=[("xn", b)], w=[("XN", r0)], dma=True)
            transpose8(ph, xn[b], ("xn", b), ptb[b], ("ptb", b), xT[b][:], ("xT", b), "act")
            for k in range(8):
                ph.op("pe", lambda e, b=b, k=k: e.matmul(PL[b][:], lhsT=xT[b][:, k, :], rhs=wr[:, k, :], start=(k == 0), stop=(k == 7)),
                      r=[("xT", b), "wr"], w=[("pl", b)])
            ph.op("dve", lambda e, b=b: e.reduce_max(out=sm[b][:, 0:1], in_=PL[b][:], axis=AX.X), r=[("pl", b)], w=[("mx", b)])
            ph.op("dve", lambda e, b=b: e.tensor_scalar(out=sm[b][:, 1:2], in0=sm[b][:, 0:1], scalar1=-1.0, scalar2=None, op0=ALU.mult),
                  r=[("mx", b)], w=[("nmx", b)])
            ph.op("act", lambda e, b=b: e.activation(out=ex[b][:], in_=PL[b][:], func=AF.Exp, bias=sm[b][:, 1:2], scale=1.0, accum_out=sm[b][:, 2:3]),
                  r=[("pl", b), ("nmx", b)], w=[("ex", b), ("sum", b)])
            ph.op("dve", lambda e, b=b: e.reciprocal(out=sm[b][:, 3:4], in_=sm[b][:, 2:3]), r=[("sum", b)], w=[("rsum", b)])
            ph.op("dve", lambda e, b=b, s=s, i=i: e.tensor_scalar(out=aff_tok[:, i, s * 16:(s + 1) * 16], in0=ex[b][:], scalar1=sm[b][:, 3:4],
                                                                 scalar2=None, op0=ALU.mult),
                  r=[("ex", b), ("rsum", b)], w=[("aff", ti)])
        ph.emit()

    def phase_P(l, final):
        ph = Phase(nc, f"P{l}")
        wpg = ph.sb("wpg", [128, 8, D], BF16)
        wpp = ph.sb("wpp", [128, 2, D], BF16)
        gple = ph.sb("gple", [128, D], F32)
        gnx = ph.sb("gnx", [128, D], F32)
        hn = [ph.sb(f"hn{i}", [128, D], F32) for i in range(2)]
        pt = [ph.sb(f"pt{i}", [128, PLE], F32) for i in range(2)]
        pb = [ph.sb(f"pb{i}", [128, PLE], BF16) for i in range(2)]
        pT = [ph.sb(f"pT{i}", [128, 2, 128], BF16) for i in range(2)]
        junk = ph.sb("junk", [128, D], BF16)
        sst = [ph.sb(f"sst{i}", [128, 4], F32) for i in range(4)]
        xn = [ph.sb(f"xn{i}", [128, D], BF16) for i in range(2)]
        xT = [ph.sb(f"xT{i}", [128, 8, 128], BF16) for i in range(2)]
        sgm = [ph.sb(f"sgm{i}", [128, D], F32) for i in range(2)]
        h2 = [ph.sb(f"h2{i}", [128, D], F32) for i in range(2)]
        xo = [ph.sb(f"xo{i}", [128, D], F32 if final else BF16) for i in range(2)]
        ptb = [ph.ps(f"ptb{i}", [128, 8, 128], BF16) for i in range(2)]
        ptp = ph.ps("ptp", [128, 8, 128], BF16)
        PG = ph.ps("pg", [128, 1024], F32)
        PQ = ph.ps("pq", [128, 1024], F32)
        ph.op("pool", lambda e: e.dma_start(out=wpg[:], in_=ple_gate[l].rearrange("(k p) n -> p k n", p=128)), w=["wpg"], dma=True)
        ph.op("pool", lambda e: e.dma_start(out=wpp[:], in_=ple_proj[l].rearrange("(k p) n -> p k n", p=128)), w=["wpp"], dma=True)
        ph.op("sp", lambda e: e.dma_start(out=gple[:], in_=norm_ple[l:l + 1, :].partition_broadcast(128)), w=["gple"], dma=True)
        gsrc = final_norm[0:1, :] if final else norm_mix[l + 1:l + 2, :]
        ph.op("sp", lambda e: e.dma_start(out=gnx[:], in_=gsrc.partition_broadcast(128)), w=["gnx"], dma=True)
        for ti in range(NTL):
            b = ti % 2
            r0 = ti * 128
            ph.op("sp", lambda e, b=b, r0=r0: e.dma_start(out=hn[b][:], in_=H[r0:r0 + 128, :]), w=[("hn", b)], dma=True)
            ph.op("sp", lambda e, b=b, r0=r0: e.dma_start(out=pt[b][:], in_=p_in[l, r0:r0 + 128, :]), w=[("pt", b)], dma=True)
            rmsnorm(ph, hn[b][:], ("hn", b), gple, "gple", xn[b][:], ("xn", b), junk, sst[b], b)
            transpose8(ph, xn[b], ("xn", b), ptb[b], ("ptb", b), xT[b][:], ("xT", b), "act")
            ph.op("pool", lambda e, b=b: e.tensor_copy(out=pb[b][:], in_=pt[b][:]), r=[("pt", b)], w=[("pb", b)])
            transpose8(ph, pb[b], ("pb", b), ptp, "ptp", pT[b][:], ("pT", b), "dve", n=2)
            for nh in range(2):
                for k in range(8):
                    ph.op("pe", lambda e, b=b, nh=nh, k=k: e.matmul(PG[:, nh * 512:(nh + 1) * 512], lhsT=xT[b][:, k, :],
                                                                   rhs=wpg[:, k, nh * 512:(nh + 1) * 512], start=(k == 0), stop=(k == 7)),
                          r=[("xT", b), "wpg"], w=[("pg", nh)])
                for k in range(2):
                    ph.op("pe", lambda e, b=b, nh=nh, k=k: e.matmul(PQ[:, nh * 512:(nh + 1) * 512], lhsT=pT[b][:, k, :],
                                                                   rhs=wpp[:, k, nh * 512:(nh + 1) * 512], start=(k == 0), stop=(k == 1)),
                          r=[("pT", b), "wpp"], w=[("pq", nh)])
                sl = slice(nh * 512, (nh + 1) * 512)
                ph.op("act", lambda e, b=b, sl=sl: e.activation(out=sgm[b][:, sl], in_=PG[:, sl], func=AF.Sigmoid), r=[("pg", nh)], w=[("sgm", b, nh)])
                ph.op("dve", lambda e, b=b, sl=sl: e.tensor_tensor(out=sgm[b][:, sl], in0=sgm[b][:, sl], in1=PQ[:, sl], op=ALU.mult),
                      r=[("sgm", b, nh), ("pq", nh)], w=[("sgm", b, nh)])
                ph.op("pool", lambda e, b=b, sl=sl: e.tensor_tensor(out=h2[b][:, sl], in0=sgm[b][:, sl], in1=hn[b][:, sl], op=ALU.add),
                      r=[("sgm", b, nh), ("hn", b)], w=[("h2", b, nh)])
            hk = [("h2", b, 0), ("h2", b, 1)]
            if not final:
                ph.op("sp", lambda e, b=b, r0=r0: e.dma_start(out=H[r0:r0 + 128, :], in_=h2[b][:]), r=hk, w=[("H", r0)], dma=True)
            ph.op("act", lambda e, b=b: e.activation(out=junk[:], in_=h2[b][:], func=AF.Square, accum_out=sst[2 + b][:, 0:1]),
                  r=hk, w=[("ss2", b)])
            ph.op("act", lambda e, b=b: e.activation(out=sst[2 + b][:, 1:2], in_=sst[2 + b][:, 0:1], func=AF.Sqrt, bias=epsn[:, 0:1], scale=1.0 / D),
                  r=[("ss2", b)], w=[("rs2", b)])
            ph.op("dve", lambda e, b=b: e.reciprocal(out=sst[2 + b][:, 2:3], in_=sst[2 + b][:, 1:2]), r=[("rs2", b)], w=[("ri2", b)])
            ph.op("dve", lambda e, b=b: e.scalar_tensor_tensor(out=xo[b][:], in0=h2[b][:], scalar=sst[2 + b][:, 2:3], in1=gnx[:], op0=ALU.mult, op1=ALU.mult),
                  r=hk + [("ri2", b), "gnx"], w=[("xo", b)])
            dst = out if final else XN
            ph.op("sp", lambda e, b=b, r0=r0, dst=dst: e.dma_start(out=dst[r0:r0 + 128, :], in_=xo[b][:]), r=[("xo", b)], w=[("O", r0)], dma=True)
        ph.emit()

    def phase_B(l):
        ph = Phase(nc, f"B{l}")
        NTOK = nseq * CAP
        ntile = nseq * 2
        TB = min(512, NTOK)
        nblk = NTOK // TB
        work = ph.sb("work", [64, S], F32)
        gates = ph.sb("gates", [64, CAP], F32)
        idxu = ph.sb("idxu", [64, CAP], U32)
        idxf = ph.sb("idxf", [64, CAP], F32)
        soff = ph.sb("soff", [64, 1], F32)
        gT = ph.sb("gT", [128, 2, 64], F32)
        iTi = ph.sb("iTi", [128, 2, 64], I32)
        ring = [ph.sb(f"ring{i}", [128, 8192], BF16) for i in range(4)]
        xg = [ph.sb(f"xg{i}", [128, ntile, D], BF16) for i in range(2)]
        xgT = ph.sb("xgT", [128, 8, NTOK], BF16)
        hT = ph.sb("hT", [128, 16, NTOK], BF16)
        sg = [ph.sb(f"sg{i}", [128, 512], F32) for i in range(2)]
        yt = [ph.sb(f"yt{i}", [128, D], F32) for i in range(2)]
        ptb = [ph.ps(f"ptb{i}", [128, 8, 128], BF16) for i in range(2)]
        PGU = [ph.ps(f"pgu{i}", [128, 1024], F32) for i in range(2)]
        PD = ph.ps("pd", [128, 1024], F32)

        ph.op("sp", lambda e: e.dma_start(out=soff[:], in_=c_seqoff), w=["soff"], dma=True)
        for g in range(4):
            for ii in range(4):
                i = g * 4 + ii
                ph.op("pe", lambda e, g=g, ii=ii, i=i: e.transpose(out=PGU[g % 2][0:64, ii * 128:(ii + 1) * 128], in_=aff_tok[:, i, :], identity=identf[:]),
                      r=["aff"], w=[("pgu", g % 2, 0)])
            ph.op("dve", lambda e, g=g: e.tensor_copy(out=work[:, g * 512:(g + 1) * 512], in_=PGU[g % 2][0:64, 0:512]),
                  r=[("pgu", g % 2, 0)], w=["work"])
        for r in range(CAP // 8):
            sl = slice(r * 8, (r + 1) * 8)
            ph.op("dve", lambda e, sl=sl: e.max(out=gates[:, sl], in_=work[:]), r=["work"], w=[("gt", r)])
            ph.op("dve", lambda e, sl=sl: e.max_index(out=idxu[:, sl], in_max=gates[:, sl], in_values=work[:]), r=["work", ("gt", r)], w=[("ix", r)])
            ph.op("dve", lambda e, sl=sl: e.match_replace(out=work[:], in_to_replace=gates[:, sl], in_values=work[:], imm_value=-1.0),
                  r=["work", ("gt", r)], w=["work"])
        gkeys = [("gt", r) for r in range(CAP // 8)]
        ikeys = [("ix", r) for r in range(CAP // 8)]
        ph.op("dve", lambda e: e.tensor_copy(out=idxf[:], in_=idxu[:]), r=ikeys, w=["idxf"])
        ph.op("dve", lambda e: e.tensor_scalar(out=idxf[:], in0=idxf[:], scalar1=soff[:, 0:1], scalar2=None, op0=ALU.add), r=["idxf", "soff"], w=["idxf"])
        tp = PGU[0][:, 0:256].rearrange("p (a c) -> p a c", c=64)
        for hf in range(2):
            ph.op("pe", lambda e, hf=hf: e.transpose(out=tp[:, hf, :], in_=gates[:, hf * 128:(hf + 1) * 128], identity=identf[0:64, 0:64]),
                  r=gkeys, w=[("pgu", 0, 0)])
            ph.op("pe", lambda e, hf=hf: e.transpose(out=tp[:, 2 + hf, :], in_=idxf[:, hf * 128:(hf + 1) * 128], identity=identf[0:64, 0:64]),
                  r=["idxf"], w=[("pgu", 0, 0)])
        ph.op("dve", lambda e: e.tensor_copy(out=gT[:], in_=tp[:, 0:2, :]), r=[("pgu", 0, 0)], w=["gT"])
        ph.op("act", lambda e: e.activation(out=iTi[:], in_=tp[:, 2:4, :], func=AF.Copy), r=[("pgu", 0, 0)], w=["iTi"])

        wg_v = w_gate[l].rearrange("e (k p) f -> e p k f", p=128)
        wu_v = w_up[l].rearrange("e (k p) f -> e p k f", p=128)
        wd_v = w_down[l].rearrange("e (k p) n -> e p k n", p=128)

        def load_slab(g):
            e_, j = divmod(g, 6)
            if e_ >= NE:
                return
            slot = g % 4
            if j < 4:
                dstg = ring[slot][:, 0:4096].rearrange("p (k n) -> p k n", n=512)
                dstu = ring[slot][:, 4096:8192].rearrange("p (k n) -> p k n", n=512)
                ph.op("pool", lambda e: e.dma_start(out=dstg, in_=wg_v[e_, :, :, j * 512:(j + 1) * 512]), w=[("ring", slot, 0)], dma=True)
                ph.op("pool", lambda e: e.dma_start(out=dstu, in_=wu_v[e_, :, :, j * 512:(j + 1) * 512]), w=[("ring", slot, 1)], dma=True)
            else:
                dh = j - 4
                dst = ring[slot][:].rearrange("p (k n) -> p k n", n=1024)
                ph.op("pool", lambda e: e.dma_start(out=dst, in_=wd_v[e_, :, dh * 8:(dh + 1) * 8, :]), w=[("ring", slot, 0), ("ring", slot, 1)], dma=True)

        def gather(e_):
            if e_ >= NE:
                return
            for t in range(ntile):
                s, hf = divmod(t, 2)
                col = s * 16 + e_
                ph.op("pool", lambda e, t=t, hf=hf, col=col: e.indirect_dma_start(
                    out=xg[e_ % 2][:, t, :], out_offset=None, in_=XN[:, :],
                    in_offset=bass.IndirectOffsetOnAxis(ap=iTi[:, hf, col:col + 1], axis=0)),
                    r=["iTi"], w=[("xg", e_ % 2, t)], dma=True)

        def transposes(e_):
            if e_ >= NE:
                return
            for t in range(ntile):
                transpose8(ph, xg[e_ % 2][:, t, :], ("xg", e_ % 2, t), ptb[t % 2], ("ptb", t % 2),
                           xgT[:, :, t * 128:(t + 1) * 128], ("xgT", t), "act" if t % 2 == 0 else "dve")

        for g in range(4):
            load_slab(g)
        gather(0)
        transposes(0)
        gather(1)
        prev_sc = {}
        cur_sc = {}
        cnt = 0
        for e_ in range(NE):
            for fg in range(4):
                g = e_ * 6 + fg
                slot = g % 4
                sv = ring[slot][:].rearrange("p (a k n) -> p a k n", a=2, k=8)
                for fc4 in range(4):
                    fc = fg * 4 + fc4
                    for hb in range(nblk):
                        q = cnt % 2
                        cnt += 1
                        tks = [("xgT", t) for t in range(hb * (TB // 128), (hb + 1) * (TB // 128))]
                        for a in range(2):
                            for k in range(8):
                                ph.op("pe", lambda e, q=q, a=a, k=k, sv=sv, fc4=fc4, hb=hb: e.matmul(
                                    PGU[q][:, a * 512:a * 512 + TB], lhsT=sv[:, a, k, fc4 * 128:(fc4 + 1) * 128],
                                    rhs=xgT[:, k, hb * TB:(hb + 1) * TB], start=(k == 0), stop=(k == 7)),
                                    r=[("ring", slot, a)] + tks, w=[("pgu", q, a)])
                        ph.op("act", lambda e, q=q: e.activation(out=sg[q][:, 0:TB], in_=PGU[q][:, 0:TB], func=AF.Silu), r=[("pgu", q, 0)], w=[("sg", q)])
                        ph.op("dve", lambda e, q=q, fc=fc, hb=hb: e.tensor_tensor(out=hT[:, fc, hb * TB:(hb + 1) * TB], in0=sg[q][:, 0:TB],
                                                                                 in1=PGU[q][:, 512:512 + TB], op=ALU.mult),
                              r=[("sg", q), ("pgu", q, 1)], w=[("hT", fc, hb)])
                load_slab(g + 4)
            transposes(e_ + 1)
            gather(e_ + 2)
            d0 = ring[(e_ * 6 + 4) % 4][:].rearrange("p (k n) -> p k n", n=1024)
            d1 = ring[(e_ * 6 + 5) % 4][:].rearrange("p (k n) -> p k n", n=1024)
            dkeys = [("ring", (e_ * 6 + 4) % 4, 0), ("ring", (e_ * 6 + 4) % 4, 1), ("ring", (e_ * 6 + 5) % 4, 0), ("ring", (e_ * 6 + 5) % 4, 1)]
            for t in range(ntile):
                s, hf = divmod(t, 2)
                col = s * 16 + e_
                hb = (t * 128) // TB
                b = t % 2
                for nh in range(2):
                    for fc in range(16):
                        dsl = d0 if fc < 8 else d1
                        ph.op("pe", lambda e, nh=nh, fc=fc, dsl=dsl, t=t: e.matmul(
                            PD[:, nh * 512:(nh + 1) * 512], lhsT=hT[:, fc, t * 128:(t + 1) * 128], rhs=dsl[:, fc % 8, nh * 512:(nh + 1) * 512],
                            start=(fc == 0), stop=(fc == 15)), r=dkeys + [("hT", fc_, hb) for fc_ in range(16)], w=[("pd", nh)])
                    sl = slice(nh * 512, (nh + 1) * 512)
                    if nh == 0:
                        ph.op("act", lambda e, b=b, sl=sl, hf=hf, col=col: e.activation(out=yt[b][:, sl], in_=PD[:, sl], func=AF.Copy, scale=gT[:, hf, col:col + 1]),
                              r=[("pd", nh), "gT"], w=[("yt", b, nh)])
                    else:
                        ph.op("dve", lambda e, b=b, sl=sl, hf=hf, col=col: e.tensor_scalar(out=yt[b][:, sl], in0=PD[:, sl], scalar1=gT[:, hf, col:col + 1],
                                                                                          scalar2=None, op0=ALU.mult),
                              r=[("pd", nh), "gT"], w=[("yt", b, nh)])
                o = ph.op("pool", lambda e, b=b, hf=hf, col=col: e.indirect_dma_start(
                    out=H[:, :], out_offset=bass.IndirectOffsetOnAxis(ap=iTi[:, hf, col:col + 1], axis=0),
                    in_=yt[b][:, :], in_offset=None, compute_op=ALU.add),
                    r=[("yt", b, 0), ("yt", b, 1), "iTi"], w=[], dma=True, after=prev_sc.get(s, []))
                cur_sc.setdefault(s, []).append(o)
            prev_sc = cur_sc
            cur_sc = {}
            load_slab(e_ * 6 + 4 + 4)
            load_slab(e_ * 6 + 5 + 4)
        ph.emit()

    def phase_C(s):
        ph = Phase(nc, f"C{s}")
        xnT = ph.sb("xnT", [128, 8, S], BF16)
        xl = [ph.sb(f"xl{i}", [128, D], BF16) for i in range(2)]
        ws = [ph.sb(f"ws{i}", [128, 4096], BF16) for i in range(4)]
        cs = [ph.sb(f"cs{i}", [128, 2, 512], F32) for i in range(2)]
        qT = ph.sb("qT", [128, 2, S], BF16)
        kT = ph.sb("kT", [128, 2, S], BF16)
        kf = ph.sb("kf", [128, NT, 256], BF16)
        kb = ph.sb("kb", [128, NT, 256], BF16)
        vt = ph.sb("vt", [128, NT, 512], BF16)
        rt = [ph.sb(f"rt{i}", [128, 512], F32) for i in range(4)]
        Sf32 = ph.sb("Sf32", [128, 1024], F32)
        Sb32 = ph.sb("Sb32", [128, 1024], F32)
        Sfb = [ph.sb(f"Sfb{i}", [128, 1024], BF16) for i in range(2)]
        Sbst = [ph.sb(f"Sbst{i}", [128, 1024], BF16) for i in range(2)]
        Sbin = [ph.sb(f"Sbin{i}", [128, 1024], BF16) for i in range(3)]
        ldb = ph.sb("ldb", [128, 8], F32)
        Mt = ph.sb("Mt", [128, 4, 128], F32)
        tmpM = [ph.sb(f"tmpM{i}", [128, 128], F32) for i in range(2)]
        qd = ph.sb("qd", [128, 4, 2, 128], F32)
        kd = ph.sb("kd", [128, 4, 2], F32)
        cdc = ph.sb("cdc", [128, 4, 2], F32)
        dm = ph.sb("dm", [128, 4, 128], F32)
        rw = ph.sb("rw", [128, 2, 128], F32)
        cl = ph.sb("cl", [128, 3], F32)
        Pm = [ph.sb(f"Pm{i}", [128, 128], BF16) for i in range(2)]
        qfb = [ph.sb(f"qfb{i}", [128, 2, 2, 128], BF16) for i in range(2)]
        sgl = [ph.sb(f"sgl{i}", [128, 512], F32) for i in range(2)]
        on = [ph.sb(f"on{i}", [128, 512], F32) for i in range(2)]
        go = [ph.sb(f"go{i}", [128, 512], BF16) for i in range(2)]
        goT = [ph.sb(f"goT{i}", [128, 4, 128], BF16) for i in range(2)]
        mo = [ph.sb(f"mo{i}", [128, D], F32) for i in range(2)]
        bst = [ph.sb(f"bst{i}", [128, 6], F32) for i in range(2)]
        mv = [ph.sb(f"mv{i}", [128, 4], F32) for i in range(2)]
        ptb = ph.ps("ptb", [128, 8, 128], BF16)
        PG = ph.ps("pg", [128, 512], F32)
        PA = ph.ps("pa", [128, 1024], F32)
        PB = ph.ps("pb", [128, 1024], F32)
        PO = ph.ps("po", [128, 512], F32)
        PSC = ph.ps("psc", [128, 128], F32)
        ptk = ptb[:].rearrange("p a b -> p (a b)").rearrange("p (a b) -> p a b", b=256)

        ph.op("sp", lambda e: e.dma_start(out=ldb[:], in_=ret_ld.partition_broadcast(128)), w=["ldb"], dma=True)
        ph.op("sp", lambda e: e.dma_start(out=dm[:], in_=c_dmat.rearrange("a j i -> j a i")), w=["dm"], dma=True)
        ph.op("sp", lambda e: e.dma_start(out=rw[:], in_=c_rows.rearrange("a p i -> p a i")), w=["rw"], dma=True)
        ph.op("sp", lambda e: e.dma_start(out=cl[:], in_=c_cols), w=["cl"], dma=True)
        for h in range(4):
            ph.op("act", lambda e, h=h: e.activation(out=tmpM[0][:], in_=dm[:, 0, :], func=AF.Exp, scale=ldb[:, h:h + 1]), r=["dm", "ldb"], w=["tmA"])
            ph.op("dve", lambda e, h=h: e.tensor_tensor(out=tmpM[0][:], in0=tmpM[0][:], in1=dm[:, 1, :], op=ALU.mult), r=["tmA", "dm"], w=["tmA"])
            ph.op("act", lambda e, h=h: e.activation(out=tmpM[1][:], in_=dm[:, 2, :], func=AF.Exp, scale=ldb[:, 4 + h:5 + h]), r=["dm", "ldb"], w=["tmB"])
            ph.op("dve", lambda e, h=h: e.tensor_tensor(out=tmpM[1][:], in0=tmpM[1][:], in1=dm[:, 3, :], op=ALU.mult), r=["tmB", "dm"], w=["tmB"])
            ph.op("dve", lambda e, h=h: e.tensor_tensor(out=Mt[:, h, :], in0=tmpM[0][:], in1=tmpM[1][:], op=ALU.add), r=["tmA", "tmB"], w=[("Mt", h)])
            for d_ in range(2):
                lc = ldb[:, 4 * d_ + h:4 * d_ + h + 1]
                ph.op("act", lambda e, h=h, d_=d_, lc=lc: e.activation(out=qd[:, h, d_, :], in_=rw[:, d_, :], func=AF.Exp, scale=lc), r=["rw", "ldb"], w=[("qd", h)])
                ph.op("act", lambda e, h=h, d_=d_, lc=lc: e.activation(out=kd[:, h, d_:d_ + 1], in_=cl[:, d_:d_ + 1], func=AF.Exp, scale=lc), r=["cl", "ldb"], w=[("kd", h)])
                ph.op("act", lambda e, h=h, d_=d_, lc=lc: e.activation(out=cdc[:, h, d_:d_ + 1], in_=cl[:, 2:3], func=AF.Exp, scale=lc), r=["cl", "ldb"], w=[("cdc", h)])
        for i in range(NT):
            b = i % 2
            r0 = s * S + i * 128
            ph.op("sp", lambda e, b=b, r0=r0: e.dma_start(out=xl[b][:], in_=XN[r0:r0 + 128, :]), w=[("xl", b)], dma=True)
            transpose8(ph, xl[b], ("xl", b), ptb, "ptb", xnT[:, :, i * 128:(i + 1) * 128], ("xnT", i), "act" if b == 0 else "dve")
        allx = [("xnT", i) for i in range(NT)]
        if CCUT == "c1":
            ph.emit()
            return
        w_in_v = ret_w_in.rearrange("(k p) n -> p k n", p=128)
        w_out_v = ret_w_out.rearrange("(k p) n -> p k n", p=128)
        ws0v = ws[0][:].rearrange("p (k n) -> p k n", n=512)
        ws1v = ws[1][:].rearrange("p (k n) -> p k n", n=512)
        ws2v = ws[2][:].rearrange("p (k n) -> p k n", n=512)
        ws3v = ws[3][:].rearrange("p (k n) -> p k n", n=1024)
        prev_acc = {}
        cnt = 0
        for h in range(4):
            ph.op("pool", lambda e, h=h: e.dma_start(out=ws0v[:, :, 0:256], in_=w_in_v[:, :, h * 256:(h + 1) * 256]), w=[("ws0", 0)], dma=True)
            ph.op("pool", lambda e, h=h: e.dma_start(out=ws0v[:, :, 256:512], in_=w_in_v[:, :, 1024 + h * 256:1024 + (h + 1) * 256]), w=[("ws0", 1)], dma=True)
            ph.op("pool", lambda e, h=h: e.dma_start(out=ws1v, in_=w_in_v[:, :, 2048 + h * 512:2048 + (h + 1) * 512]), w=["ws1"], dma=True)
            ph.op("pool", lambda e, h=h: e.dma_start(out=ws2v, in_=w_in_v[:, :, 4096 + h * 512:4096 + (h + 1) * 512]), w=["ws2"], dma=True)
            ph.op("pool", lambda e, h=h: e.dma_start(out=ws3v, in_=w_out_v[:, h * 4:(h + 1) * 4, :]), w=["ws3"], dma=True)
            for tb in range(4):
                cb_ = tb % 2
                ph.op("sp", lambda e, cb_=cb_, tb=tb: e.dma_start(out=cs[cb_][:], in_=c_cs[:, :, tb * 512:(tb + 1) * 512]), w=[("cs", cb_)], dma=True)
                tks = [("xnT", i) for i in range(tb * 4, tb * 4 + 4)]
                for which in range(2):
                    PX, pk = (PA, "pa") if cnt % 2 == 0 else (PB, "pb")
                    cnt += 1
                    off = which * 256
                    for dc in range(2):
                        for k in range(8):
                            ph.op("pe", lambda e, PX=PX, dc=dc, k=k, off=off, tb=tb: e.matmul(
                                PX[:, dc * 512:(dc + 1) * 512], lhsT=ws0v[:, k, off + dc * 128:off + (dc + 1) * 128],
                                rhs=xnT[:, k, tb * 512:(tb + 1) * 512], start=(k == 0), stop=(k == 7)),
                                r=[("ws0", which)] + tks, w=[(pk, dc)])
                    sc = 1.0 if which == 0 else 1.0 / 16.0
                    combos = [(0, 0), (1, 1), (0, 1), (1, 0)]
                    for ri, (xi, ci) in enumerate(combos):
                        ph.op("dve", lambda e, PX=PX, ri=ri, xi=xi, ci=ci, sc=sc, cb_=cb_: e.scalar_tensor_tensor(
                            out=rt[ri][:], in0=PX[:, xi * 512:(xi + 1) * 512], scalar=sc, in1=cs[cb_][:, ci, :], op0=ALU.mult, op1=ALU.mult),
                            r=[(pk, xi), ("cs", cb_)], w=[("rt", ri)])
                    dstT, dk = (qT, "qT") if which == 0 else (kT, "kT")
                    ph.op("pool", lambda e, dstT=dstT, tb=tb: e.tensor_tensor(out=dstT[:, 0, tb * 512:(tb + 1) * 512], in0=rt[0][:], in1=rt[1][:], op=ALU.subtract),
                          r=[("rt", 0), ("rt", 1)], w=[(dk, tb, 0)])
                    ph.op("pool", lambda e, dstT=dstT, tb=tb: e.tensor_tensor(out=dstT[:, 1, tb * 512:(tb + 1) * 512], in0=rt[2][:], in1=rt[3][:], op=ALU.add),
                          r=[("rt", 2), ("rt", 3)], w=[(dk, tb, 1)])
            if CCUT == "c2":
                break
            for i in range(NT):
                PV, pk = (PG, "pg") if i % 2 == 0 else (PO, "po")
                for k in range(8):
                    ph.op("pe", lambda e, PV=PV, i=i, k=k: e.matmul(PV[:], lhsT=xnT[:, k, i * 128:(i + 1) * 128], rhs=ws1v[:, k, :], start=(k == 0), stop=(k == 7)),
                          r=[("xnT", i), "ws1"], w=[pk])
                ph.op("act", lambda e, PV=PV, i=i: e.activation(out=vt[:, i, :], in_=PV[:], func=AF.Copy), r=[pk], w=[("vt", i)])
            if CCUT == "c3":
                break
            for g4 in range(4):
                for ii in range(4):
                    i = g4 * 4 + ii
                    for dc in range(2):
                        ph.op("pe", lambda e, ii=ii, dc=dc, i=i: e.transpose(out=ptk[:, ii, dc * 128:(dc + 1) * 128], in_=kT[:, dc, i * 128:(i + 1) * 128], identity=identb[:]),
                              r=[("kT", g4, dc)], w=["ptb"])
                ph.op("act", lambda e, g4=g4, h=h: e.activation(out=kf[:, g4 * 4:(g4 + 1) * 4, :], in_=ptk, func=AF.Copy, scale=kd[:, h, 0:1]),
                      r=["ptb", ("kd", h)], w=[("kf", g4)])
                ph.op("dve", lambda e, g4=g4, h=h: e.tensor_scalar(out=kb[:, g4 * 4:(g4 + 1) * 4, :], in0=ptk, scalar1=kd[:, h, 1:2], scalar2=None, op0=ALU.mult),
                      r=["ptb", ("kd", h), ("kf", g4)], w=[("kb", g4)])
            if CCUT == "proj":
                break
            ph.op("dve", lambda e: e.memset(Sb32[:], 0.0), w=["Sb32"])
            for c in range(NT - 1, 0, -1):
                PX, pk = (PA, "pa") if c % 2 == 0 else (PB, "pb")
                for dc in range(2):
                    ph.op("pe", lambda e, PX=PX, dc=dc, c=c: e.matmul(PX[:, dc * 512:(dc + 1) * 512], lhsT=kb[:, c, dc * 128:(dc + 1) * 128], rhs=vt[:, c, :], start=True, stop=True),
                          r=[("kb", c // 4), ("vt", c)], w=[(pk, dc)])
                for dc in range(2):
                    ph.op("dve", lambda e, PX=PX, h=h, dc=dc: e.scalar_tensor_tensor(out=Sb32[:, dc * 512:(dc + 1) * 512], in0=Sb32[:, dc * 512:(dc + 1) * 512],
                                                                              scalar=cdc[:, h, 1:2], in1=PX[:, dc * 512:(dc + 1) * 512], op0=ALU.mult, op1=ALU.add),
                          r=["Sb32", (pk, dc), ("cdc", h)], w=["Sb32"])
                ph.op("act", lambda e, c=c: e.activation(out=Sbst[c % 2][:], in_=Sb32[:], func=AF.Copy), r=["Sb32"], w=[("Sbst", c % 2)])
                ph.op("sp", lambda e, c=c: e.dma_start(out=SBD[c - 1], in_=Sbst[c % 2][:]), r=[("Sbst", c % 2)], w=[("SBD", c - 1)], dma=True)
            if CCUT == "bwd":
                break
            ph.op("dve", lambda e: e.memset(Sf32[:], 0.0), w=["Sf32"])

            def prefetch(c):
                if c < NT - 1:
                    ph.op("sp", lambda e, c=c: e.dma_start(out=Sbin[c % 3][:], in_=SBD[c]), r=[("SBD", c)], w=[("Sbin", c % 3)], dma=True)

            def tail(c):
                b = c % 2
                r0 = s * S + c * 128
                for vc in range(4):
                    ph.op("pe", lambda e, b=b, vc=vc: e.transpose(out=ptb[:, vc, :], in_=go[b][:, vc * 128:(vc + 1) * 128], identity=identb[:]),
                          r=[("go", b)], w=["ptb"])
                ph.op("act", lambda e, b=b: e.activation(out=goT[b][:], in_=ptb[:, 0:4, :], func=AF.Copy), r=["ptb"], w=[("goT", b)])
                for nh in range(2):
                    for vc in range(4):
                        ph.op("pe", lambda e, b=b, nh=nh, vc=vc: e.matmul(PB[:, nh * 512:(nh + 1) * 512], lhsT=goT[b][:, vc, :], rhs=ws3v[:, vc, nh * 512:(nh + 1) * 512],
                                                                         start=(vc == 0), stop=(vc == 3)),
                              r=[("goT", b), "ws3"], w=[("pb", nh)])
                ph.op("act", lambda e, b=b: e.activation(out=mo[b][:, 0:512], in_=PB[:, 0:512], func=AF.Copy), r=[("pb", 0)], w=[("mo", b, 0)])
                ph.op("dve", lambda e, b=b: e.tensor_copy(out=mo[b][:, 512:1024], in_=PB[:, 512:1024]), r=[("pb", 1)], w=[("mo", b, 1)])
                o = ph.op("pool", lambda e, b=b, r0=r0: e.dma_start(out=H[r0:r0 + 128, :], in_=mo[b][:], accum_op=ALU.add),
                          r=[("mo", b, 0), ("mo", b, 1)], w=[], dma=True, after=prev_acc.get(c, []))
                prev_acc[c] = [o]

            prefetch(0)
            prefetch(1)
            for c in range(NT):
                b = c % 2
                tbk = c // 4
                if c < NT - 1:
                    for dc in range(2):
                        ph.op("pe", lambda e, dc=dc, c=c: e.matmul(PA[:, dc * 512:(dc + 1) * 512], lhsT=kf[:, c, dc * 128:(dc + 1) * 128], rhs=vt[:, c, :], start=True, stop=True),
                              r=[("kf", c // 4), ("vt", c)], w=[("pa", dc)])
                for dc in range(2):
                    ph.op("pe", lambda e, dc=dc, c=c: e.matmul(PSC[:], lhsT=kT[:, dc, c * 128:(c + 1) * 128], rhs=qT[:, dc, c * 128:(c + 1) * 128], start=(dc == 0), stop=(dc == 1)),
                          r=[("kT", tbk, 0), ("kT", tbk, 1), ("qT", tbk, 0), ("qT", tbk, 1)], w=["psc"])
                ph.op("dve", lambda e, b=b, h=h: e.tensor_tensor(out=Pm[b][:], in0=PSC[:], in1=Mt[:, h, :], op=ALU.mult), r=["psc", ("Mt", h)], w=[("Pm", b)])
                for d_ in range(2):
                    for dc in range(2):
                        ph.op("pool", lambda e, b=b, d_=d_, dc=dc, c=c, h=h: e.tensor_tensor(out=qfb[b][:, d_, dc, :], in0=qT[:, dc, c * 128:(c + 1) * 128],
                                                                                           in1=qd[:, h, d_, :], op=ALU.mult),
                              r=[("qT", tbk, dc), ("qd", h)], w=[("qfb", b, d_)])
                for k in range(8):
                    ph.op("pe", lambda e, c=c, k=k: e.matmul(PG[:], lhsT=xnT[:, k, c * 128:(c + 1) * 128], rhs=ws2v[:, k, :], start=(k == 0), stop=(k == 7)),
                          r=[("xnT", c), "ws2"], w=["pg"])
                ph.op("act", lambda e, b=b: e.activation(out=sgl[b][:], in_=PG[:], func=AF.Silu), r=["pg"], w=[("sgl", b)])
                mms = [(Pm[b][:], vt[:, c, :], [("Pm", b), ("vt", c)])]
                if c > 0:
                    for dc in range(2):
                        mms.append((qfb[b][:, 0, dc, :], Sfb[c % 2][:, dc * 512:(dc + 1) * 512], [("qfb", b, 0), ("Sfb", c % 2)]))
                if c < NT - 1:
                    for dc in range(2):
                        mms.append((qfb[b][:, 1, dc, :], Sbin[c % 3][:, dc * 512:(dc + 1) * 512], [("qfb", b, 1), ("Sbin", c % 3)]))
                for mi, (l_, r_, ks) in enumerate(mms):
                    ph.op("pe", lambda e, l_=l_, r_=r_, mi=mi, n=len(mms): e.matmul(PO[:], lhsT=l_, rhs=r_, start=(mi == 0), stop=(mi == n - 1)), r=ks, w=["po"])
                if c < NT - 1:
                    for dc in range(2):
                        ph.op("dve", lambda e, h=h, dc=dc: e.scalar_tensor_tensor(out=Sf32[:, dc * 512:(dc + 1) * 512], in0=Sf32[:, dc * 512:(dc + 1) * 512],
                                                                              scalar=cdc[:, h, 0:1], in1=PA[:, dc * 512:(dc + 1) * 512], op0=ALU.mult, op1=ALU.add),
                              r=["Sf32", ("pa", dc), ("cdc", h)], w=["Sf32"])
                    ph.op("act", lambda e, c=c: e.activation(out=Sfb[(c + 1) % 2][:], in_=Sf32[:], func=AF.Copy), r=["Sf32"], w=[("Sfb", (c + 1) % 2)])
                ph.op("dve", lambda e, b=b: e.bn_stats(out=bst[b][:], in_=PO[:]), r=["po"], w=[("bst", b)])
                ph.op("dve", lambda e, b=b: e.bn_aggr(out=mv[b][:, 0:2], in_=bst[b][:]), r=[("bst", b)], w=[("mv", b)])
                ph.op("act", lambda e, b=b: e.activation(out=mv[b][:, 2:3], in_=mv[b][:, 1:2], func=AF.Sqrt, bias=epsn[:, 1:2], scale=1.0), r=[("mv", b)], w=[("sd", b)])
                ph.op("dve", lambda e, b=b: e.reciprocal(out=mv[b][:, 3:4], in_=mv[b][:, 2:3]), r=[("sd", b)], w=[("rsd", b)])
                ph.op("dve", lambda e, b=b: e.tensor_scalar(out=on[b][:], in0=PO[:], scalar1=mv[b][:, 0:1], scalar2=mv[b][:, 3:4], op0=ALU.subtract, op1=ALU.mult),
                      r=["po", ("mv", b), ("rsd", b)], w=[("on", b)])
                ph.op("pool", lambda e, b=b: e.tensor_tensor(out=go[b][:], in0=on[b][:], in1=sgl[b][:], op=ALU.mult), r=[("on", b), ("sgl", b)], w=[("go", b)])
                prefetch(c + 2)
                if c > 0 and CCUT != "notail":
                    tail(c - 1)
            if CCUT != "notail":
                tail(NT - 1)
        ph.emit()

    sched = []
    for s in range(nseq):
        sched.append(("A", lambda s=s: phase_A(s)))
    sched.append(("R0", lambda: phase_R(0)))
    sched.append(("B0", lambda: phase_B(0)))
    sched.append(("P0", lambda: phase_P(0, False)))
    for s in range(nseq):
        sched.append(("C", lambda s=s: phase_C(s)))
    sched.append(("R1", lambda: phase_R(1)))
    sched.append(("B1", lambda: phase_B(1)))
    sched.append(("P1", lambda: phase_P(1, True)))
    only = _os.environ.get("SCHED_ONLY", "")
    for name, fn in sched:
        if only and name != only:
            continue
        fn()
        if stop_after is not None and name == stop_after:
            break
    gst.close()
    dbg = {"H": H}
    return nc, dbg


def make_in_maps(inputs, nseq, ncores):
    c = _consts()
    f32 = np.float32
    x = np.asarray(inputs["x"], f32)
    p = np.asarray(inputs["p"], f32)
    shared = dict(
        norm_mix=np.asarray(inputs["norm_mix"], f32), norm_ffn=np.asarray(inputs["norm_ffn"], f32),
        norm_ple=np.asarray(inputs["norm_ple"], f32), final_norm=np.asarray(inputs["final_norm"], f32).reshape(1, D),
        conv_w_in=np.asarray(inputs["conv_w_in"], f32)[0],
        conv_wb=np.ascontiguousarray(np.concatenate([np.asarray(inputs["conv_w"], f32)[0], np.asarray(inputs["conv_b"], f32)], 0)),
        conv_w_out=np.asarray(inputs["conv_w_out"], f32)[0],
        ret_w_in=np.asarray(inputs["ret_w_in"], f32)[0], ret_ld=np.asarray(inputs["ret_log_decay"], f32).reshape(1, 8),
        ret_w_out=np.asarray(inputs["ret_w_out"], f32)[0], router_w=np.asarray(inputs["router_w"], f32),
        exp_w_gate=np.asarray(inputs["exp_w_gate"], f32), exp_w_up=np.asarray(inputs["exp_w_up"], f32),
        exp_w_down=np.asarray(inputs["exp_w_down"], f32), ple_w_proj=np.asarray(inputs["ple_w_proj"], f32),
        ple_w_gate=np.asarray(inputs["ple_w_gate"], f32), **c)
    maps = []
    for ci in range(ncores):
        m = dict(shared)
        m["x"] = np.ascontiguousarray(x[ci * nseq:(ci + 1) * nseq].reshape(nseq * S, D))
        m["p"] = np.ascontiguousarray(p[:, ci * nseq:(ci + 1) * nseq].reshape(2, nseq * S, PLE))
        maps.append(m)
    return maps


def kernel(**inputs):
    B = np.asarray(inputs["x"]).shape[0]
    nseq = B // NCORES
    nc, _ = build(nseq)
    maps = make_in_maps(inputs, nseq, NCORES)
    res = run_bass_kernel_spmd(nc, maps, core_ids=list(range(NCORES)))
    outs = [np.asarray(r["out"], np.float32).reshape(nseq, S, D) for r in res.results]
    return np.concatenate(outs, 0)
```

```python
import numpy as np
from contextlib import ExitStack
import concourse.bass as bass
import concourse.mybir as mybir
from concourse.bass_utils import run_bass_kernel_spmd

F32 = mybir.dt.float32
BF16 = mybir.dt.bfloat16
I32 = mybir.dt.int32
U32 = mybir.dt.uint32
AF = mybir.ActivationFunctionType
ALU = mybir.AluOpType
AX = mybir.AxisListType

ENGS = ["pe", "act", "dve", "pool", "sp"]
N_DMA_SEMS = 20
SEMS = {}


class Op:
    __slots__ = ("eng", "fn", "waits", "idx", "flag", "cnt", "dsem", "dval", "is_dma", "pre")

    def __init__(self, eng, fn, idx, is_dma):
        self.eng = eng
        self.fn = fn
        self.idx = idx
        self.is_dma = is_dma
        self.flag = False
        self.cnt = None
        self.waits = []
        self.dsem = None
        self.dval = None
        self.pre = None


class Phase:
    def __init__(self, nc, name):
        self.nc = nc
        self.name = name
        self.q = {e: [] for e in ENGS}
        self.last_w = {}
        self.readers = {}
        self.stack = ExitStack()
        self.dma_rr = 0
        self.dma_last = [None] * N_DMA_SEMS
        self.dma_cnt = list(SEMS["dcnt"])
        self.n_ops = 0

    def sb(self, name, shape, dt):
        return self.stack.enter_context(self.nc.sbuf_tensor(f"{self.name}_{name}", list(shape), dt))

    def ps(self, name, shape, dt=F32):
        return self.stack.enter_context(self.nc.psum_tensor(f"{self.name}_{name}", list(shape), dt))

    def op(self, eng, fn, r=(), w=(), dma=False, after=(), pe_acc=False):
        o = Op(eng, fn, len(self.q[eng]), dma)
        deps = []
        for k in r:
            lw = self.last_w.get(k)
            if lw is not None:
                deps.append(lw)
        for k in w:
            lw = self.last_w.get(k)
            if lw is not None:
                deps.append(lw)
            deps.extend(self.readers.get(k, ()))
        deps.extend(after)
        seen = set()
        for d in deps:
            if d is o or id(d) in seen:
                continue
            seen.add(id(d))
            if d.eng == "pe" and eng == "pe" and not d.is_dma:
                continue
            o.waits.append(d)
            if not d.is_dma:
                d.flag = True
        if dma:
            s = self.dma_rr % N_DMA_SEMS
            self.dma_rr += 1
            o.pre = self.dma_last[s]
            self.dma_cnt[s] += 1
            o.dsem = s
            o.dval = 16 * self.dma_cnt[s]
            self.dma_last[s] = o
        for k in w:
            self.last_w[k] = o
            self.readers[k] = []
        for k in r:
            if k not in w:
                self.readers.setdefault(k, []).append(o)
        self.q[eng].append(o)
        self.n_ops += 1
        return o

    def emit(self):
        nc = self.nc
        st = self.stack
        esem = SEMS["esem"]
        dsem = SEMS["dsem"]
        ebase = dict(SEMS["ebase"])
        dbase = [16 * c for c in SEMS["dcnt"]]
        final = {}
        for e in ENGS:
            comp = [o for o in self.q[e] if not o.is_dma]
            if comp:
                comp[-1].flag = True
            c = ebase[e]
            for o in self.q[e]:
                if not o.is_dma and o.flag:
                    c += 1
                    o.cnt = c
            final[e] = c
        dfinal = [16 * c for c in self.dma_cnt]
        SEMS["ebase"] = dict(final)
        SEMS["dcnt"] = list(self.dma_cnt)
        q = self.q

        def run(e, eng):
            waited_e = dict(ebase)
            waited_d = list(dbase)
            for o in q[e]:
                ws = list(o.waits)
                if o.pre is not None:
                    ws.append(o.pre)
                for d in ws:
                    if d.is_dma:
                        if waited_d[d.dsem] < d.dval:
                            eng.wait_ge(dsem[d.dsem], d.dval)
                            waited_d[d.dsem] = d.dval
                    else:
                        if waited_e[d.eng] < d.cnt:
                            eng.wait_ge(esem[d.eng], d.cnt)
                            waited_e[d.eng] = d.cnt
                ins = o.fn(eng)
                if o.is_dma:
                    ins.then_inc(dsem[o.dsem], 16)
                elif o.flag:
                    ins.then_inc(esem[e], 1)
            for x in ENGS:
                if final[x] > waited_e[x]:
                    eng.wait_ge(esem[x], final[x])
            for i in range(N_DMA_SEMS):
                if dfinal[i] > waited_d[i]:
                    eng.wait_ge(dsem[i], dfinal[i])

        with nc.Block() as block:
            @block.tensor
            def _(eng):
                run("pe", eng)

            @block.scalar
            def _(eng):
                run("act", eng)

            @block.vector
            def _(eng):
                run("dve", eng)

            @block.gpsimd
            def _(eng):
                run("pool", eng)

            @block.sync
            def _(eng):
                run("sp", eng)
        self.stack.close()


S = 2048
D = 1024
NT = 16
NE = 16
CAP = 256
FF = 2048
PLE = 256
NCORES = 8
import os as _os
CCUT = _os.environ.get('CCUT', '')


def _consts():
    half = 128
    inv = (10000.0 ** (-np.arange(half, dtype=np.float32) / np.float32(half))).astype(np.float32)
    pos = np.arange(S, dtype=np.float32)
    ang = (inv[:, None] * pos[None, :]).astype(np.float32)
    cs = np.stack([np.cos(ang.astype(np.float64)), np.sin(ang.astype(np.float64))], 1).astype(np.float32)
    j = np.arange(128, dtype=np.float32)[:, None]
    i = np.arange(128, dtype=np.float32)[None, :]
    dmat = np.stack([np.maximum(i - j, 0), (j <= i).astype(np.float32),
                     np.maximum(j - i, 0), (j > i).astype(np.float32)], 0).astype(np.float32)
    rows = np.stack([np.broadcast_to(i + 1, (128, 128)), np.broadcast_to(128 - i, (128, 128))], 0).astype(np.float32)
    cols = np.concatenate([127 - j, j, np.full((128, 1), 128.0, np.float32)], 1).astype(np.float32)
    seqoff = ((np.arange(64) // 16) * S).astype(np.float32)[:, None]
    return dict(ident=np.eye(128, dtype=np.float32), cs=np.ascontiguousarray(cs), dmat=dmat,
                rows=np.ascontiguousarray(rows), cols=np.ascontiguousarray(cols), seqoff=seqoff)


def build(nseq, stop_after=None, debug=False):
    nc = bass.Bass("TRN2", target_bir_lowering=False)
    T = nseq * S
    NTL = nseq * NT

    def din(name, shape, dt=F32):
        return nc.dram_tensor(name, list(shape), dt, kind="ExternalInput").ap()

    x = din("x", [T, D])
    p_in = din("p", [2, T, PLE])
    norm_mix = din("norm_mix", [2, D])
    norm_ffn = din("norm_ffn", [2, D])
    norm_ple = din("norm_ple", [2, D])
    final_norm = din("final_norm", [1, D])
    conv_w_in = din("conv_w_in", [D, 3 * D])
    conv_wb = din("conv_wb", [4, D])
    conv_w_out = din("conv_w_out", [D, D])
    ret_w_in = din("ret_w_in", [D, 6 * D])
    ret_ld = din("ret_ld", [1, 8])
    ret_w_out = din("ret_w_out", [2 * D, D])
    router_w = din("router_w", [2, D, NE])
    w_gate = din("exp_w_gate", [2, NE, D, FF])
    w_up = din("exp_w_up", [2, NE, D, FF])
    w_down = din("exp_w_down", [2, NE, FF, D])
    ple_proj = din("ple_w_proj", [2, PLE, D])
    ple_gate = din("ple_w_gate", [2, D, D])
    c_ident = din("ident", [128, 128])
    c_cs = din("cs", [128, 2, S])
    c_dmat = din("dmat", [4, 128, 128])
    c_rows = din("rows", [2, 128, 128])
    c_cols = din("cols", [128, 3])
    c_seqoff = din("seqoff", [64, 1])
    out = nc.dram_tensor("out", [T, D], F32, kind="ExternalOutput").ap()
    H = nc.dram_tensor("Hres", [T, D], F32, kind="ExternalOutput" if debug else "Internal").ap()
    XN = nc.dram_tensor("XNs", [T, D], BF16, kind="Internal").ap()
    SBD = nc.dram_tensor("SBD", [16, 128, 1024], BF16, kind="Internal").ap()

    gst = ExitStack()
    SEMS["esem"] = {e: gst.enter_context(nc.semaphore(f"s_{e}")) for e in ENGS}
    SEMS["dsem"] = [gst.enter_context(nc.semaphore(f"d{i}")) for i in range(N_DMA_SEMS)]
    SEMS["ebase"] = {e: 0 for e in ENGS}
    SEMS["dcnt"] = [0] * N_DMA_SEMS
    identf = gst.enter_context(nc.sbuf_tensor("g_identf", [128, 128], F32))
    identb = gst.enter_context(nc.sbuf_tensor("g_identb", [128, 128], BF16))
    aff_tok = gst.enter_context(nc.sbuf_tensor("g_aff", [128, NT, 64], F32))
    epsn = gst.enter_context(nc.sbuf_tensor("g_eps", [128, 2], F32))

    ph = Phase(nc, "I")
    ph.op("sp", lambda e: e.dma_start(out=identf[:], in_=c_ident), w=["idf"], dma=True)
    ph.op("dve", lambda e: e.tensor_copy(out=identb[:], in_=identf[:]), r=["idf"], w=["idb"])
    ph.op("dve", lambda e: e.memset(aff_tok[:], 0.0), w=["aff"])
    ph.op("dve", lambda e: e.memset(epsn[:, 0:1], 1e-6), w=["eps0"])
    ph.op("dve", lambda e: e.memset(epsn[:, 1:2], 1e-5), w=["eps1"])
    ph.emit()

    def newton(ph, src, skey, tile, c0, tag, scale, eps):
        v = tile[:, c0:c0 + 1]
        y = tile[:, c0 + 1:c0 + 2]
        t = tile[:, c0 + 2:c0 + 3]
        vi = v.bitcast(I32)
        yi = y.bitcast(I32)
        kv, ky, kt = ("nv", tag), ("ny", tag), ("nt", tag)
        ph.op("dve", lambda e: e.tensor_scalar(out=v, in0=src, scalar1=scale, scalar2=eps, op0=ALU.mult, op1=ALU.add), r=[skey], w=[kv])
        ph.op("dve", lambda e: e.tensor_single_scalar(out=yi, in_=vi, scalar=1, op=ALU.logical_shift_right), r=[kv], w=[ky])
        ph.op("dve", lambda e: e.tensor_scalar(out=yi, in0=yi, scalar1=-1, scalar2=0x5f3759df, op0=ALU.mult, op1=ALU.add), r=[ky], w=[ky])
        for _ in range(2):
            ph.op("dve", lambda e: e.scalar_tensor_tensor(out=t, in0=v, scalar=1.0, in1=y, op0=ALU.mult, op1=ALU.mult), r=[kv, ky], w=[kt])
            ph.op("dve", lambda e: e.tensor_tensor(out=t, in0=t, in1=y, op=ALU.mult), r=[kt, ky], w=[kt])
            ph.op("dve", lambda e: e.tensor_scalar(out=t, in0=t, scalar1=-0.5, scalar2=1.5, op0=ALU.mult, op1=ALU.add), r=[kt], w=[kt])
            ph.op("dve", lambda e: e.tensor_tensor(out=y, in0=y, in1=t, op=ALU.mult), r=[kt, ky], w=[ky])
        return y, ky

    def rmsnorm(ph, hin, hkey, gb, gkey, outt, okey, junk, sst, tag):
        hkeys = hkey if isinstance(hkey, list) else [hkey]
        ph.op("act", lambda e: e.activation(out=junk[:], in_=hin, func=AF.Square, accum_out=sst[:, 3:4]),
              r=hkeys, w=[("ss", tag)])
        y, ky = newton(ph, sst[:, 3:4], ("ss", tag), sst, 0, tag, 1.0 / D, 1e-6)
        ph.op("dve", lambda e: e.scalar_tensor_tensor(out=outt, in0=hin, scalar=y, in1=gb[:], op0=ALU.mult, op1=ALU.mult),
              r=hkeys + [ky, gkey], w=[okey])

    def transpose8(ph, src, skey, ptb, pkey, dst, dkey, eng, n=8):
        for k in range(n):
            ph.op("pe", lambda e, k=k: e.transpose(out=ptb[:, k, :], in_=src[:, k * 128:(k + 1) * 128], identity=identb[:]),
                  r=[skey], w=[pkey])
        if eng == "act":
            ph.op("act", lambda e: e.activation(out=dst, in_=ptb[:, 0:n, :], func=AF.Copy), r=[pkey], w=[dkey])
        else:
            ph.op("dve", lambda e: e.tensor_copy(out=dst, in_=ptb[:, 0:n, :]), r=[pkey], w=[dkey])

    def phase_A(s):
        ph = Phase(nc, f"A{s}")
        xnT = ph.sb("xnT", [128, 8, S], BF16)
        zT = ph.sb("zT", [128, 8, S], BF16)
        wout = ph.sb("wout", [128, 8, D], BF16)
        win = [ph.sb(f"win{i}", [128, 3, 8, 128], BF16) for i in range(2)]
        gmix = ph.sb("gmix", [128, D], F32)
        cw4 = ph.sb("cw4", [4, D], F32)
        cwb = ph.sb("cwb", [128, 8, 4], F32)
        xt = [ph.sb(f"xt{i}", [128, D], F32) for i in range(2)]
        junk = ph.sb("junk", [128, D], BF16)
        sst = [ph.sb(f"sst{i}", [128, 4], F32) for i in range(2)]
        xn = [ph.sb(f"xn{i}", [128, D], BF16) for i in range(2)]
        u = [ph.sb(f"u{i}", [128, S + 2], F32) for i in range(2)]
        bsb = [ph.sb(f"bsb{i}", [128, S], F32) for i in range(2)]
        csb = [ph.sb(f"csb{i}", [128, 512], F32) for i in range(2)]
        yc = [ph.sb(f"yc{i}", [128, S], F32) for i in range(2)]
        hn = [ph.sb(f"hn{i}", [128, D], F32) for i in range(2)]
        ptb = [ph.ps(f"ptb{i}", [128, 8, 128], BF16) for i in range(2)]
        PP = [ph.ps(f"pp{i}", [128, 1024], F32) for i in range(3)]

        ph.op("sp", lambda e: e.dma_start(out=gmix[:], in_=norm_mix[0:1, :].partition_broadcast(128)), w=["gmix"], dma=True)
        ph.op("sp", lambda e: e.dma_start(out=cw4[:], in_=conv_wb), w=["cw4"], dma=True)
        ph.op("pool", lambda e: e.dma_start(out=wout[:], in_=conv_w_out.rearrange("(k p) n -> p k n", p=128)), w=["wout"], dma=True)
        cps = PP[0][:, 0:32].rearrange("p (j w) -> p j w", w=4)
        for j in range(8):
            ph.op("pe", lambda e, j=j: e.transpose(out=cps[:, j, :], in_=cw4[:, j * 128:(j + 1) * 128], identity=identf[0:4, 0:4]),
                  r=["cw4"], w=[("pp", 0, 0)])
        ph.op("dve", lambda e: e.tensor_copy(out=cwb[:], in_=cps), r=[("pp", 0, 0)], w=["cwb"])
        for i in range(2):
            ph.op("dve", lambda e, i=i: e.memset(u[i][:, 0:1], 0.0), w=[("u", i)])
            ph.op("dve", lambda e, i=i: e.memset(u[i][:, S + 1:S + 2], 0.0), w=[("u", i)])
        for i in range(NT):
            b = i % 2
            r0 = s * S + i * 128
            ph.op("sp", lambda e, b=b, r0=r0: e.dma_start(out=xt[b][:], in_=x[r0:r0 + 128, :]), w=[("xt", b)], dma=True)
            rmsnorm(ph, xt[b][:], ("xt", b), gmix, "gmix", xn[b][:], ("xn", b), junk, sst[b], b)
            transpose8(ph, xn[b], ("xn", b), ptb[b], ("ptb", b), xnT[:, :, i * 128:(i + 1) * 128], ("xnT", i), "act")
        xnT_keys = [("xnT", i) for i in range(NT)]
        w_in_v = conv_w_in.rearrange("(k p) (t n) -> p t k n", p=128, t=3)
        for j in range(8):
            jb = j % 2
            ph.op("pool", lambda e, j=j, jb=jb: e.dma_start(out=win[jb][:], in_=w_in_v[:, :, :, j * 128:(j + 1) * 128]),
                  w=[("win", jb)], dma=True)
            for tb in range(4):
                q = (j * 4 + tb) % 2
                bps = PP[q][:, 0:512]
                cps_ = PP[q][:, 512:1024]
                vps = PP[2][:, q * 512:(q + 1) * 512]
                tks = [("xnT", i) for i in range(tb * 4, tb * 4 + 4)]
                for t, (dst, key) in enumerate([(bps, ("pp", q, 0)), (cps_, ("pp", q, 1)), (vps, ("pp", 2, q))]):
                    for k in range(8):
                        ph.op("pe", lambda e, dst=dst, t=t, k=k, jb=jb, tb=tb: e.matmul(
                            dst, lhsT=win[jb][:, t, k, :], rhs=xnT[:, k, tb * 512:(tb + 1) * 512], start=(k == 0), stop=(k == 7)),
                            r=[("win", jb)] + tks, w=[key])
                ph.op("act", lambda e, q=q, cps_=cps_: e.activation(out=csb[q][:], in_=cps_, func=AF.Copy), r=[("pp", q, 1)], w=[("csb", q)])
                ph.op("act", lambda e, jb=jb, tb=tb, bps=bps: e.activation(out=bsb[jb][:, tb * 512:(tb + 1) * 512], in_=bps, func=AF.Copy),
                      r=[("pp", q, 0)], w=[("bsb", jb)])
                ph.op("dve", lambda e, jb=jb, tb=tb, q=q, vps=vps: e.tensor_tensor(
                    out=u[jb][:, 1 + tb * 512:1 + (tb + 1) * 512], in0=csb[q][:], in1=vps, op=ALU.mult),
                    r=[("csb", q), ("pp", 2, q)], w=[("u", jb)])
            ph.op("act", lambda e, j=j, jb=jb: e.activation(out=yc[jb][:], in_=u[jb][:, 1:S + 1], func=AF.Identity,
                                                            bias=cwb[:, j, 3:4], scale=cwb[:, j, 1:2]),
                  r=[("u", jb), "cwb"], w=[("yc", jb)])
            ph.op("dve", lambda e, j=j, jb=jb: e.scalar_tensor_tensor(out=yc[jb][:], in0=u[jb][:, 0:S], scalar=cwb[:, j, 0:1], in1=yc[jb][:],
                                                                     op0=ALU.mult, op1=ALU.add),
                  r=[("u", jb), "cwb", ("yc", jb)], w=[("yc", jb)])
            ph.op("dve", lambda e, j=j, jb=jb: e.scalar_tensor_tensor(out=yc[jb][:], in0=u[jb][:, 2:S + 2], scalar=cwb[:, j, 2:3], in1=yc[jb][:],
                                                                      op0=ALU.mult, op1=ALU.add),
                  r=[("u", jb), "cwb", ("yc", jb)], w=[("yc", jb)])
            ph.op("pool", lambda e, j=j, jb=jb: e.tensor_tensor(out=zT[:, j, :], in0=bsb[jb][:], in1=yc[jb][:], op=ALU.mult),
                  r=[("bsb", jb), ("yc", jb)], w=[("zT", j)])
        zkeys = [("zT", j) for j in range(8)]
        for i in range(NT):
            b = i % 2
            r0 = s * S + i * 128
            ph.op("sp", lambda e, b=b, r0=r0: e.dma_start(out=xt[b][:], in_=x[r0:r0 + 128, :]), w=[("xt", b)], dma=True)
            for nh in range(2):
                for k in range(8):
                    ph.op("pe", lambda e, b=b, nh=nh, k=k, i=i: e.matmul(
                        PP[b][:, nh * 512:(nh + 1) * 512], lhsT=zT[:, k, i * 128:(i + 1) * 128], rhs=wout[:, k, nh * 512:(nh + 1) * 512],
                        start=(k == 0), stop=(k == 7)), r=zkeys + ["wout"], w=[("pp", b, nh)])
                ph.op("dve", lambda e, b=b, nh=nh: e.tensor_tensor(out=hn[b][:, nh * 512:(nh + 1) * 512], in0=xt[b][:, nh * 512:(nh + 1) * 512],
                                                                  in1=PP[b][:, nh * 512:(nh + 1) * 512], op=ALU.add),
                      r=[("xt", b), ("pp", b, nh)], w=[("hn", b, nh)])
            ph.op("sp", lambda e, b=b, r0=r0: e.dma_start(out=H[r0:r0 + 128, :], in_=hn[b][:]), r=[("hn", b, 0), ("hn", b, 1)], w=[("H", r0)], dma=True)
        ph.emit()

    def phase_R(l):
        ph = Phase(nc, f"R{l}")
        NB = 4
        gffn = ph.sb("gffn", [128, D], F32)
        wr = ph.sb("wr", [128, 8, NE], BF16)
        hn = [ph.sb(f"hn{i}", [128, D], F32) for i in range(NB)]
        junk = ph.sb("junk", [128, D], BF16)
        sst = [ph.sb(f"sst{i}", [128, 4], F32) for i in range(NB)]
        xn = [ph.sb(f"xn{i}", [128, D], BF16) for i in range(NB)]
        xT = [ph.sb(f"xT{i}", [128, 8, 128], BF16) for i in range(NB)]
        sm = [ph.sb(f"sm{i}", [128, 4], F32) for i in range(NB)]
        ex = [ph.sb(f"ex{i}", [128, NE], F32) for i in range(NB)]
        ptb = [ph.ps(f"ptb{i}", [128, 8, 128], BF16) for i in range(2)]
        PL = [ph.ps(f"pl{i}", [128, NE], F32) for i in range(2)]
        ph.op("sp", lambda e: e.dma_start(out=gffn[:], in_=norm_ffn[l:l + 1, :].partition_broadcast(128)), w=["gffn"], dma=True)
        ph.op("pool", lambda e: e.dma_start(out=wr[:], in_=router_w[l].rearrange("(k p) n -> p k n", p=128)), w=["wr"], dma=True)
        for ti in range(NTL):
            b = ti % NB
            pb2 = ti % 2
            s, i = divmod(ti, NT)
            r0 = ti * 128
            ph.op("sp", lambda e, b=b, r0=r0: e.dma_start(out=hn[b][:], in_=H[r0:r0 + 128, :]), w=[("hn", b)], dma=True)
            rmsnorm(ph, hn[b][:], ("hn", b), gffn, "gffn", xn[b][:], ("xn", b), junk, sst[b], b)
            ph.op("sp", lambda e, b=b, r0=r0: e.dma_start(out=XN[r0:r0 + 128, :], in_=xn[b][:]), r=[("xn", b)], w=[("XN", r0)], dma=True)
            transpose8(ph, xn[b], ("xn", b), ptb[pb2], ("ptb", pb2), xT[b][:], ("xT", b), "act")
            for k in range(8):
                ph.op("pe", lambda e, b=b, k=k, pb2=pb2: e.matmul(PL[pb2][:], lhsT=xT[b][:, k, :], rhs=wr[:, k, :], start=(k == 0), stop=(k == 7)),
                      r=[("xT", b), "wr"], w=[("pl", pb2)])
            ph.op("dve", lambda e, b=b, pb2=pb2: e.reduce_max(out=sm[b][:, 0:1], in_=PL[pb2][:], axis=AX.X), r=[("pl", pb2)], w=[("mx", b)])
            ph.op("dve", lambda e, b=b: e.tensor_scalar(out=sm[b][:, 1:2], in0=sm[b][:, 0:1], scalar1=-1.0, scalar2=None, op0=ALU.mult),
                  r=[("mx", b)], w=[("nmx", b)])
            ph.op("act", lambda e, b=b, pb2=pb2: e.activation(out=ex[b][:], in_=PL[pb2][:], func=AF.Exp, bias=sm[b][:, 1:2], scale=1.0, accum_out=sm[b][:, 2:3]),
                  r=[("pl", pb2), ("nmx", b)], w=[("ex", b), ("sum", b)])
            ph.op("dve", lambda e, b=b: e.reciprocal(out=sm[b][:, 3:4], in_=sm[b][:, 2:3]), r=[("sum", b)], w=[("rsum", b)])
            ph.op("dve", lambda e, b=b, s=s, i=i: e.tensor_scalar(out=aff_tok[:, i, s * 16:(s + 1) * 16], in0=ex[b][:], scalar1=sm[b][:, 3:4],
                                                                 scalar2=None, op0=ALU.mult),
                  r=[("ex", b), ("rsum", b)], w=[("aff", ti)])
        ph.emit()

    def phase_P(l, final):
        ph = Phase(nc, f"P{l}")
        NB = 4
        wpg = ph.sb("wpg", [128, 8, D], BF16)
        wpp = ph.sb("wpp", [128, 2, D], BF16)
        gple = ph.sb("gple", [128, D], F32)
        gnx = ph.sb("gnx", [128, D], F32)
        hn = [ph.sb(f"hn{i}", [128, D], F32) for i in range(NB)]
        pt = [ph.sb(f"pt{i}", [128, PLE], F32) for i in range(NB)]
        pb = [ph.sb(f"pb{i}", [128, PLE], BF16) for i in range(NB)]
        pT = [ph.sb(f"pT{i}", [128, 2, 128], BF16) for i in range(NB)]
        junk = ph.sb("junk", [128, D], BF16)
        sst = [ph.sb(f"sst{i}", [128, 4], F32) for i in range(2 * NB)]
        xn = [ph.sb(f"xn{i}", [128, D], BF16) for i in range(NB)]
        xT = [ph.sb(f"xT{i}", [128, 8, 128], BF16) for i in range(NB)]
        sgm = [ph.sb(f"sgm{i}", [128, D], F32) for i in range(NB)]
        h2 = [ph.sb(f"h2{i}", [128, D], F32) for i in range(NB)]
        xo = [ph.sb(f"xo{i}", [128, D], F32 if final else BF16) for i in range(NB)]
        ptb = [ph.ps(f"ptb{i}", [128, 8, 128], BF16) for i in range(2)]
        ptp = ph.ps("ptp", [128, 8, 128], BF16)
        PG = ph.ps("pg", [128, 1024], F32)
        PQ = ph.ps("pq", [128, 1024], F32)
        ph.op("pool", lambda e: e.dma_start(out=wpg[:], in_=ple_gate[l].rearrange("(k p) n -> p k n", p=128)), w=["wpg"], dma=True)
        ph.op("pool", lambda e: e.dma_start(out=wpp[:], in_=ple_proj[l].rearrange("(k p) n -> p k n", p=128)), w=["wpp"], dma=True)
        ph.op("sp", lambda e: e.dma_start(out=gple[:], in_=norm_ple[l:l + 1, :].partition_broadcast(128)), w=["gple"], dma=True)
        gsrc = final_norm[0:1, :] if final else norm_mix[l + 1:l + 2, :]
        ph.op("sp", lambda e: e.dma_start(out=gnx[:], in_=gsrc.partition_broadcast(128)), w=["gnx"], dma=True)
        for ti in range(NTL):
            b = ti % NB
            pb2 = ti % 2
            r0 = ti * 128
            ph.op("sp", lambda e, b=b, r0=r0: e.dma_start(out=hn[b][:], in_=H[r0:r0 + 128, :]), w=[("hn", b)], dma=True)
            ph.op("sp", lambda e, b=b, r0=r0: e.dma_start(out=pt[b][:], in_=p_in[l, r0:r0 + 128, :]), w=[("pt", b)], dma=True)
            rmsnorm(ph, hn[b][:], ("hn", b), gple, "gple", xn[b][:], ("xn", b), junk, sst[b], b)
            transpose8(ph, xn[b], ("xn", b), ptb[pb2], ("ptb", pb2), xT[b][:], ("xT", b), "act")
            ph.op("pool", lambda e, b=b: e.tensor_copy(out=pb[b][:], in_=pt[b][:]), r=[("pt", b)], w=[("pb", b)])
            transpose8(ph, pb[b], ("pb", b), ptp, "ptp", pT[b][:], ("pT", b), "dve", n=2)
            for nh in range(2):
                for k in range(8):
                    ph.op("pe", lambda e, b=b, nh=nh, k=k: e.matmul(PG[:, nh * 512:(nh + 1) * 512], lhsT=xT[b][:, k, :],
                                                                   rhs=wpg[:, k, nh * 512:(nh + 1) * 512], start=(k == 0), stop=(k == 7)),
                          r=[("xT", b), "wpg"], w=[("pg", nh)])
                for k in range(2):
                    ph.op("pe", lambda e, b=b, nh=nh, k=k: e.matmul(PQ[:, nh * 512:(nh + 1) * 512], lhsT=pT[b][:, k, :],
                                                                   rhs=wpp[:, k, nh * 512:(nh + 1) * 512], start=(k == 0), stop=(k == 1)),
                          r=[("pT", b), "wpp"], w=[("pq", nh)])
                sl = slice(nh * 512, (nh + 1) * 512)
                ph.op("act", lambda e, b=b, sl=sl: e.activation(out=sgm[b][:, sl], in_=PG[:, sl], func=AF.Sigmoid), r=[("pg", nh)], w=[("sgm", b, nh)])
                ph.op("dve", lambda e, b=b, sl=sl: e.tensor_tensor(out=sgm[b][:, sl], in0=sgm[b][:, sl], in1=PQ[:, sl], op=ALU.mult),
                      r=[("sgm", b, nh), ("pq", nh)], w=[("sgm", b, nh)])
                ph.op("pool", lambda e, b=b, sl=sl: e.tensor_tensor(out=h2[b][:, sl], in0=sgm[b][:, sl], in1=hn[b][:, sl], op=ALU.add),
                      r=[("sgm", b, nh), ("hn", b)], w=[("h2", b, nh)])
            hk = [("h2", b, 0), ("h2", b, 1)]
            if not final:
                ph.op("sp", lambda e, b=b, r0=r0: e.dma_start(out=H[r0:r0 + 128, :], in_=h2[b][:]), r=hk, w=[("H", r0)], dma=True)
            rmsnorm(ph, h2[b][:], hk, gnx, "gnx", xo[b][:], ("xo", b), junk, sst[NB + b], ("n2", b))
            dst = out if final else XN
            ph.op("sp", lambda e, b=b, r0=r0, dst=dst: e.dma_start(out=dst[r0:r0 + 128, :], in_=xo[b][:]), r=[("xo", b)], w=[("O", r0)], dma=True)
        ph.emit()

    def phase_B(l):
        ph = Phase(nc, f"B{l}")
        NTOK = nseq * CAP
        ntile = nseq * 2
        TB = min(512, NTOK)
        nblk = NTOK // TB
        work = ph.sb("work", [64, S], F32)
        gates = ph.sb("gates", [64, CAP], F32)
        idxu = ph.sb("idxu", [64, CAP], U32)
        idxf = ph.sb("idxf", [64, CAP], F32)
        soff = ph.sb("soff", [64, 1], F32)
        gT = ph.sb("gT", [128, 2, 64], F32)
        iTi = ph.sb("iTi", [128, 2, 64], I32)
        ring = [ph.sb(f"ring{i}", [128, 8192], BF16) for i in range(4)]
        xg = [ph.sb(f"xg{i}", [128, ntile, D], BF16) for i in range(2)]
        xgT = ph.sb("xgT", [128, 8, NTOK], BF16)
        hT = ph.sb("hT", [128, 16, NTOK], BF16)
        sg = [ph.sb(f"sg{i}", [128, 512], F32) for i in range(2)]
        yt = [ph.sb(f"yt{i}", [128, D], F32) for i in range(2)]
        ptb = [ph.ps(f"ptb{i}", [128, 8, 128], BF16) for i in range(2)]
        PGU = [ph.ps(f"pgu{i}", [128, 1024], F32) for i in range(2)]
        PD = ph.ps("pd", [128, 1024], F32)

        ph.op("sp", lambda e: e.dma_start(out=soff[:], in_=c_seqoff), w=["soff"], dma=True)
        for g in range(4):
            for ii in range(4):
                i = g * 4 + ii
                ph.op("pe", lambda e, g=g, ii=ii, i=i: e.transpose(out=PGU[g % 2][0:64, ii * 128:(ii + 1) * 128], in_=aff_tok[:, i, :], identity=identf[:]),
                      r=["aff"], w=[("pgu", g % 2, 0)])
            ph.op("dve", lambda e, g=g: e.tensor_copy(out=work[:, g * 512:(g + 1) * 512], in_=PGU[g % 2][0:64, 0:512]),
                  r=[("pgu", g % 2, 0)], w=["work"])
        for r in range(CAP // 8):
            sl = slice(r * 8, (r + 1) * 8)
            ph.op("dve", lambda e, sl=sl: e.max(out=gates[:, sl], in_=work[:]), r=["work"], w=[("gt", r)])
            ph.op("dve", lambda e, sl=sl: e.max_index(out=idxu[:, sl], in_max=gates[:, sl], in_values=work[:]), r=["work", ("gt", r)], w=[("ix", r)])
            ph.op("dve", lambda e, sl=sl: e.match_replace(out=work[:], in_to_replace=gates[:, sl], in_values=work[:], imm_value=-1.0),
                  r=["work", ("gt", r)], w=["work"])
        gkeys = [("gt", r) for r in range(CAP // 8)]
        ikeys = [("ix", r) for r in range(CAP // 8)]
        ph.op("dve", lambda e: e.tensor_copy(out=idxf[:], in_=idxu[:]), r=ikeys, w=["idxf"])
        ph.op("dve", lambda e: e.tensor_scalar(out=idxf[:], in0=idxf[:], scalar1=soff[:, 0:1], scalar2=None, op0=ALU.add), r=["idxf", "soff"], w=["idxf"])
        tp = PGU[0][:, 0:256].rearrange("p (a c) -> p a c", c=64)
        for hf in range(2):
            ph.op("pe", lambda e, hf=hf: e.transpose(out=tp[:, hf, :], in_=gates[:, hf * 128:(hf + 1) * 128], identity=identf[0:64, 0:64]),
                  r=gkeys, w=[("pgu", 0, 0)])
            ph.op("pe", lambda e, hf=hf: e.transpose(out=tp[:, 2 + hf, :], in_=idxf[:, hf * 128:(hf + 1) * 128], identity=identf[0:64, 0:64]),
                  r=["idxf"], w=[("pgu", 0, 0)])
        ph.op("dve", lambda e: e.tensor_copy(out=gT[:], in_=tp[:, 0:2, :]), r=[("pgu", 0, 0)], w=["gT"])
        ph.op("act", lambda e: e.activation(out=iTi[:], in_=tp[:, 2:4, :], func=AF.Copy), r=[("pgu", 0, 0)], w=["iTi"])

        wg_v = w_gate[l].rearrange("e (k p) f -> e p k f", p=128)
        wu_v = w_up[l].rearrange("e (k p) f -> e p k f", p=128)
        wd_v = w_down[l].rearrange("e (k p) n -> e p k n", p=128)

        def load_slab(g):
            e_, j = divmod(g, 6)
            if e_ >= NE:
                return
            slot = g % 4
            if j < 4:
                dstg = ring[slot][:, 0:4096].rearrange("p (k n) -> p k n", n=512)
                dstu = ring[slot][:, 4096:8192].rearrange("p (k n) -> p k n", n=512)
                ph.op("pool", lambda e: e.dma_start(out=dstg, in_=wg_v[e_, :, :, j * 512:(j + 1) * 512]), w=[("ring", slot, 0)], dma=True)
                ph.op("pool", lambda e: e.dma_start(out=dstu, in_=wu_v[e_, :, :, j * 512:(j + 1) * 512]), w=[("ring", slot, 1)], dma=True)
            else:
                dh = j - 4
                dst = ring[slot][:].rearrange("p (k n) -> p k n", n=1024)
                ph.op("pool", lambda e: e.dma_start(out=dst, in_=wd_v[e_, :, dh * 8:(dh + 1) * 8, :]), w=[("ring", slot, 0), ("ring", slot, 1)], dma=True)

        def gather(e_):
            if e_ >= NE:
                return
            for t in range(ntile):
                s, hf = divmod(t, 2)
                col = s * 16 + e_
                ph.op("pool", lambda e, t=t, hf=hf, col=col: e.indirect_dma_start(
                    out=xg[e_ % 2][:, t, :], out_offset=None, in_=XN[:, :],
                    in_offset=bass.IndirectOffsetOnAxis(ap=iTi[:, hf, col:col + 1], axis=0)),
                    r=["iTi"], w=[("xg", e_ % 2, t)], dma=True)

        def transposes(e_):
            if e_ >= NE:
                return
            for t in range(ntile):
                transpose8(ph, xg[e_ % 2][:, t, :], ("xg", e_ % 2, t), ptb[t % 2], ("ptb", t % 2),
                           xgT[:, :, t * 128:(t + 1) * 128], ("xgT", t), "act" if t % 2 == 0 else "dve")

        for g in range(4):
            load_slab(g)
        gather(0)
        transposes(0)
        gather(1)
        prev_sc = {}
        cur_sc = {}
        cnt = 0
        for e_ in range(NE):
            for fg in range(4):
                g = e_ * 6 + fg
                slot = g % 4
                sv = ring[slot][:].rearrange("p (a k n) -> p a k n", a=2, k=8)
                for fc4 in range(4):
                    fc = fg * 4 + fc4
                    for hb in range(nblk):
                        q = cnt % 2
                        cnt += 1
                        tks = [("xgT", t) for t in range(hb * (TB // 128), (hb + 1) * (TB // 128))]
                        for a in range(2):
                            for k in range(8):
                                ph.op("pe", lambda e, q=q, a=a, k=k, sv=sv, fc4=fc4, hb=hb: e.matmul(
                                    PGU[q][:, a * 512:a * 512 + TB], lhsT=sv[:, a, k, fc4 * 128:(fc4 + 1) * 128],
                                    rhs=xgT[:, k, hb * TB:(hb + 1) * TB], start=(k == 0), stop=(k == 7)),
                                    r=[("ring", slot, a)] + tks, w=[("pgu", q, a)])
                        ph.op("act", lambda e, q=q: e.activation(out=sg[q][:, 0:TB], in_=PGU[q][:, 0:TB], func=AF.Silu), r=[("pgu", q, 0)], w=[("sg", q)])
                        ph.op("dve", lambda e, q=q, fc=fc, hb=hb: e.tensor_tensor(out=hT[:, fc, hb * TB:(hb + 1) * TB], in0=sg[q][:, 0:TB],
                                                                                 in1=PGU[q][:, 512:512 + TB], op=ALU.mult),
                              r=[("sg", q), ("pgu", q, 1)], w=[("hT", fc, hb)])
                load_slab(g + 4)
            transposes(e_ + 1)
            gather(e_ + 2)
            d0 = ring[(e_ * 6 + 4) % 4][:].rearrange("p (k n) -> p k n", n=1024)
            d1 = ring[(e_ * 6 + 5) % 4][:].rearrange("p (k n) -> p k n", n=1024)
            dkeys = [("ring", (e_ * 6 + 4) % 4, 0), ("ring", (e_ * 6 + 4) % 4, 1), ("ring", (e_ * 6 + 5) % 4, 0), ("ring", (e_ * 6 + 5) % 4, 1)]
            for t in range(ntile):
                s, hf = divmod(t, 2)
                col = s * 16 + e_
                hb = (t * 128) // TB
                b = t % 2
                for nh in range(2):
                    for fc in range(16):
                        dsl = d0 if fc < 8 else d1
                        ph.op("pe", lambda e, nh=nh, fc=fc, dsl=dsl, t=t: e.matmul(
                            PD[:, nh * 512:(nh + 1) * 512], lhsT=hT[:, fc, t * 128:(t + 1) * 128], rhs=dsl[:, fc % 8, nh * 512:(nh + 1) * 512],
                            start=(fc == 0), stop=(fc == 15)), r=dkeys + [("hT", fc_, hb) for fc_ in range(16)], w=[("pd", nh)])
                    sl = slice(nh * 512, (nh + 1) * 512)
                    if nh == 0:
                        ph.op("act", lambda e, b=b, sl=sl, hf=hf, col=col: e.activation(out=yt[b][:, sl], in_=PD[:, sl], func=AF.Copy, scale=gT[:, hf, col:col + 1]),
                              r=[("pd", nh), "gT"], w=[("yt", b, nh)])
                    else:
                        ph.op("dve", lambda e, b=b, sl=sl, hf=hf, col=col: e.tensor_scalar(out=yt[b][:, sl], in0=PD[:, sl], scalar1=gT[:, hf, col:col + 1],
                                                                                          scalar2=None, op0=ALU.mult),
                              r=[("pd", nh), "gT"], w=[("yt", b, nh)])
                o = ph.op("pool", lambda e, b=b, hf=hf, col=col: e.indirect_dma_start(
                    out=H[:, :], out_offset=bass.IndirectOffsetOnAxis(ap=iTi[:, hf, col:col + 1], axis=0),
                    in_=yt[b][:, :], in_offset=None, compute_op=ALU.add),
                    r=[("yt", b, 0), ("yt", b, 1), "iTi"], w=[], dma=True, after=prev_sc.get(s, []))
                cur_sc.setdefault(s, []).append(o)
            prev_sc = cur_sc
            cur_sc = {}
            load_slab(e_ * 6 + 4 + 4)
            load_slab(e_ * 6 + 5 + 4)
        ph.emit()

    def phase_C(s):
        ph = Phase(nc, f"C{s}")
        xnT = ph.sb("xnT", [128, 8, S], BF16)
        xl = [ph.sb(f"xl{i}", [128, D], BF16) for i in range(2)]
        ws = [ph.sb(f"ws{i}", [128, 4096], BF16) for i in range(4)]
        cs = [ph.sb(f"cs{i}", [128, 2, 512], F32) for i in range(2)]
        qT = ph.sb("qT", [128, 2, S], BF16)
        kT = ph.sb("kT", [128, 2, S], BF16)
        kf = ph.sb("kf", [128, NT, 256], BF16)
        kb = ph.sb("kb", [128, NT, 256], BF16)
        vt = ph.sb("vt", [128, NT, 512], BF16)
        rt = [ph.sb(f"rt{i}", [128, 512], F32) for i in range(4)]
        Sf32 = ph.sb("Sf32", [128, 1024], F32)
        Sb32 = ph.sb("Sb32", [128, 1024], F32)
        Sfb = [ph.sb(f"Sfb{i}", [128, 1024], BF16) for i in range(2)]
        Sbst = [ph.sb(f"Sbst{i}", [128, 1024], BF16) for i in range(2)]
        Sbin = [ph.sb(f"Sbin{i}", [128, 1024], BF16) for i in range(3)]
        ldb = ph.sb("ldb", [128, 8], F32)
        Mt = ph.sb("Mt", [128, 4, 128], F32)
        tmpM = [ph.sb(f"tmpM{i}", [128, 128], F32) for i in range(2)]
        qd = ph.sb("qd", [128, 4, 2, 128], F32)
        kd = ph.sb("kd", [128, 4, 2], F32)
        cdc = ph.sb("cdc", [128, 4, 2], F32)
        dm = ph.sb("dm", [128, 4, 128], F32)
        rw = ph.sb("rw", [128, 2, 128], F32)
        cl = ph.sb("cl", [128, 3], F32)
        Pm = [ph.sb(f"Pm{i}", [128, 128], BF16) for i in range(2)]
        qfb = [ph.sb(f"qfb{i}", [128, 2, 2, 128], BF16) for i in range(2)]
        sgl = [ph.sb(f"sgl{i}", [128, 512], F32) for i in range(2)]
        on = [ph.sb(f"on{i}", [128, 512], F32) for i in range(2)]
        go = [ph.sb(f"go{i}", [128, 512], BF16) for i in range(2)]
        goT = [ph.sb(f"goT{i}", [128, 4, 128], BF16) for i in range(2)]
        mo = [ph.sb(f"mo{i}", [128, D], F32) for i in range(2)]
        bst = [ph.sb(f"bst{i}", [128, 6], F32) for i in range(2)]
        mv = [ph.sb(f"mv{i}", [128, 8], F32) for i in range(2)]
        ptb = ph.ps("ptb", [128, 8, 128], BF16)
        PG = ph.ps("pg", [128, 512], F32)
        PA = ph.ps("pa", [128, 1024], F32)
        PB = ph.ps("pb", [128, 1024], F32)
        PO = ph.ps("po", [128, 512], F32)
        PSC = ph.ps("psc", [128, 128], F32)
        ptk = ptb[:].rearrange("p a b -> p (a b)").rearrange("p (a b) -> p a b", b=256)

        ph.op("sp", lambda e: e.dma_start(out=ldb[:], in_=ret_ld.partition_broadcast(128)), w=["ldb"], dma=True)
        ph.op("sp", lambda e: e.dma_start(out=dm[:], in_=c_dmat.rearrange("a j i -> j a i")), w=["dm"], dma=True)
        ph.op("sp", lambda e: e.dma_start(out=rw[:], in_=c_rows.rearrange("a p i -> p a i")), w=["rw"], dma=True)
        ph.op("sp", lambda e: e.dma_start(out=cl[:], in_=c_cols), w=["cl"], dma=True)
        for h in range(4):
            ph.op("act", lambda e, h=h: e.activation(out=tmpM[0][:], in_=dm[:, 0, :], func=AF.Exp, scale=ldb[:, h:h + 1]), r=["dm", "ldb"], w=["tmA"])
            ph.op("dve", lambda e, h=h: e.tensor_tensor(out=tmpM[0][:], in0=tmpM[0][:], in1=dm[:, 1, :], op=ALU.mult), r=["tmA", "dm"], w=["tmA"])
            ph.op("act", lambda e, h=h: e.activation(out=tmpM[1][:], in_=dm[:, 2, :], func=AF.Exp, scale=ldb[:, 4 + h:5 + h]), r=["dm", "ldb"], w=["tmB"])
            ph.op("dve", lambda e, h=h: e.tensor_tensor(out=tmpM[1][:], in0=tmpM[1][:], in1=dm[:, 3, :], op=ALU.mult), r=["tmB", "dm"], w=["tmB"])
            ph.op("dve", lambda e, h=h: e.tensor_tensor(out=Mt[:, h, :], in0=tmpM[0][:], in1=tmpM[1][:], op=ALU.add), r=["tmA", "tmB"], w=[("Mt", h)])
            for d_ in range(2):
                lc = ldb[:, 4 * d_ + h:4 * d_ + h + 1]
                ph.op("act", lambda e, h=h, d_=d_, lc=lc: e.activation(out=qd[:, h, d_, :], in_=rw[:, d_, :], func=AF.Exp, scale=lc), r=["rw", "ldb"], w=[("qd", h)])
                ph.op("act", lambda e, h=h, d_=d_, lc=lc: e.activation(out=kd[:, h, d_:d_ + 1], in_=cl[:, d_:d_ + 1], func=AF.Exp, scale=lc), r=["cl", "ldb"], w=[("kd", h)])
                ph.op("act", lambda e, h=h, d_=d_, lc=lc: e.activation(out=cdc[:, h, d_:d_ + 1], in_=cl[:, 2:3], func=AF.Exp, scale=lc), r=["cl", "ldb"], w=[("cdc", h)])
        for i in range(NT):
            b = i % 2
            r0 = s * S + i * 128
            ph.op("sp", lambda e, b=b, r0=r0: e.dma_start(out=xl[b][:], in_=XN[r0:r0 + 128, :]), w=[("xl", b)], dma=True)
            transpose8(ph, xl[b], ("xl", b), ptb, "ptb", xnT[:, :, i * 128:(i + 1) * 128], ("xnT", i), "act" if b == 0 else "dve")
        allx = [("xnT", i) for i in range(NT)]
        if CCUT == "c1":
            ph.emit()
            return
        w_in_v = ret_w_in.rearrange("(k p) n -> p k n", p=128)
        w_out_v = ret_w_out.rearrange("(k p) n -> p k n", p=128)
        ws0v = ws[0][:].rearrange("p (k n) -> p k n", n=512)
        ws1v = ws[1][:].rearrange("p (k n) -> p k n", n=512)
        ws2v = ws[2][:].rearrange("p (k n) -> p k n", n=512)
        ws3v = ws[3][:].rearrange("p (k n) -> p k n", n=1024)
        prev_acc = {}
        cnt = 0
        for h in range(4):
            ph.op("pool", lambda e, h=h: e.dma_start(out=ws0v[:, :, 0:256], in_=w_in_v[:, :, h * 256:(h + 1) * 256]), w=[("ws0", 0)], dma=True)
            ph.op("pool", lambda e, h=h: e.dma_start(out=ws0v[:, :, 256:512], in_=w_in_v[:, :, 1024 + h * 256:1024 + (h + 1) * 256]), w=[("ws0", 1)], dma=True)
            ph.op("pool", lambda e, h=h: e.dma_start(out=ws1v, in_=w_in_v[:, :, 2048 + h * 512:2048 + (h + 1) * 512]), w=["ws1"], dma=True)
            ph.op("pool", lambda e, h=h: e.dma_start(out=ws2v, in_=w_in_v[:, :, 4096 + h * 512:4096 + (h + 1) * 512]), w=["ws2"], dma=True)
            ph.op("pool", lambda e, h=h: e.dma_start(out=ws3v, in_=w_out_v[:, h * 4:(h + 1) * 4, :]), w=["ws3"], dma=True)
            for tb in range(4):
                cb_ = tb % 2
                ph.op("sp", lambda e, cb_=cb_, tb=tb: e.dma_start(out=cs[cb_][:], in_=c_cs[:, :, tb * 512:(tb + 1) * 512]), w=[("cs", cb_)], dma=True)
                tks = [("xnT", i) for i in range(tb * 4, tb * 4 + 4)]
                for which in range(2):
                    PX, pk = (PA, "pa") if cnt % 2 == 0 else (PB, "pb")
                    cnt += 1
                    off = which * 256
                    for dc in range(2):
                        for k in range(8):
                            ph.op("pe", lambda e, PX=PX, dc=dc, k=k, off=off, tb=tb: e.matmul(
                                PX[:, dc * 512:(dc + 1) * 512], lhsT=ws0v[:, k, off + dc * 128:off + (dc + 1) * 128],
                                rhs=xnT[:, k, tb * 512:(tb + 1) * 512], start=(k == 0), stop=(k == 7)),
                                r=[("ws0", which)] + tks, w=[(pk, dc)])
                    sc = 1.0 if which == 0 else 1.0 / 16.0
                    combos = [(0, 0), (1, 1), (0, 1), (1, 0)]
                    for ri, (xi, ci) in enumerate(combos):
                        ph.op("dve", lambda e, PX=PX, ri=ri, xi=xi, ci=ci, sc=sc, cb_=cb_: e.scalar_tensor_tensor(
                            out=rt[ri][:], in0=PX[:, xi * 512:(xi + 1) * 512], scalar=sc, in1=cs[cb_][:, ci, :], op0=ALU.mult, op1=ALU.mult),
                            r=[(pk, xi), ("cs", cb_)], w=[("rt", ri)])
                    dstT, dk = (qT, "qT") if which == 0 else (kT, "kT")
                    ph.op("pool", lambda e, dstT=dstT, tb=tb: e.tensor_tensor(out=dstT[:, 0, tb * 512:(tb + 1) * 512], in0=rt[0][:], in1=rt[1][:], op=ALU.subtract),
                          r=[("rt", 0), ("rt", 1)], w=[(dk, tb, 0)])
                    ph.op("pool", lambda e, dstT=dstT, tb=tb: e.tensor_tensor(out=dstT[:, 1, tb * 512:(tb + 1) * 512], in0=rt[2][:], in1=rt[3][:], op=ALU.add),
                          r=[("rt", 2), ("rt", 3)], w=[(dk, tb, 1)])
            if CCUT == "c2":
                break
            for i in range(NT):
                PV, pk = (PG, "pg") if i % 2 == 0 else (PO, "po")
                for k in range(8):
                    ph.op("pe", lambda e, PV=PV, i=i, k=k: e.matmul(PV[:], lhsT=xnT[:, k, i * 128:(i + 1) * 128], rhs=ws1v[:, k, :], start=(k == 0), stop=(k == 7)),
                          r=[("xnT", i), "ws1"], w=[pk])
                ph.op("act", lambda e, PV=PV, i=i: e.activation(out=vt[:, i, :], in_=PV[:], func=AF.Copy), r=[pk], w=[("vt", i)])
            if CCUT == "c3":
                break
            for g4 in range(4):
                for ii in range(4):
                    i = g4 * 4 + ii
                    for dc in range(2):
                        ph.op("pe", lambda e, ii=ii, dc=dc, i=i: e.transpose(out=ptk[:, ii, dc * 128:(dc + 1) * 128], in_=kT[:, dc, i * 128:(i + 1) * 128], identity=identb[:]),
                              r=[("kT", g4, dc)], w=["ptb"])
                ph.op("act", lambda e, g4=g4, h=h: e.activation(out=kf[:, g4 * 4:(g4 + 1) * 4, :], in_=ptk, func=AF.Copy, scale=kd[:, h, 0:1]),
                      r=["ptb", ("kd", h)], w=[("kf", g4)])
                ph.op("dve", lambda e, g4=g4, h=h: e.tensor_scalar(out=kb[:, g4 * 4:(g4 + 1) * 4, :], in0=ptk, scalar1=kd[:, h, 1:2], scalar2=None, op0=ALU.mult),
                      r=["ptb", ("kd", h), ("kf", g4)], w=[("kb", g4)])
            if CCUT == "proj":
                break
            ph.op("dve", lambda e: e.memset(Sb32[:], 0.0), w=["Sb32"])
            for c in range(NT - 1, 0, -1):
                PX, pk = (PA, "pa") if c % 2 == 0 else (PB, "pb")
                for dc in range(2):
                    ph.op("pe", lambda e, PX=PX, dc=dc, c=c: e.matmul(PX[:, dc * 512:(dc + 1) * 512], lhsT=kb[:, c, dc * 128:(dc + 1) * 128], rhs=vt[:, c, :], start=True, stop=True),
                          r=[("kb", c // 4), ("vt", c)], w=[(pk, dc)])
                for dc in range(2):
                    ph.op("dve", lambda e, PX=PX, h=h, dc=dc: e.scalar_tensor_tensor(out=Sb32[:, dc * 512:(dc + 1) * 512], in0=Sb32[:, dc * 512:(dc + 1) * 512],
                                                                              scalar=cdc[:, h, 1:2], in1=PX[:, dc * 512:(dc + 1) * 512], op0=ALU.mult, op1=ALU.add),
                          r=["Sb32", (pk, dc), ("cdc", h)], w=["Sb32"])
                ph.op("act", lambda e, c=c: e.activation(out=Sbst[c % 2][:], in_=Sb32[:], func=AF.Copy), r=["Sb32"], w=[("Sbst", c % 2)])
                ph.op("sp", lambda e, c=c: e.dma_start(out=SBD[c - 1], in_=Sbst[c % 2][:]), r=[("Sbst", c % 2)], w=[("SBD", c - 1)], dma=True)
            if CCUT == "bwd":
                break
            ph.op("dve", lambda e: e.memset(Sf32[:], 0.0), w=["Sf32"])

            def prefetch(c):
                if c < NT - 1:
                    ph.op("sp", lambda e, c=c: e.dma_start(out=Sbin[c % 3][:], in_=SBD[c]), r=[("SBD", c)], w=[("Sbin", c % 3)], dma=True)

            def tail(c):
                b = c % 2
                r0 = s * S + c * 128
                for vc in range(4):
                    ph.op("pe", lambda e, b=b, vc=vc: e.transpose(out=ptb[:, vc, :], in_=go[b][:, vc * 128:(vc + 1) * 128], identity=identb[:]),
                          r=[("go", b)], w=["ptb"])
                ph.op("act", lambda e, b=b: e.activation(out=goT[b][:], in_=ptb[:, 0:4, :], func=AF.Copy), r=["ptb"], w=[("goT", b)])
                for nh in range(2):
                    for vc in range(4):
                        ph.op("pe", lambda e, b=b, nh=nh, vc=vc: e.matmul(PB[:, nh * 512:(nh + 1) * 512], lhsT=goT[b][:, vc, :], rhs=ws3v[:, vc, nh * 512:(nh + 1) * 512],
                                                                         start=(vc == 0), stop=(vc == 3)),
                              r=[("goT", b), "ws3"], w=[("pb", nh)])
                ph.op("act", lambda e, b=b: e.activation(out=mo[b][:, 0:512], in_=PB[:, 0:512], func=AF.Copy), r=[("pb", 0)], w=[("mo", b, 0)])
                ph.op("dve", lambda e, b=b: e.tensor_copy(out=mo[b][:, 512:1024], in_=PB[:, 512:1024]), r=[("pb", 1)], w=[("mo", b, 1)])
                o = ph.op("pool", lambda e, b=b, r0=r0: e.dma_start(out=H[r0:r0 + 128, :], in_=mo[b][:], accum_op=ALU.add),
                          r=[("mo", b, 0), ("mo", b, 1)], w=[], dma=True, after=prev_acc.get(c, []))
                prev_acc[c] = [o]

            prefetch(0)
            prefetch(1)
            for c in range(NT):
                b = c % 2
                tbk = c // 4
                if c < NT - 1:
                    for dc in range(2):
                        ph.op("pe", lambda e, dc=dc, c=c: e.matmul(PA[:, dc * 512:(dc + 1) * 512], lhsT=kf[:, c, dc * 128:(dc + 1) * 128], rhs=vt[:, c, :], start=True, stop=True),
                              r=[("kf", c // 4), ("vt", c)], w=[("pa", dc)])
                for dc in range(2):
                    ph.op("pe", lambda e, dc=dc, c=c: e.matmul(PSC[:], lhsT=kT[:, dc, c * 128:(c + 1) * 128], rhs=qT[:, dc, c * 128:(c + 1) * 128], start=(dc == 0), stop=(dc == 1)),
                          r=[("kT", tbk, 0), ("kT", tbk, 1), ("qT", tbk, 0), ("qT", tbk, 1)], w=["psc"])
                ph.op("dve", lambda e, b=b, h=h: e.tensor_tensor(out=Pm[b][:], in0=PSC[:], in1=Mt[:, h, :], op=ALU.mult), r=["psc", ("Mt", h)], w=[("Pm", b)])
                for d_ in range(2):
                    for dc in range(2):
                        ph.op("dve", lambda e, b=b, d_=d_, dc=dc, c=c, h=h: e.tensor_tensor(out=qfb[b][:, d_, dc, :], in0=qT[:, dc, c * 128:(c + 1) * 128],
                                                                                           in1=qd[:, h, d_, :], op=ALU.mult),
                              r=[("qT", tbk, dc), ("qd", h)], w=[("qfb", b, d_)])
                for k in range(8):
                    ph.op("pe", lambda e, c=c, k=k: e.matmul(PG[:], lhsT=xnT[:, k, c * 128:(c + 1) * 128], rhs=ws2v[:, k, :], start=(k == 0), stop=(k == 7)),
                          r=[("xnT", c), "ws2"], w=["pg"])
                ph.op("act", lambda e, b=b: e.activation(out=sgl[b][:], in_=PG[:], func=AF.Silu), r=["pg"], w=[("sgl", b)])
                mms = [(Pm[b][:], vt[:, c, :], [("Pm", b), ("vt", c)])]
                if c > 0:
                    for dc in range(2):
                        mms.append((qfb[b][:, 0, dc, :], Sfb[c % 2][:, dc * 512:(dc + 1) * 512], [("qfb", b, 0), ("Sfb", c % 2)]))
                if c < NT - 1:
                    for dc in range(2):
                        mms.append((qfb[b][:, 1, dc, :], Sbin[c % 3][:, dc * 512:(dc + 1) * 512], [("qfb", b, 1), ("Sbin", c % 3)]))
                for mi, (l_, r_, ks) in enumerate(mms):
                    ph.op("pe", lambda e, l_=l_, r_=r_, mi=mi, n=len(mms): e.matmul(PO[:], lhsT=l_, rhs=r_, start=(mi == 0), stop=(mi == n - 1)), r=ks, w=["po"])
                if c < NT - 1:
                    for dc in range(2):
                        ph.op("dve", lambda e, h=h, dc=dc: e.scalar_tensor_tensor(out=Sf32[:, dc * 512:(dc + 1) * 512], in0=Sf32[:, dc * 512:(dc + 1) * 512],
                                                                              scalar=cdc[:, h, 0:1], in1=PA[:, dc * 512:(dc + 1) * 512], op0=ALU.mult, op1=ALU.add),
                              r=["Sf32", ("pa", dc), ("cdc", h)], w=["Sf32"])
                    ph.op("act", lambda e, c=c: e.activation(out=Sfb[(c + 1) % 2][:], in_=Sf32[:], func=AF.Copy), r=["Sf32"], w=[("Sfb", (c + 1) % 2)])
                ph.op("dve", lambda e, b=b: e.bn_stats(out=bst[b][:], in_=PO[:]), r=["po"], w=[("bst", b)])
                ph.op("dve", lambda e, b=b: e.bn_aggr(out=mv[b][:, 0:2], in_=bst[b][:]), r=[("bst", b)], w=[("mv", b)])
                yy, kyy = newton(ph, mv[b][:, 1:2], ("mv", b), mv[b], 4, ("gn", b), 1.0, 1e-5)
                ph.op("dve", lambda e, b=b, yy=yy: e.tensor_scalar(out=on[b][:], in0=PO[:], scalar1=mv[b][:, 0:1], scalar2=yy, op0=ALU.subtract, op1=ALU.mult),
                      r=["po", ("mv", b), kyy], w=[("on", b)])
                ph.op("pool", lambda e, b=b: e.tensor_tensor(out=go[b][:], in0=on[b][:], in1=sgl[b][:], op=ALU.mult), r=[("on", b), ("sgl", b)], w=[("go", b)])
                prefetch(c + 2)
                if c > 0 and CCUT != "notail":
                    tail(c - 1)
            if CCUT != "notail":
                tail(NT - 1)
        ph.emit()

    sched = []
    for s in range(nseq):
        sched.append(("A", lambda s=s: phase_A(s)))
    sched.append(("R0", lambda: phase_R(0)))
    sched.append(("B0", lambda: phase_B(0)))
    sched.append(("P0", lambda: phase_P(0, False)))
    for s in range(nseq):
        sched.append(("C", lambda s=s: phase_C(s)))
    sched.append(("R1", lambda: phase_R(1)))
    sched.append(("B1", lambda: phase_B(1)))
    sched.append(("P1", lambda: phase_P(1, True)))
    only = _os.environ.get("SCHED_ONLY", "")
    for name, fn in sched:
        if only and name != only:
            continue
        fn()
        if stop_after is not None and name == stop_after:
            break
    gst.close()
    dbg = {"H": H}
    return nc, dbg


def make_in_maps(inputs, nseq, ncores):
    c = _consts()
    f32 = np.float32
    x = np.asarray(inputs["x"], f32)
    p = np.asarray(inputs["p"], f32)
    shared = dict(
        norm_mix=np.asarray(inputs["norm_mix"], f32), norm_ffn=np.asarray(inputs["norm_ffn"], f32),
        norm_ple=np.asarray(inputs["norm_ple"], f32), final_norm=np.asarray(inputs["final_norm"], f32).reshape(1, D),
        conv_w_in=np.asarray(inputs["conv_w_in"], f32)[0],
        conv_wb=np.ascontiguousarray(np.concatenate([np.asarray(inputs["conv_w"], f32)[0], np.asarray(inputs["conv_b"], f32)], 0)),
        conv_w_out=np.asarray(inputs["conv_w_out"], f32)[0],
        ret_w_in=np.asarray(inputs["ret_w_in"], f32)[0], ret_ld=np.asarray(inputs["ret_log_decay"], f32).reshape(1, 8),
        ret_w_out=np.asarray(inputs["ret_w_out"], f32)[0], router_w=np.asarray(inputs["router_w"], f32),
        exp_w_gate=np.asarray(inputs["exp_w_gate"], f32), exp_w_up=np.asarray(inputs["exp_w_up"], f32),
        exp_w_down=np.asarray(inputs["exp_w_down"], f32), ple_w_proj=np.asarray(inputs["ple_w_proj"], f32),
        ple_w_gate=np.asarray(inputs["ple_w_gate"], f32), **c)
    maps = []
    for ci in range(ncores):
        m = dict(shared)
        m["x"] = np.ascontiguousarray(x[ci * nseq:(ci + 1) * nseq].reshape(nseq * S, D))
        m["p"] = np.ascontiguousarray(p[:, ci * nseq:(ci + 1) * nseq].reshape(2, nseq * S, PLE))
        maps.append(m)
    return maps


def kernel(**inputs):
    B = np.asarray(inputs["x"]).shape[0]
    nseq = B // NCORES
    nc, _ = build(nseq)
    maps = make_in_maps(inputs, nseq, NCORES)
    res = run_bass_kernel_spmd(nc, maps, core_ids=list(range(NCORES)))
    outs = [np.asarray(r["out"], np.float32).reshape(nseq, S, D) for r in res.results]
    return np.concatenate(outs, 0)
```

```python
import numpy as np
from contextlib import ExitStack
import concourse.bass as bass
import concourse.mybir as mybir
from concourse.bass_utils import run_bass_kernel_spmd

F32 = mybir.dt.float32
BF16 = mybir.dt.bfloat16
I32 = mybir.dt.int32
U32 = mybir.dt.uint32
AF = mybir.ActivationFunctionType
ALU = mybir.AluOpType
AX = mybir.AxisListType

ENGS = ["pe", "act", "dve", "pool", "sp"]
N_DMA_SEMS = 20
SEMS = {}


class Op:
    __slots__ = ("eng", "fn", "waits", "idx", "flag", "cnt", "dsem", "dval", "is_dma", "pre")

    def __init__(self, eng, fn, idx, is_dma):
        self.eng = eng
        self.fn = fn
        self.idx = idx
        self.is_dma = is_dma
        self.flag = False
        self.cnt = None
        self.waits = []
        self.dsem = None
        self.dval = None
        self.pre = None


class Phase:
    def __init__(self, nc, name):
        self.nc = nc
        self.name = name
        self.q = {e: [] for e in ENGS}
        self.last_w = {}
        self.readers = {}
        self.stack = ExitStack()
        self.dma_rr = 0
        self.dma_last = [None] * N_DMA_SEMS
        self.dma_cnt = list(SEMS["dcnt"])
        self.n_ops = 0

    def sb(self, name, shape, dt):
        return self.stack.enter_context(self.nc.sbuf_tensor(f"{self.name}_{name}", list(shape), dt))

    def ps(self, name, shape, dt=F32):
        return self.stack.enter_context(self.nc.psum_tensor(f"{self.name}_{name}", list(shape), dt))

    def op(self, eng, fn, r=(), w=(), dma=False, after=(), pe_acc=False):
        o = Op(eng, fn, len(self.q[eng]), dma)
        deps = []
        for k in r:
            lw = self.last_w.get(k)
            if lw is not None:
                deps.append(lw)
        for k in w:
            lw = self.last_w.get(k)
            if lw is not None:
                deps.append(lw)
            deps.extend(self.readers.get(k, ()))
        deps.extend(after)
        seen = set()
        for d in deps:
            if d is o or id(d) in seen:
                continue
            seen.add(id(d))
            if d.eng == "pe" and eng == "pe" and not d.is_dma:
                continue
            o.waits.append(d)
            if not d.is_dma:
                d.flag = True
        if dma:
            s = self.dma_rr % N_DMA_SEMS
            self.dma_rr += 1
            o.pre = self.dma_last[s]
            self.dma_cnt[s] += 1
            o.dsem = s
            o.dval = 16 * self.dma_cnt[s]
            self.dma_last[s] = o
        for k in w:
            self.last_w[k] = o
            self.readers[k] = []
        for k in r:
            if k not in w:
                self.readers.setdefault(k, []).append(o)
        self.q[eng].append(o)
        self.n_ops += 1
        return o

    def emit(self):
        nc = self.nc
        st = self.stack
        esem = SEMS["esem"]
        dsem = SEMS["dsem"]
        ebase = dict(SEMS["ebase"])
        dbase = [16 * c for c in SEMS["dcnt"]]
        final = {}
        for e in ENGS:
            comp = [o for o in self.q[e] if not o.is_dma]
            if comp:
                comp[-1].flag = True
            c = ebase[e]
            for o in self.q[e]:
                if not o.is_dma and o.flag:
                    c += 1
                    o.cnt = c
            final[e] = c
        dfinal = [16 * c for c in self.dma_cnt]
        SEMS["ebase"] = dict(final)
        SEMS["dcnt"] = list(self.dma_cnt)
        q = self.q

        def run(e, eng):
            waited_e = dict(ebase)
            waited_d = list(dbase)
            for o in q[e]:
                ws = list(o.waits)
                if o.pre is not None:
                    ws.append(o.pre)
                for d in ws:
                    if d.is_dma:
                        if waited_d[d.dsem] < d.dval:
                            eng.wait_ge(dsem[d.dsem], d.dval)
                            waited_d[d.dsem] = d.dval
                    else:
                        if waited_e[d.eng] < d.cnt:
                            eng.wait_ge(esem[d.eng], d.cnt)
                            waited_e[d.eng] = d.cnt
                ins = o.fn(eng)
                if o.is_dma:
                    ins.then_inc(dsem[o.dsem], 16)
                elif o.flag:
                    ins.then_inc(esem[e], 1)
            for x in ENGS:
                if final[x] > waited_e[x]:
                    eng.wait_ge(esem[x], final[x])
            for i in range(N_DMA_SEMS):
                if dfinal[i] > waited_d[i]:
                    eng.wait_ge(dsem[i], dfinal[i])

        with nc.Block() as block:
            @block.tensor
            def _(eng):
                run("pe", eng)

            @block.scalar
            def _(eng):
                run("act", eng)

            @block.vector
            def _(eng):
                run("dve", eng)

            @block.gpsimd
            def _(eng):
                run("pool", eng)

            @block.sync
            def _(eng):
                run("sp", eng)
        self.stack.close()


S = 2048
D = 1024
NT = 16
NE = 16
CAP = 256
FF = 2048
PLE = 256
NCORES = 8
import os as _os
CCUT = _os.environ.get('CCUT', '')


def _consts():
    half = 128
    inv = (10000.0 ** (-np.arange(half, dtype=np.float32) / np.float32(half))).astype(np.float32)
    pos = np.arange(S, dtype=np.float32)
    ang = (inv[:, None] * pos[None, :]).astype(np.float32)
    cs = np.stack([np.cos(ang.astype(np.float64)), np.sin(ang.astype(np.float64))], 1).astype(np.float32)
    j = np.arange(128, dtype=np.float32)[:, None]
    i = np.arange(128, dtype=np.float32)[None, :]
    dmat = np.stack([np.maximum(i - j, 0), (j <= i).astype(np.float32),
                     np.maximum(j - i, 0), (j > i).astype(np.float32)], 0).astype(np.float32)
    rows = np.stack([np.broadcast_to(i + 1, (128, 128)), np.broadcast_to(128 - i, (128, 128))], 0).astype(np.float32)
    cols = np.concatenate([127 - j, j, np.full((128, 1), 128.0, np.float32)], 1).astype(np.float32)
    seqoff = ((np.arange(64) // 16) * S).astype(np.float32)[:, None]
    return dict(ident=np.eye(128, dtype=np.float32), cs=np.ascontiguousarray(cs), dmat=dmat,
                rows=np.ascontiguousarray(rows), cols=np.ascontiguousarray(cols), seqoff=seqoff)


def build(nseq, stop_after=None, debug=False):
    nc = bass.Bass("TRN2", target_bir_lowering=False)
    T = nseq * S
    NTL = nseq * NT

    def din(name, shape, dt=F32):
        return nc.dram_tensor(name, list(shape), dt, kind="ExternalInput").ap()

    x = din("x", [T, D])
    p_in = din("p", [2, T, PLE])
    norm_mix = din("norm_mix", [2, D])
    norm_ffn = din("norm_ffn", [2, D])
    norm_ple = din("norm_ple", [2, D])
    final_norm = din("final_norm", [1, D])
    conv_w_in = din("conv_w_in", [D, 3 * D])
    conv_wb = din("conv_wb", [4, D])
    conv_w_out = din("conv_w_out", [D, D])
    ret_w_in = din("ret_w_in", [D, 6 * D])
    ret_ld = din("ret_ld", [1, 8])
    ret_w_out = din("ret_w_out", [2 * D, D])
    router_w = din("router_w", [2, D, NE])
    w_gate = din("exp_w_gate", [2, NE, D, FF])
    w_up = din("exp_w_up", [2, NE, D, FF])
    w_down = din("exp_w_down", [2, NE, FF, D])
    ple_proj = din("ple_w_proj", [2, PLE, D])
    ple_gate = din("ple_w_gate", [2, D, D])
    c_ident = din("ident", [128, 128])
    c_cs = din("cs", [128, 2, S])
    c_dmat = din("dmat", [4, 128, 128])
    c_rows = din("rows", [2, 128, 128])
    c_cols = din("cols", [128, 3])
    c_seqoff = din("seqoff", [64, 1])
    out = nc.dram_tensor("out", [T, D], F32, kind="ExternalOutput").ap()
    H = nc.dram_tensor("Hres", [T, D], F32, kind="ExternalOutput" if debug else "Internal").ap()
    XN = nc.dram_tensor("XNs", [T, D], BF16, kind="Internal").ap()
    SBD = nc.dram_tensor("SBD", [16, 128, 1024], BF16, kind="Internal").ap()

    gst = ExitStack()
    SEMS["esem"] = {e: gst.enter_context(nc.semaphore(f"s_{e}")) for e in ENGS}
    SEMS["dsem"] = [gst.enter_context(nc.semaphore(f"d{i}")) for i in range(N_DMA_SEMS)]
    SEMS["ebase"] = {e: 0 for e in ENGS}
    SEMS["dcnt"] = [0] * N_DMA_SEMS
    identf = gst.enter_context(nc.sbuf_tensor("g_identf", [128, 128], F32))
    identb = gst.enter_context(nc.sbuf_tensor("g_identb", [128, 128], BF16))
    aff_tok = gst.enter_context(nc.sbuf_tensor("g_aff", [128, NT, 64], F32))
    epsn = gst.enter_context(nc.sbuf_tensor("g_eps", [128, 2], F32))

    ph = Phase(nc, "I")
    ph.op("sp", lambda e: e.dma_start(out=identf[:], in_=c_ident), w=["idf"], dma=True)
    ph.op("dve", lambda e: e.tensor_copy(out=identb[:], in_=identf[:]), r=["idf"], w=["idb"])
    ph.op("dve", lambda e: e.memset(aff_tok[:], 0.0), w=["aff"])
    ph.op("dve", lambda e: e.memset(epsn[:, 0:1], 1e-6), w=["eps0"])
    ph.op("dve", lambda e: e.memset(epsn[:, 1:2], 1e-5), w=["eps1"])
    ph.emit()

    def newton(ph, src, skey, tile, c0, tag, scale, eps, it_eng="pool"):
        v = tile[:, c0:c0 + 1]
        y = tile[:, c0 + 1:c0 + 2]
        t = tile[:, c0 + 2:c0 + 3]
        vi = v.bitcast(I32)
        yi = y.bitcast(I32)
        kv, ky, kt = ("nv", tag), ("ny", tag), ("nt", tag)
        ph.op("dve", lambda e: e.tensor_scalar(out=v, in0=src, scalar1=scale, scalar2=eps, op0=ALU.mult, op1=ALU.add), r=[skey], w=[kv])
        ph.op("dve", lambda e: e.tensor_single_scalar(out=yi, in_=vi, scalar=1, op=ALU.logical_shift_right), r=[kv], w=[ky])
        ph.op("dve", lambda e: e.tensor_scalar(out=yi, in0=yi, scalar1=-1, scalar2=0x5f3759df, op0=ALU.mult, op1=ALU.add), r=[ky], w=[ky])
        for _ in range(2):
            if it_eng == "dve":
                ph.op("dve", lambda e: e.scalar_tensor_tensor(out=t, in0=v, scalar=y, in1=y, op0=ALU.mult, op1=ALU.mult), r=[kv, ky], w=[kt])
            else:
                ph.op(it_eng, lambda e: e.tensor_tensor(out=t, in0=v, in1=y, op=ALU.mult), r=[kv, ky], w=[kt])
                ph.op(it_eng, lambda e: e.tensor_tensor(out=t, in0=t, in1=y, op=ALU.mult), r=[kt, ky], w=[kt])
            ph.op(it_eng, lambda e: e.tensor_scalar(out=t, in0=t, scalar1=-0.5, scalar2=1.5, op0=ALU.mult, op1=ALU.add), r=[kt], w=[kt])
            ph.op(it_eng, lambda e: e.tensor_tensor(out=y, in0=y, in1=t, op=ALU.mult), r=[kt, ky], w=[ky])
        return y, ky

    def rmsnorm(ph, hin, hkey, gb, gkey, outt, okey, junk, sst, tag, it_eng="pool"):
        hkeys = hkey if isinstance(hkey, list) else [hkey]
        ph.op("act", lambda e: e.activation(out=junk[:], in_=hin, func=AF.Square, accum_out=sst[:, 3:4]),
              r=hkeys, w=[("ss", tag)])
        y, ky = newton(ph, sst[:, 3:4], ("ss", tag), sst, 0, tag, 1.0 / D, 1e-6, it_eng)
        ph.op("dve", lambda e: e.scalar_tensor_tensor(out=outt, in0=hin, scalar=y, in1=gb[:], op0=ALU.mult, op1=ALU.mult),
              r=hkeys + [ky, gkey], w=[okey])

    def transpose8(ph, src, skey, ptb, pkey, dst, dkey, eng, n=8):
        for k in range(n):
            ph.op("pe", lambda e, k=k: e.transpose(out=ptb[:, k, :], in_=src[:, k * 128:(k + 1) * 128], identity=identb[:]),
                  r=[skey], w=[pkey])
        if eng == "act":
            ph.op("act", lambda e: e.activation(out=dst, in_=ptb[:, 0:n, :], func=AF.Copy), r=[pkey], w=[dkey])
        else:
            ph.op("dve", lambda e: e.tensor_copy(out=dst, in_=ptb[:, 0:n, :]), r=[pkey], w=[dkey])

    def phase_A(s):
        ph = Phase(nc, f"A{s}")
        xnT = ph.sb("xnT", [128, 8, S], BF16)
        zT = ph.sb("zT", [128, 8, S], BF16)
        wout = ph.sb("wout", [128, 8, D], BF16)
        win = [ph.sb(f"win{i}", [128, 3, 8, 128], BF16) for i in range(2)]
        gmix = ph.sb("gmix", [128, D], F32)
        cw4 = ph.sb("cw4", [4, D], F32)
        cwb = ph.sb("cwb", [128, 8, 4], F32)
        xt = [ph.sb(f"xt{i}", [128, D], F32) for i in range(2)]
        junk = ph.sb("junk", [128, D], BF16)
        sst = [ph.sb(f"sst{i}", [128, 4], F32) for i in range(2)]
        xn = [ph.sb(f"xn{i}", [128, D], BF16) for i in range(2)]
        u = [ph.sb(f"u{i}", [128, S + 2], F32) for i in range(2)]
        bsb = [ph.sb(f"bsb{i}", [128, S], F32) for i in range(2)]
        csb = [ph.sb(f"csb{i}", [128, 512], F32) for i in range(2)]
        yc = [ph.sb(f"yc{i}", [128, S], F32) for i in range(2)]
        hn = [ph.sb(f"hn{i}", [128, D], F32) for i in range(2)]
        ptb = [ph.ps(f"ptb{i}", [128, 8, 128], BF16) for i in range(2)]
        PP = [ph.ps(f"pp{i}", [128, 1024], F32) for i in range(3)]

        ph.op("sp", lambda e: e.dma_start(out=gmix[:], in_=norm_mix[0:1, :].partition_broadcast(128)), w=["gmix"], dma=True)
        ph.op("sp", lambda e: e.dma_start(out=cw4[:], in_=conv_wb), w=["cw4"], dma=True)
        ph.op("pool", lambda e: e.dma_start(out=wout[:], in_=conv_w_out.rearrange("(k p) n -> p k n", p=128)), w=["wout"], dma=True)
        cps = PP[0][:, 0:32].rearrange("p (j w) -> p j w", w=4)
        for j in range(8):
            ph.op("pe", lambda e, j=j: e.transpose(out=cps[:, j, :], in_=cw4[:, j * 128:(j + 1) * 128], identity=identf[0:4, 0:4]),
                  r=["cw4"], w=[("pp", 0, 0)])
        ph.op("dve", lambda e: e.tensor_copy(out=cwb[:], in_=cps), r=[("pp", 0, 0)], w=["cwb"])
        for i in range(2):
            ph.op("dve", lambda e, i=i: e.memset(u[i][:, 0:1], 0.0), w=[("u", i)])
            ph.op("dve", lambda e, i=i: e.memset(u[i][:, S + 1:S + 2], 0.0), w=[("u", i)])
        for i in range(NT):
            b = i % 2
            r0 = s * S + i * 128
            ph.op("sp", lambda e, b=b, r0=r0: e.dma_start(out=xt[b][:], in_=x[r0:r0 + 128, :]), w=[("xt", b)], dma=True)
            rmsnorm(ph, xt[b][:], ("xt", b), gmix, "gmix", xn[b][:], ("xn", b), junk, sst[b], b)
            transpose8(ph, xn[b], ("xn", b), ptb[b], ("ptb", b), xnT[:, :, i * 128:(i + 1) * 128], ("xnT", i), "act")
        xnT_keys = [("xnT", i) for i in range(NT)]
        w_in_v = conv_w_in.rearrange("(k p) (t n) -> p t k n", p=128, t=3)
        for j in range(8):
            jb = j % 2
            ph.op("pool", lambda e, j=j, jb=jb: e.dma_start(out=win[jb][:], in_=w_in_v[:, :, :, j * 128:(j + 1) * 128]),
                  w=[("win", jb)], dma=True)
            for tb in range(4):
                q = (j * 4 + tb) % 2
                bps = PP[q][:, 0:512]
                cps_ = PP[q][:, 512:1024]
                vps = PP[2][:, q * 512:(q + 1) * 512]
                tks = [("xnT", i) for i in range(tb * 4, tb * 4 + 4)]
                for t, (dst, key) in enumerate([(bps, ("pp", q, 0)), (cps_, ("pp", q, 1)), (vps, ("pp", 2, q))]):
                    for k in range(8):
                        ph.op("pe", lambda e, dst=dst, t=t, k=k, jb=jb, tb=tb: e.matmul(
                            dst, lhsT=win[jb][:, t, k, :], rhs=xnT[:, k, tb * 512:(tb + 1) * 512], start=(k == 0), stop=(k == 7)),
                            r=[("win", jb)] + tks, w=[key])
                ph.op("act", lambda e, q=q, cps_=cps_: e.activation(out=csb[q][:], in_=cps_, func=AF.Copy), r=[("pp", q, 1)], w=[("csb", q)])
                ph.op("act", lambda e, jb=jb, tb=tb, bps=bps: e.activation(out=bsb[jb][:, tb * 512:(tb + 1) * 512], in_=bps, func=AF.Copy),
                      r=[("pp", q, 0)], w=[("bsb", jb)])
                ph.op("dve", lambda e, jb=jb, tb=tb, q=q, vps=vps: e.tensor_tensor(
                    out=u[jb][:, 1 + tb * 512:1 + (tb + 1) * 512], in0=csb[q][:], in1=vps, op=ALU.mult),
                    r=[("csb", q), ("pp", 2, q)], w=[("u", jb)])
            ph.op("act", lambda e, j=j, jb=jb: e.activation(out=yc[jb][:], in_=u[jb][:, 1:S + 1], func=AF.Identity,
                                                            bias=cwb[:, j, 3:4], scale=cwb[:, j, 1:2]),
                  r=[("u", jb), "cwb"], w=[("yc", jb)])
            ph.op("dve", lambda e, j=j, jb=jb: e.scalar_tensor_tensor(out=yc[jb][:], in0=u[jb][:, 0:S], scalar=cwb[:, j, 0:1], in1=yc[jb][:],
                                                                     op0=ALU.mult, op1=ALU.add),
                  r=[("u", jb), "cwb", ("yc", jb)], w=[("yc", jb)])
            ph.op("dve", lambda e, j=j, jb=jb: e.scalar_tensor_tensor(out=yc[jb][:], in0=u[jb][:, 2:S + 2], scalar=cwb[:, j, 2:3], in1=yc[jb][:],
                                                                      op0=ALU.mult, op1=ALU.add),
                  r=[("u", jb), "cwb", ("yc", jb)], w=[("yc", jb)])
            ph.op("pool", lambda e, j=j, jb=jb: e.tensor_tensor(out=zT[:, j, :], in0=bsb[jb][:], in1=yc[jb][:], op=ALU.mult),
                  r=[("bsb", jb), ("yc", jb)], w=[("zT", j)])
        zkeys = [("zT", j) for j in range(8)]
        def loads_A3(i):
            b = i % 2
            r0 = s * S + i * 128
            ph.op("sp", lambda e, b=b, r0=r0: e.dma_start(out=xt[b][:], in_=x[r0:r0 + 128, :]), w=[("xt", b)], dma=True)

        loads_A3(0)
        for i in range(NT):
            b = i % 2
            r0 = s * S + i * 128
            if i + 1 < NT:
                loads_A3(i + 1)
            for nh in range(2):
                for k in range(8):
                    ph.op("pe", lambda e, b=b, nh=nh, k=k, i=i: e.matmul(
                        PP[b][:, nh * 512:(nh + 1) * 512], lhsT=zT[:, k, i * 128:(i + 1) * 128], rhs=wout[:, k, nh * 512:(nh + 1) * 512],
                        start=(k == 0), stop=(k == 7)), r=zkeys + ["wout"], w=[("pp", b, nh)])
                ph.op("dve", lambda e, b=b, nh=nh: e.tensor_tensor(out=hn[b][:, nh * 512:(nh + 1) * 512], in0=xt[b][:, nh * 512:(nh + 1) * 512],
                                                                  in1=PP[b][:, nh * 512:(nh + 1) * 512], op=ALU.add),
                      r=[("xt", b), ("pp", b, nh)], w=[("hn", b, nh)])
            ph.op("sp", lambda e, b=b, r0=r0: e.dma_start(out=H[r0:r0 + 128, :], in_=hn[b][:]), r=[("hn", b, 0), ("hn", b, 1)], w=[("H", r0)], dma=True)
        ph.emit()

    def phase_R(l):
        ph = Phase(nc, f"R{l}")
        NB = 4
        gffn = ph.sb("gffn", [128, D], F32)
        wr = ph.sb("wr", [128, 8, NE], BF16)
        hn = [ph.sb(f"hn{i}", [128, D], F32) for i in range(NB)]
        junk = ph.sb("junk", [128, D], BF16)
        sst = [ph.sb(f"sst{i}", [128, 4], F32) for i in range(NB)]
        xn = [ph.sb(f"xn{i}", [128, D], BF16) for i in range(NB)]
        xT = [ph.sb(f"xT{i}", [128, 8, 128], BF16) for i in range(NB)]
        sm = [ph.sb(f"sm{i}", [128, 4], F32) for i in range(NB)]
        ex = [ph.sb(f"ex{i}", [128, NE], F32) for i in range(NB)]
        ptb = [ph.ps(f"ptb{i}", [128, 8, 128], BF16) for i in range(2)]
        PL = [ph.ps(f"pl{i}", [128, NE], F32) for i in range(2)]
        ph.op("sp", lambda e: e.dma_start(out=gffn[:], in_=norm_ffn[l:l + 1, :].partition_broadcast(128)), w=["gffn"], dma=True)
        ph.op("pool", lambda e: e.dma_start(out=wr[:], in_=router_w[l].rearrange("(k p) n -> p k n", p=128)), w=["wr"], dma=True)
        def loads_R(ti):
            b = ti % NB
            r0 = ti * 128
            ph.op("sp", lambda e, b=b, r0=r0: e.dma_start(out=hn[b][:], in_=H[r0:r0 + 128, :]), w=[("hn", b)], dma=True)

        PDIST = 2
        for ti in range(min(PDIST, NTL)):
            loads_R(ti)
        for ti in range(NTL):
            b = ti % NB
            pb2 = ti % 2
            s, i = divmod(ti, NT)
            r0 = ti * 128
            if ti + PDIST < NTL:
                loads_R(ti + PDIST)
            rmsnorm(ph, hn[b][:], ("hn", b), gffn, "gffn", xn[b][:], ("xn", b), junk, sst[b], b)
            ph.op("sp", lambda e, b=b, r0=r0: e.dma_start(out=XN[r0:r0 + 128, :], in_=xn[b][:]), r=[("xn", b)], w=[("XN", r0)], dma=True)
            transpose8(ph, xn[b], ("xn", b), ptb[pb2], ("ptb", pb2), xT[b][:], ("xT", b), "act")
            for k in range(8):
                ph.op("pe", lambda e, b=b, k=k, pb2=pb2: e.matmul(PL[pb2][:], lhsT=xT[b][:, k, :], rhs=wr[:, k, :], start=(k == 0), stop=(k == 7)),
                      r=[("xT", b), "wr"], w=[("pl", pb2)])
            ph.op("dve", lambda e, b=b, pb2=pb2: e.reduce_max(out=sm[b][:, 0:1], in_=PL[pb2][:], axis=AX.X), r=[("pl", pb2)], w=[("mx", b)])
            ph.op("dve", lambda e, b=b: e.tensor_scalar(out=sm[b][:, 1:2], in0=sm[b][:, 0:1], scalar1=-1.0, scalar2=None, op0=ALU.mult),
                  r=[("mx", b)], w=[("nmx", b)])
            ph.op("act", lambda e, b=b, pb2=pb2: e.activation(out=ex[b][:], in_=PL[pb2][:], func=AF.Exp, bias=sm[b][:, 1:2], scale=1.0, accum_out=sm[b][:, 2:3]),
                  r=[("pl", pb2), ("nmx", b)], w=[("ex", b), ("sum", b)])
            ph.op("dve", lambda e, b=b: e.reciprocal(out=sm[b][:, 3:4], in_=sm[b][:, 2:3]), r=[("sum", b)], w=[("rsum", b)])
            ph.op("dve", lambda e, b=b, s=s, i=i: e.tensor_scalar(out=aff_tok[:, i, s * 16:(s + 1) * 16], in0=ex[b][:], scalar1=sm[b][:, 3:4],
                                                                 scalar2=None, op0=ALU.mult),
                  r=[("ex", b), ("rsum", b)], w=[("aff", ti)])
        ph.emit()

    def phase_P(l, final):
        ph = Phase(nc, f"P{l}")
        NB = 4
        wpg = ph.sb("wpg", [128, 8, D], BF16)
        wpp = ph.sb("wpp", [128, 2, D], BF16)
        gple = ph.sb("gple", [128, D], F32)
        gnx = ph.sb("gnx", [128, D], F32)
        hn = [ph.sb(f"hn{i}", [128, D], F32) for i in range(NB)]
        pt = [ph.sb(f"pt{i}", [128, PLE], F32) for i in range(NB)]
        pb = [ph.sb(f"pb{i}", [128, PLE], BF16) for i in range(NB)]
        pT = [ph.sb(f"pT{i}", [128, 2, 128], BF16) for i in range(NB)]
        junk = ph.sb("junk", [128, D], BF16)
        sst = [ph.sb(f"sst{i}", [128, 4], F32) for i in range(2 * NB)]
        xn = [ph.sb(f"xn{i}", [128, D], BF16) for i in range(NB)]
        xT = [ph.sb(f"xT{i}", [128, 8, 128], BF16) for i in range(NB)]
        sgm = [ph.sb(f"sgm{i}", [128, D], F32) for i in range(NB)]
        h2 = [ph.sb(f"h2{i}", [128, D], F32) for i in range(NB)]
        xo = [ph.sb(f"xo{i}", [128, D], F32 if final else BF16) for i in range(NB)]
        ptb = [ph.ps(f"ptb{i}", [128, 8, 128], BF16) for i in range(2)]
        ptp = ph.ps("ptp", [128, 8, 128], BF16)
        PG = ph.ps("pg", [128, 1024], F32)
        PQ = ph.ps("pq", [128, 1024], F32)
        ph.op("pool", lambda e: e.dma_start(out=wpg[:], in_=ple_gate[l].rearrange("(k p) n -> p k n", p=128)), w=["wpg"], dma=True)
        ph.op("pool", lambda e: e.dma_start(out=wpp[:], in_=ple_proj[l].rearrange("(k p) n -> p k n", p=128)), w=["wpp"], dma=True)
        ph.op("sp", lambda e: e.dma_start(out=gple[:], in_=norm_ple[l:l + 1, :].partition_broadcast(128)), w=["gple"], dma=True)
        gsrc = final_norm[0:1, :] if final else norm_mix[l + 1:l + 2, :]
        ph.op("sp", lambda e: e.dma_start(out=gnx[:], in_=gsrc.partition_broadcast(128)), w=["gnx"], dma=True)
        def loads_P(ti):
            b = ti % NB
            r0 = ti * 128
            ph.op("sp", lambda e, b=b, r0=r0: e.dma_start(out=hn[b][:], in_=H[r0:r0 + 128, :]), w=[("hn", b)], dma=True)
            ph.op("sp", lambda e, b=b, r0=r0: e.dma_start(out=pt[b][:], in_=p_in[l, r0:r0 + 128, :]), w=[("pt", b)], dma=True)

        PDIST = 2
        for ti in range(min(PDIST, NTL)):
            loads_P(ti)
        for ti in range(NTL):
            b = ti % NB
            pb2 = ti % 2
            r0 = ti * 128
            if ti + PDIST < NTL:
                loads_P(ti + PDIST)
            rmsnorm(ph, hn[b][:], ("hn", b), gple, "gple", xn[b][:], ("xn", b), junk, sst[b], b, "dve")
            transpose8(ph, xn[b], ("xn", b), ptb[pb2], ("ptb", pb2), xT[b][:], ("xT", b), "act")
            ph.op("pool", lambda e, b=b: e.tensor_copy(out=pb[b][:], in_=pt[b][:]), r=[("pt", b)], w=[("pb", b)])
            transpose8(ph, pb[b], ("pb", b), ptp, "ptp", pT[b][:], ("pT", b), "dve", n=2)
            for nh in range(2):
                for k in range(8):
                    ph.op("pe", lambda e, b=b, nh=nh, k=k: e.matmul(PG[:, nh * 512:(nh + 1) * 512], lhsT=xT[b][:, k, :],
                                                                   rhs=wpg[:, k, nh * 512:(nh + 1) * 512], start=(k == 0), stop=(k == 7)),
                          r=[("xT", b), "wpg"], w=[("pg", nh)])
                for k in range(2):
                    ph.op("pe", lambda e, b=b, nh=nh, k=k: e.matmul(PQ[:, nh * 512:(nh + 1) * 512], lhsT=pT[b][:, k, :],
                                                                   rhs=wpp[:, k, nh * 512:(nh + 1) * 512], start=(k == 0), stop=(k == 1)),
                          r=[("pT", b), "wpp"], w=[("pq", nh)])
                sl = slice(nh * 512, (nh + 1) * 512)
                ph.op("act", lambda e, b=b, sl=sl: e.activation(out=sgm[b][:, sl], in_=PG[:, sl], func=AF.Sigmoid), r=[("pg", nh)], w=[("sgm", b, nh)])
                ph.op("dve", lambda e, b=b, sl=sl: e.tensor_tensor(out=sgm[b][:, sl], in0=sgm[b][:, sl], in1=PQ[:, sl], op=ALU.mult),
                      r=[("sgm", b, nh), ("pq", nh)], w=[("sgm", b, nh)])
                ph.op("pool", lambda e, b=b, sl=sl: e.tensor_tensor(out=h2[b][:, sl], in0=sgm[b][:, sl], in1=hn[b][:, sl], op=ALU.add),
                      r=[("sgm", b, nh), ("hn", b)], w=[("h2", b, nh)])
            hk = [("h2", b, 0), ("h2", b, 1)]
            if not final:
                ph.op("sp", lambda e, b=b, r0=r0: e.dma_start(out=H[r0:r0 + 128, :], in_=h2[b][:]), r=hk, w=[("H", r0)], dma=True)
            rmsnorm(ph, h2[b][:], hk, gnx, "gnx", xo[b][:], ("xo", b), junk, sst[NB + b], ("n2", b))
            dst = out if final else XN
            ph.op("sp", lambda e, b=b, r0=r0, dst=dst: e.dma_start(out=dst[r0:r0 + 128, :], in_=xo[b][:]), r=[("xo", b)], w=[("O", r0)], dma=True)
        ph.emit()

    def phase_B(l):
        ph = Phase(nc, f"B{l}")
        NTOK = nseq * CAP
        ntile = nseq * 2
        TB = min(512, NTOK)
        nblk = NTOK // TB
        work = ph.sb("work", [64, S], F32)
        gates = ph.sb("gates", [64, CAP], F32)
        idxu = ph.sb("idxu", [64, CAP], U32)
        idxf = ph.sb("idxf", [64, CAP], F32)
        soff = ph.sb("soff", [64, 1], F32)
        gT = ph.sb("gT", [128, 2, 64], F32)
        iTi = ph.sb("iTi", [128, 2, 64], I32)
        ring = [ph.sb(f"ring{i}", [128, 8192], BF16) for i in range(4)]
        xg = [ph.sb(f"xg{i}", [128, ntile, D], BF16) for i in range(2)]
        xgT = ph.sb("xgT", [128, 8, NTOK], BF16)
        hT = ph.sb("hT", [128, 16, NTOK], BF16)
        sg = [ph.sb(f"sg{i}", [128, 512], F32) for i in range(2)]
        yt = [ph.sb(f"yt{i}", [128, D], F32) for i in range(2)]
        ptb = [ph.ps(f"ptb{i}", [128, 8, 128], BF16) for i in range(2)]
        PGU = [ph.ps(f"pgu{i}", [128, 1024], F32) for i in range(2)]
        PD = ph.ps("pd", [128, 1024], F32)

        ph.op("sp", lambda e: e.dma_start(out=soff[:], in_=c_seqoff), w=["soff"], dma=True)
        for g in range(4):
            for ii in range(4):
                i = g * 4 + ii
                ph.op("pe", lambda e, g=g, ii=ii, i=i: e.transpose(out=PGU[g % 2][0:64, ii * 128:(ii + 1) * 128], in_=aff_tok[:, i, :], identity=identf[:]),
                      r=["aff"], w=[("pgu", g % 2, 0)])
            ph.op("dve", lambda e, g=g: e.tensor_copy(out=work[:, g * 512:(g + 1) * 512], in_=PGU[g % 2][0:64, 0:512]),
                  r=[("pgu", g % 2, 0)], w=["work"])
        for r in range(CAP // 8):
            sl = slice(r * 8, (r + 1) * 8)
            ph.op("dve", lambda e, sl=sl: e.max(out=gates[:, sl], in_=work[:]), r=["work"], w=[("gt", r)])
            ph.op("dve", lambda e, sl=sl: e.max_index(out=idxu[:, sl], in_max=gates[:, sl], in_values=work[:]), r=["work", ("gt", r)], w=[("ix", r)])
            ph.op("dve", lambda e, sl=sl: e.match_replace(out=work[:], in_to_replace=gates[:, sl], in_values=work[:], imm_value=-1.0),
                  r=["work", ("gt", r)], w=["work"])
        gkeys = [("gt", r) for r in range(CAP // 8)]
        ikeys = [("ix", r) for r in range(CAP // 8)]
        ph.op("dve", lambda e: e.tensor_copy(out=idxf[:], in_=idxu[:]), r=ikeys, w=["idxf"])
        ph.op("dve", lambda e: e.tensor_scalar(out=idxf[:], in0=idxf[:], scalar1=soff[:, 0:1], scalar2=None, op0=ALU.add), r=["idxf", "soff"], w=["idxf"])
        tp = PGU[0][:, 0:256].rearrange("p (a c) -> p a c", c=64)
        for hf in range(2):
            ph.op("pe", lambda e, hf=hf: e.transpose(out=tp[:, hf, :], in_=gates[:, hf * 128:(hf + 1) * 128], identity=identf[0:64, 0:64]),
                  r=gkeys, w=[("pgu", 0, 0)])
            ph.op("pe", lambda e, hf=hf: e.transpose(out=tp[:, 2 + hf, :], in_=idxf[:, hf * 128:(hf + 1) * 128], identity=identf[0:64, 0:64]),
                  r=["idxf"], w=[("pgu", 0, 0)])
        ph.op("dve", lambda e: e.tensor_copy(out=gT[:], in_=tp[:, 0:2, :]), r=[("pgu", 0, 0)], w=["gT"])
        ph.op("act", lambda e: e.activation(out=iTi[:], in_=tp[:, 2:4, :], func=AF.Copy), r=[("pgu", 0, 0)], w=["iTi"])

        wg_v = w_gate[l].rearrange("e (k p) f -> e p k f", p=128)
        wu_v = w_up[l].rearrange("e (k p) f -> e p k f", p=128)
        wd_v = w_down[l].rearrange("e (k p) n -> e p k n", p=128)

        def load_slab(g):
            e_, j = divmod(g, 6)
            if e_ >= NE:
                return
            slot = g % 4
            if j < 4:
                dstg = ring[slot][:, 0:4096].rearrange("p (k n) -> p k n", n=512)
                dstu = ring[slot][:, 4096:8192].rearrange("p (k n) -> p k n", n=512)
                ph.op("pool", lambda e: e.dma_start(out=dstg, in_=wg_v[e_, :, :, j * 512:(j + 1) * 512]), w=[("ring", slot, 0)], dma=True)
                ph.op("pool", lambda e: e.dma_start(out=dstu, in_=wu_v[e_, :, :, j * 512:(j + 1) * 512]), w=[("ring", slot, 1)], dma=True)
            else:
                dh = j - 4
                dst = ring[slot][:].rearrange("p (k n) -> p k n", n=1024)
                ph.op("pool", lambda e: e.dma_start(out=dst, in_=wd_v[e_, :, dh * 8:(dh + 1) * 8, :]), w=[("ring", slot, 0), ("ring", slot, 1)], dma=True)

        def gather(e_):
            if e_ >= NE:
                return
            for t in range(ntile):
                s, hf = divmod(t, 2)
                col = s * 16 + e_
                ph.op("pool", lambda e, t=t, hf=hf, col=col: e.indirect_dma_start(
                    out=xg[e_ % 2][:, t, :], out_offset=None, in_=XN[:, :],
                    in_offset=bass.IndirectOffsetOnAxis(ap=iTi[:, hf, col:col + 1], axis=0)),
                    r=["iTi"], w=[("xg", e_ % 2, t)], dma=True)

        def transposes(e_):
            if e_ >= NE:
                return
            for t in range(ntile):
                transpose8(ph, xg[e_ % 2][:, t, :], ("xg", e_ % 2, t), ptb[t % 2], ("ptb", t % 2),
                           xgT[:, :, t * 128:(t + 1) * 128], ("xgT", t), "act" if t % 2 == 0 else "dve")

        for g in range(4):
            load_slab(g)
        gather(0)
        transposes(0)
        gather(1)
        prev_sc = {}
        cur_sc = {}
        cnt = 0
        for e_ in range(NE):
            for fg in range(4):
                g = e_ * 6 + fg
                slot = g % 4
                sv = ring[slot][:].rearrange("p (a k n) -> p a k n", a=2, k=8)
                for fc4 in range(4):
                    fc = fg * 4 + fc4
                    for hb in range(nblk):
                        q = cnt % 2
                        cnt += 1
                        tks = [("xgT", t) for t in range(hb * (TB // 128), (hb + 1) * (TB // 128))]
                        for a in range(2):
                            for k in range(8):
                                ph.op("pe", lambda e, q=q, a=a, k=k, sv=sv, fc4=fc4, hb=hb: e.matmul(
                                    PGU[q][:, a * 512:a * 512 + TB], lhsT=sv[:, a, k, fc4 * 128:(fc4 + 1) * 128],
                                    rhs=xgT[:, k, hb * TB:(hb + 1) * TB], start=(k == 0), stop=(k == 7)),
                                    r=[("ring", slot, a)] + tks, w=[("pgu", q, a)])
                        ph.op("act", lambda e, q=q: e.activation(out=sg[q][:, 0:TB], in_=PGU[q][:, 0:TB], func=AF.Silu), r=[("pgu", q, 0)], w=[("sg", q)])
                        ph.op("dve", lambda e, q=q, fc=fc, hb=hb: e.tensor_tensor(out=hT[:, fc, hb * TB:(hb + 1) * TB], in0=sg[q][:, 0:TB],
                                                                                 in1=PGU[q][:, 512:512 + TB], op=ALU.mult),
                              r=[("sg", q), ("pgu", q, 1)], w=[("hT", fc, hb)])
                load_slab(g + 4)
            transposes(e_ + 1)
            gather(e_ + 2)
            d0 = ring[(e_ * 6 + 4) % 4][:].rearrange("p (k n) -> p k n", n=1024)
            d1 = ring[(e_ * 6 + 5) % 4][:].rearrange("p (k n) -> p k n", n=1024)
            dkeys = [("ring", (e_ * 6 + 4) % 4, 0), ("ring", (e_ * 6 + 4) % 4, 1), ("ring", (e_ * 6 + 5) % 4, 0), ("ring", (e_ * 6 + 5) % 4, 1)]
            for t in range(ntile):
                s, hf = divmod(t, 2)
                col = s * 16 + e_
                hb = (t * 128) // TB
                b = t % 2
                for nh in range(2):
                    for fc in range(16):
                        dsl = d0 if fc < 8 else d1
                        ph.op("pe", lambda e, nh=nh, fc=fc, dsl=dsl, t=t: e.matmul(
                            PD[:, nh * 512:(nh + 1) * 512], lhsT=hT[:, fc, t * 128:(t + 1) * 128], rhs=dsl[:, fc % 8, nh * 512:(nh + 1) * 512],
                            start=(fc == 0), stop=(fc == 15)), r=dkeys + [("hT", fc_, hb) for fc_ in range(16)], w=[("pd", nh)])
                    sl = slice(nh * 512, (nh + 1) * 512)
                    if nh == 0:
                        ph.op("act", lambda e, b=b, sl=sl, hf=hf, col=col: e.activation(out=yt[b][:, sl], in_=PD[:, sl], func=AF.Copy, scale=gT[:, hf, col:col + 1]),
                              r=[("pd", nh), "gT"], w=[("yt", b, nh)])
                    else:
                        ph.op("dve", lambda e, b=b, sl=sl, hf=hf, col=col: e.tensor_scalar(out=yt[b][:, sl], in0=PD[:, sl], scalar1=gT[:, hf, col:col + 1],
                                                                                          scalar2=None, op0=ALU.mult),
                              r=[("pd", nh), "gT"], w=[("yt", b, nh)])
                o = ph.op("pool", lambda e, b=b, hf=hf, col=col: e.indirect_dma_start(
                    out=H[:, :], out_offset=bass.IndirectOffsetOnAxis(ap=iTi[:, hf, col:col + 1], axis=0),
                    in_=yt[b][:, :], in_offset=None, compute_op=ALU.add),
                    r=[("yt", b, 0), ("yt", b, 1), "iTi"], w=[], dma=True, after=prev_sc.get(s, []))
                cur_sc.setdefault(s, []).append(o)
            prev_sc = cur_sc
            cur_sc = {}
            load_slab(e_ * 6 + 4 + 4)
            load_slab(e_ * 6 + 5 + 4)
        ph.emit()

    def phase_C(s):
        ph = Phase(nc, f"C{s}")
        xnT = ph.sb("xnT", [128, 8, S], BF16)
        xl = [ph.sb(f"xl{i}", [128, D], BF16) for i in range(2)]
        ws = [ph.sb(f"ws{i}", [128, 4096], BF16) for i in range(4)]
        cs = [ph.sb(f"cs{i}", [128, 2, 512], F32) for i in range(1)]
        qT = ph.sb("qT", [128, 2, S], BF16)
        kT = ph.sb("kT", [128, 2, S], BF16)
        kf = ph.sb("kf", [128, NT, 256], BF16)
        kb = ph.sb("kb", [128, NT, 256], BF16)
        vt = ph.sb("vt", [128, NT, 512], BF16)
        rt = [ph.sb(f"rt{i}", [128, 512], F32) for i in range(4)]
        Sf32 = ph.sb("Sf32", [128, 1024], F32)
        Sb32 = [ph.sb(f"Sb32{i}", [128, 1024], F32) for i in range(2)]
        Sfb = [ph.sb(f"Sfb{i}", [128, 1024], BF16) for i in range(2)]
        Sbst = [ph.sb(f"Sbst{i}", [128, 1024], BF16) for i in range(2)]
        Sbin = [ph.sb(f"Sbin{i}", [128, 1024], BF16) for i in range(3)]
        ldb = ph.sb("ldb", [128, 8], F32)
        Mt = ph.sb("Mt", [128, 4, 128], F32)
        tmpM = [ph.sb(f"tmpM{i}", [128, 128], F32) for i in range(2)]
        qd = ph.sb("qd", [128, 4, 2, 2, 128], F32)
        kd = ph.sb("kd", [128, 4, 2], F32)
        cdc = ph.sb("cdc", [128, 4, 2], F32)
        dm = ph.sb("dm", [128, 4, 128], F32)
        rw = ph.sb("rw", [128, 2, 128], F32)
        cl = ph.sb("cl", [128, 3], F32)
        Pm = [ph.sb(f"Pm{i}", [128, 128], BF16) for i in range(2)]
        qfb = [ph.sb(f"qfb{i}", [128, 2, 2, 128], BF16) for i in range(2)]
        sgl = [ph.sb(f"sgl{i}", [128, 512], F32) for i in range(2)]
        on = [ph.sb(f"on{i}", [128, 512], F32) for i in range(2)]
        go = [ph.sb(f"go{i}", [128, 512], BF16) for i in range(2)]
        goT = [ph.sb(f"goT{i}", [128, 4, 128], BF16) for i in range(2)]
        mo = [ph.sb(f"mo{i}", [128, D], F32) for i in range(2)]
        bst = [ph.sb(f"bst{i}", [128, 6], F32) for i in range(2)]
        mv = [ph.sb(f"mv{i}", [128, 8], F32) for i in range(2)]
        ptb = ph.ps("ptb", [128, 8, 128], BF16)
        PG = ph.ps("pg", [128, 512], F32)
        PA = ph.ps("pa", [128, 1024], F32)
        PB = ph.ps("pb", [128, 1024], F32)
        PO = ph.ps("po", [128, 512], F32)
        PSC = ph.ps("psc", [128, 128], F32)
        ptk = ptb[:].rearrange("p a b -> p (a b)").rearrange("p (a b) -> p a b", b=256)

        ph.op("sp", lambda e: e.dma_start(out=ldb[:], in_=ret_ld.partition_broadcast(128)), w=["ldb"], dma=True)
        ph.op("sp", lambda e: e.dma_start(out=dm[:], in_=c_dmat.rearrange("a j i -> j a i")), w=["dm"], dma=True)
        ph.op("sp", lambda e: e.dma_start(out=rw[:], in_=c_rows.rearrange("a p i -> p a i")), w=["rw"], dma=True)
        ph.op("sp", lambda e: e.dma_start(out=cl[:], in_=c_cols), w=["cl"], dma=True)
        for h in range(4):
            ph.op("act", lambda e, h=h: e.activation(out=tmpM[0][:], in_=dm[:, 0, :], func=AF.Exp, scale=ldb[:, h:h + 1]), r=["dm", "ldb"], w=["tmA"])
            ph.op("dve", lambda e, h=h: e.tensor_tensor(out=tmpM[0][:], in0=tmpM[0][:], in1=dm[:, 1, :], op=ALU.mult), r=["tmA", "dm"], w=["tmA"])
            ph.op("act", lambda e, h=h: e.activation(out=tmpM[1][:], in_=dm[:, 2, :], func=AF.Exp, scale=ldb[:, 4 + h:5 + h]), r=["dm", "ldb"], w=["tmB"])
            ph.op("dve", lambda e, h=h: e.tensor_tensor(out=tmpM[1][:], in0=tmpM[1][:], in1=dm[:, 3, :], op=ALU.mult), r=["tmB", "dm"], w=["tmB"])
            ph.op("dve", lambda e, h=h: e.tensor_tensor(out=Mt[:, h, :], in0=tmpM[0][:], in1=tmpM[1][:], op=ALU.add), r=["tmA", "tmB"], w=[("Mt", h)])
            for d_ in range(2):
                lc = ldb[:, 4 * d_ + h:4 * d_ + h + 1]
                for dc in range(2):
                    ph.op("act", lambda e, h=h, d_=d_, lc=lc, dc=dc: e.activation(out=qd[:, h, d_, dc, :], in_=rw[:, d_, :], func=AF.Exp, scale=lc), r=["rw", "ldb"], w=[("qd", h)])
                ph.op("act", lambda e, h=h, d_=d_, lc=lc: e.activation(out=kd[:, h, d_:d_ + 1], in_=cl[:, d_:d_ + 1], func=AF.Exp, scale=lc), r=["cl", "ldb"], w=[("kd", h)])
                ph.op("act", lambda e, h=h, d_=d_, lc=lc: e.activation(out=cdc[:, h, d_:d_ + 1], in_=cl[:, 2:3], func=AF.Exp, scale=lc), r=["cl", "ldb"], w=[("cdc", h)])
        for i in range(NT):
            b = i % 2
            r0 = s * S + i * 128
            ph.op("sp", lambda e, b=b, r0=r0: e.dma_start(out=xl[b][:], in_=XN[r0:r0 + 128, :]), w=[("xl", b)], dma=True)
            transpose8(ph, xl[b], ("xl", b), ptb, "ptb", xnT[:, :, i * 128:(i + 1) * 128], ("xnT", i), "act" if b == 0 else "dve")
        allx = [("xnT", i) for i in range(NT)]
        if CCUT == "c1":
            ph.emit()
            return
        w_in_v = ret_w_in.rearrange("(k p) n -> p k n", p=128)
        w_out_v = ret_w_out.rearrange("(k p) n -> p k n", p=128)
        ws0v = ws[0][:].rearrange("p (k n) -> p k n", n=512)
        ws1v = ws[1][:].rearrange("p (k n) -> p k n", n=512)
        ws2v = ws[2][:].rearrange("p (k n) -> p k n", n=512)
        ws3v = ws[3][:].rearrange("p (k n) -> p k n", n=1024)
        prev_acc = {}
        cnt = 0
        for h in range(4):
            ph.op("pool", lambda e, h=h: e.dma_start(out=ws0v[:, :, 0:256], in_=w_in_v[:, :, h * 256:(h + 1) * 256]), w=[("ws0", 0)], dma=True)
            ph.op("pool", lambda e, h=h: e.dma_start(out=ws0v[:, :, 256:512], in_=w_in_v[:, :, 1024 + h * 256:1024 + (h + 1) * 256]), w=[("ws0", 1)], dma=True)
            ph.op("pool", lambda e, h=h: e.dma_start(out=ws1v, in_=w_in_v[:, :, 2048 + h * 512:2048 + (h + 1) * 512]), w=["ws1"], dma=True)
            ph.op("pool", lambda e, h=h: e.dma_start(out=ws2v, in_=w_in_v[:, :, 4096 + h * 512:4096 + (h + 1) * 512]), w=["ws2"], dma=True)
            ph.op("pool", lambda e, h=h: e.dma_start(out=ws3v, in_=w_out_v[:, h * 4:(h + 1) * 4, :]), w=["ws3"], dma=True)
            for tb in range(4):
                cb_ = 0
                ph.op("sp", lambda e, cb_=cb_, tb=tb: e.dma_start(out=cs[cb_][:], in_=c_cs[:, :, tb * 512:(tb + 1) * 512]), w=[("cs", cb_)], dma=True)
                tks = [("xnT", i) for i in range(tb * 4, tb * 4 + 4)]
                for which in range(2):
                    PX, pk = (PA, "pa") if cnt % 2 == 0 else (PB, "pb")
                    cnt += 1
                    off = which * 256
                    for dc in range(2):
                        for k in range(8):
                            ph.op("pe", lambda e, PX=PX, dc=dc, k=k, off=off, tb=tb: e.matmul(
                                PX[:, dc * 512:(dc + 1) * 512], lhsT=ws0v[:, k, off + dc * 128:off + (dc + 1) * 128],
                                rhs=xnT[:, k, tb * 512:(tb + 1) * 512], start=(k == 0), stop=(k == 7)),
                                r=[("ws0", which)] + tks, w=[(pk, dc)])
                    sc = 1.0 if which == 0 else 1.0 / 16.0
                    combos = [(0, 0), (1, 1), (0, 1), (1, 0)]
                    for ri, (xi, ci) in enumerate(combos):
                        ph.op("dve", lambda e, PX=PX, ri=ri, xi=xi, ci=ci, sc=sc, cb_=cb_: e.scalar_tensor_tensor(
                            out=rt[ri][:], in0=PX[:, xi * 512:(xi + 1) * 512], scalar=sc, in1=cs[cb_][:, ci, :], op0=ALU.mult, op1=ALU.mult),
                            r=[(pk, xi), ("cs", cb_)], w=[("rt", ri)])
                    dstT, dk = (qT, "qT") if which == 0 else (kT, "kT")
                    ph.op("pool", lambda e, dstT=dstT, tb=tb: e.tensor_tensor(out=dstT[:, 0, tb * 512:(tb + 1) * 512], in0=rt[0][:], in1=rt[1][:], op=ALU.subtract),
                          r=[("rt", 0), ("rt", 1)], w=[(dk, tb, 0)])
                    ph.op("pool", lambda e, dstT=dstT, tb=tb: e.tensor_tensor(out=dstT[:, 1, tb * 512:(tb + 1) * 512], in0=rt[2][:], in1=rt[3][:], op=ALU.add),
                          r=[("rt", 2), ("rt", 3)], w=[(dk, tb, 1)])
            if CCUT == "c2":
                break
            for i in range(NT):
                PV, pk = (PG, "pg") if i % 2 == 0 else (PO, "po")
                for k in range(8):
                    ph.op("pe", lambda e, PV=PV, i=i, k=k: e.matmul(PV[:], lhsT=xnT[:, k, i * 128:(i + 1) * 128], rhs=ws1v[:, k, :], start=(k == 0), stop=(k == 7)),
                          r=[("xnT", i), "ws1"], w=[pk])
                ph.op("act", lambda e, PV=PV, i=i: e.activation(out=vt[:, i, :], in_=PV[:], func=AF.Copy), r=[pk], w=[("vt", i)])
            if CCUT == "c3":
                break
            for g4 in range(4):
                for ii in range(4):
                    i = g4 * 4 + ii
                    for dc in range(2):
                        ph.op("pe", lambda e, ii=ii, dc=dc, i=i: e.transpose(out=ptk[:, ii, dc * 128:(dc + 1) * 128], in_=kT[:, dc, i * 128:(i + 1) * 128], identity=identb[:]),
                              r=[("kT", g4, dc)], w=["ptb"])
                ph.op("act", lambda e, g4=g4, h=h: e.activation(out=kf[:, g4 * 4:(g4 + 1) * 4, :], in_=ptk, func=AF.Copy, scale=kd[:, h, 0:1]),
                      r=["ptb", ("kd", h)], w=[("kf", g4)])
                ph.op("dve", lambda e, g4=g4, h=h: e.tensor_scalar(out=kb[:, g4 * 4:(g4 + 1) * 4, :], in0=ptk, scalar1=kd[:, h, 1:2], scalar2=None, op0=ALU.mult),
                      r=["ptb", ("kd", h), ("kf", g4)], w=[("kb", g4)])
            if CCUT == "proj":
                break
            ph.op("dve", lambda e: e.memset(Sb32[1][:], 0.0), w=[("Sb32", 1, 0), ("Sb32", 1, 1)])
            for c in range(NT - 1, 0, -1):
                PX, pk = (PA, "pa") if c % 2 == 0 else (PB, "pb")
                src_, dst_ = Sb32[c % 2], Sb32[(c + 1) % 2]
                for dc in range(2):
                    ph.op("pe", lambda e, PX=PX, dc=dc, c=c: e.matmul(PX[:, dc * 512:(dc + 1) * 512], lhsT=kb[:, c, dc * 128:(dc + 1) * 128], rhs=vt[:, c, :], start=True, stop=True),
                          r=[("kb", c // 4), ("vt", c)], w=[(pk, dc)])
                for dc in range(2):
                    ph.op("dve", lambda e, PX=PX, h=h, dc=dc, src_=src_, dst_=dst_: e.scalar_tensor_tensor(
                        out=dst_[:, dc * 512:(dc + 1) * 512], in0=src_[:, dc * 512:(dc + 1) * 512],
                        scalar=cdc[:, h, 1:2], in1=PX[:, dc * 512:(dc + 1) * 512], op0=ALU.mult, op1=ALU.add),
                        r=[("Sb32", c % 2, dc), (pk, dc), ("cdc", h)], w=[("Sb32", (c + 1) % 2, dc)])
                ph.op("act", lambda e, c=c, dst_=dst_: e.activation(out=Sbst[c % 2][:], in_=dst_[:], func=AF.Copy),
                      r=[("Sb32", (c + 1) % 2, 0), ("Sb32", (c + 1) % 2, 1)], w=[("Sbst", c % 2)])
                ph.op("sp", lambda e, c=c: e.dma_start(out=SBD[c - 1], in_=Sbst[c % 2][:]), r=[("Sbst", c % 2)], w=[("SBD", c - 1)], dma=True)
            if CCUT == "bwd":
                break
            ph.op("dve", lambda e: e.memset(Sf32[:], 0.0), w=["Sf32"])

            def prefetch(c):
                if c < NT - 1:
                    ph.op("sp", lambda e, c=c: e.dma_start(out=Sbin[c % 3][:], in_=SBD[c]), r=[("SBD", c)], w=[("Sbin", c % 3)], dma=True)

            def tail(c):
                b = c % 2
                r0 = s * S + c * 128
                for vc in range(4):
                    ph.op("pe", lambda e, b=b, vc=vc: e.transpose(out=ptb[:, vc, :], in_=go[b][:, vc * 128:(vc + 1) * 128], identity=identb[:]),
                          r=[("go", b)], w=["ptb"])
                ph.op("act", lambda e, b=b: e.activation(out=goT[b][:], in_=ptb[:, 0:4, :], func=AF.Copy), r=["ptb"], w=[("goT", b)])
                for nh in range(2):
                    for vc in range(4):
                        ph.op("pe", lambda e, b=b, nh=nh, vc=vc: e.matmul(PB[:, nh * 512:(nh + 1) * 512], lhsT=goT[b][:, vc, :], rhs=ws3v[:, vc, nh * 512:(nh + 1) * 512],
                                                                         start=(vc == 0), stop=(vc == 3)),
                              r=[("goT", b), "ws3"], w=[("pb", nh)])
                ph.op("act", lambda e, b=b: e.activation(out=mo[b][:, 0:512], in_=PB[:, 0:512], func=AF.Copy), r=[("pb", 0)], w=[("mo", b, 0)])
                ph.op("act", lambda e, b=b: e.activation(out=mo[b][:, 512:1024], in_=PB[:, 512:1024], func=AF.Copy), r=[("pb", 1)], w=[("mo", b, 1)])
                o = ph.op("pool", lambda e, b=b, r0=r0: e.dma_start(out=H[r0:r0 + 128, :], in_=mo[b][:], accum_op=ALU.add),
                          r=[("mo", b, 0), ("mo", b, 1)], w=[], dma=True, after=prev_acc.get(c, []))
                prev_acc[c] = [o]

            prefetch(0)
            prefetch(1)
            for c in range(NT):
                b = c % 2
                tbk = c // 4
                if c < NT - 1:
                    for dc in range(2):
                        ph.op("pe", lambda e, dc=dc, c=c: e.matmul(PA[:, dc * 512:(dc + 1) * 512], lhsT=kf[:, c, dc * 128:(dc + 1) * 128], rhs=vt[:, c, :], start=True, stop=True),
                              r=[("kf", c // 4), ("vt", c)], w=[("pa", dc)])
                for dc in range(2):
                    ph.op("pe", lambda e, dc=dc, c=c: e.matmul(PSC[:], lhsT=kT[:, dc, c * 128:(c + 1) * 128], rhs=qT[:, dc, c * 128:(c + 1) * 128], start=(dc == 0), stop=(dc == 1)),
                          r=[("kT", tbk, 0), ("kT", tbk, 1), ("qT", tbk, 0), ("qT", tbk, 1)], w=["psc"])
                ph.op("dve", lambda e, b=b, h=h: e.tensor_tensor(out=Pm[b][:], in0=PSC[:], in1=Mt[:, h, :], op=ALU.mult), r=["psc", ("Mt", h)], w=[("Pm", b)])
                for d_ in range(2):
                    ph.op("dve", lambda e, b=b, d_=d_, c=c, h=h: e.tensor_tensor(out=qfb[b][:, d_, :, :], in0=qT[:, :, c * 128:(c + 1) * 128],
                                                                                 in1=qd[:, h, d_, :, :], op=ALU.mult),
                          r=[("qT", tbk, 0), ("qT", tbk, 1), ("qd", h)], w=[("qfb", b, d_)])
                for k in range(8):
                    ph.op("pe", lambda e, c=c, k=k: e.matmul(PG[:], lhsT=xnT[:, k, c * 128:(c + 1) * 128], rhs=ws2v[:, k, :], start=(k == 0), stop=(k == 7)),
                          r=[("xnT", c), "ws2"], w=["pg"])
                ph.op("act", lambda e, b=b: e.activation(out=sgl[b][:], in_=PG[:], func=AF.Silu), r=["pg"], w=[("sgl", b)])
                mms = [(Pm[b][:], vt[:, c, :], [("Pm", b), ("vt", c)])]
                if c > 0:
                    for dc in range(2):
                        mms.append((qfb[b][:, 0, dc, :], Sfb[c % 2][:, dc * 512:(dc + 1) * 512], [("qfb", b, 0), ("Sfb", c % 2)]))
                if c < NT - 1:
                    for dc in range(2):
                        mms.append((qfb[b][:, 1, dc, :], Sbin[c % 3][:, dc * 512:(dc + 1) * 512], [("qfb", b, 1), ("Sbin", c % 3)]))
                for mi, (l_, r_, ks) in enumerate(mms):
                    ph.op("pe", lambda e, l_=l_, r_=r_, mi=mi, n=len(mms): e.matmul(PO[:], lhsT=l_, rhs=r_, start=(mi == 0), stop=(mi == n - 1)), r=ks, w=["po"])
                if c < NT - 1:
                    for dc in range(2):
                        ph.op("dve", lambda e, h=h, dc=dc: e.scalar_tensor_tensor(out=Sf32[:, dc * 512:(dc + 1) * 512], in0=Sf32[:, dc * 512:(dc + 1) * 512],
                                                                              scalar=cdc[:, h, 0:1], in1=PA[:, dc * 512:(dc + 1) * 512], op0=ALU.mult, op1=ALU.add),
                              r=["Sf32", ("pa", dc), ("cdc", h)], w=["Sf32"])
                    ph.op("act", lambda e, c=c: e.activation(out=Sfb[(c + 1) % 2][:], in_=Sf32[:], func=AF.Copy), r=["Sf32"], w=[("Sfb", (c + 1) % 2)])
                ph.op("dve", lambda e, b=b: e.bn_stats(out=bst[b][:], in_=PO[:]), r=["po"], w=[("bst", b)])
                ph.op("dve", lambda e, b=b: e.bn_aggr(out=mv[b][:, 0:2], in_=bst[b][:]), r=[("bst", b)], w=[("mv", b)])
                yy, kyy = newton(ph, mv[b][:, 1:2], ("mv", b), mv[b], 4, ("gn", b), 1.0, 1e-5)
                ph.op("dve", lambda e, b=b, yy=yy: e.tensor_scalar(out=on[b][:], in0=PO[:], scalar1=mv[b][:, 0:1], scalar2=yy, op0=ALU.subtract, op1=ALU.mult),
                      r=["po", ("mv", b), kyy], w=[("on", b)])
                ph.op("pool", lambda e, b=b: e.tensor_tensor(out=go[b][:], in0=on[b][:], in1=sgl[b][:], op=ALU.mult), r=[("on", b), ("sgl", b)], w=[("go", b)])
                prefetch(c + 2)
                if c > 0 and CCUT != "notail":
                    tail(c - 1)
            if CCUT != "notail":
                tail(NT - 1)
        ph.emit()

    sched = []
    for s in range(nseq):
        sched.append(("A", lambda s=s: phase_A(s)))
    sched.append(("R0", lambda: phase_R(0)))
    sched.append(("B0", lambda: phase_B(0)))
    sched.append(("P0", lambda: phase_P(0, False)))
    for s in range(nseq):
        sched.append(("C", lambda s=s: phase_C(s)))
    sched.append(("R1", lambda: phase_R(1)))
    sched.append(("B1", lambda: phase_B(1)))
    sched.append(("P1", lambda: phase_P(1, True)))
    only = _os.environ.get("SCHED_ONLY", "")
    for name, fn in sched:
        if only and name != only:
            continue
        fn()
        if stop_after is not None and name == stop_after:
            break
    gst.close()
    dbg = {"H": H}
    return nc, dbg


def make_in_maps(inputs, nseq, ncores):
    c = _consts()
    f32 = np.float32
    x = np.asarray(inputs["x"], f32)
    p = np.asarray(inputs["p"], f32)
    shared = dict(
        norm_mix=np.asarray(inputs["norm_mix"], f32), norm_ffn=np.asarray(inputs["norm_ffn"], f32),
        norm_ple=np.asarray(inputs["norm_ple"], f32), final_norm=np.asarray(inputs["final_norm"], f32).reshape(1, D),
        conv_w_in=np.asarray(inputs["conv_w_in"], f32)[0],
        conv_wb=np.ascontiguousarray(np.concatenate([np.asarray(inputs["conv_w"], f32)[0], np.asarray(inputs["conv_b"], f32)], 0)),
        conv_w_out=np.asarray(inputs["conv_w_out"], f32)[0],
        ret_w_in=np.asarray(inputs["ret_w_in"], f32)[0], ret_ld=np.asarray(inputs["ret_log_decay"], f32).reshape(1, 8),
        ret_w_out=np.asarray(inputs["ret_w_out"], f32)[0], router_w=np.asarray(inputs["router_w"], f32),
        exp_w_gate=np.asarray(inputs["exp_w_gate"], f32), exp_w_up=np.asarray(inputs["exp_w_up"], f32),
        exp_w_down=np.asarray(inputs["exp_w_down"], f32), ple_w_proj=np.asarray(inputs["ple_w_proj"], f32),
        ple_w_gate=np.asarray(inputs["ple_w_gate"], f32), **c)
    maps = []
    for ci in range(ncores):
        m = dict(shared)
        m["x"] = np.ascontiguousarray(x[ci * nseq:(ci + 1) * nseq].reshape(nseq * S, D))
        m["p"] = np.ascontiguousarray(p[:, ci * nseq:(ci + 1) * nseq].reshape(2, nseq * S, PLE))
        maps.append(m)
    return maps


def kernel(**inputs):
    B = np.asarray(inputs["x"]).shape[0]
    nseq = B // NCORES
    nc, _ = build(nseq)
    maps = make_in_maps(inputs, nseq, NCORES)
    res = run_bass_kernel_spmd(nc, maps, core_ids=list(range(NCORES)))
    outs = [np.asarray(r["out"], np.float32).reshape(nseq, S, D) for r in res.results]
    return np.concatenate(outs, 0)
```

```python
import numpy as np
from contextlib import ExitStack
import concourse.bass as bass
import concourse.mybir as mybir
from concourse.bass_utils import run_bass_kernel_spmd

F32 = mybir.dt.float32
BF16 = mybir.dt.bfloat16
I32 = mybir.dt.int32
U32 = mybir.dt.uint32
AF = mybir.ActivationFunctionType
ALU = mybir.AluOpType
AX = mybir.AxisListType

ENGS = ["pe", "act", "dve", "pool", "sp"]
N_DMA_SEMS = 20
SEMS = {}


class Op:
    __slots__ = ("eng", "fn", "waits", "idx", "flag", "cnt", "dsem", "dval", "is_dma", "pre")

    def __init__(self, eng, fn, idx, is_dma):
        self.eng = eng
        self.fn = fn
        self.idx = idx
        self.is_dma = is_dma
        self.flag = False
        self.cnt = None
        self.waits = []
        self.dsem = None
        self.dval = None
        self.pre = None


class Phase:
    def __init__(self, nc, name):
        self.nc = nc
        self.name = name
        self.q = {e: [] for e in ENGS}
        self.last_w = {}
        self.readers = {}
        self.stack = ExitStack()
        self.dma_rr = 0
        self.dma_last = [None] * N_DMA_SEMS
        self.dma_cnt = list(SEMS["dcnt"])
        self.n_ops = 0

    def sb(self, name, shape, dt):
        return self.stack.enter_context(self.nc.sbuf_tensor(f"{self.name}_{name}", list(shape), dt))

    def ps(self, name, shape, dt=F32):
        return self.stack.enter_context(self.nc.psum_tensor(f"{self.name}_{name}", list(shape), dt))

    def op(self, eng, fn, r=(), w=(), dma=False, after=(), pe_acc=False):
        o = Op(eng, fn, len(self.q[eng]), dma)
        deps = []
        for k in r:
            lw = self.last_w.get(k)
            if lw is not None:
                deps.append(lw)
        for k in w:
            lw = self.last_w.get(k)
            if lw is not None:
                deps.append(lw)
            deps.extend(self.readers.get(k, ()))
        deps.extend(after)
        seen = set()
        for d in deps:
            if d is o or id(d) in seen:
                continue
            seen.add(id(d))
            if d.eng == "pe" and eng == "pe" and not d.is_dma:
                continue
            o.waits.append(d)
            if not d.is_dma:
                d.flag = True
        if dma:
            s = self.dma_rr % N_DMA_SEMS
            self.dma_rr += 1
            o.pre = self.dma_last[s]
            self.dma_cnt[s] += 1
            o.dsem = s
            o.dval = 16 * self.dma_cnt[s]
            self.dma_last[s] = o
        for k in w:
            self.last_w[k] = o
            self.readers[k] = []
        for k in r:
            if k not in w:
                self.readers.setdefault(k, []).append(o)
        self.q[eng].append(o)
        self.n_ops += 1
        return o

    def emit(self):
        nc = self.nc
        st = self.stack
        esem = SEMS["esem"]
        dsem = SEMS["dsem"]
        ebase = dict(SEMS["ebase"])
        dbase = [16 * c for c in SEMS["dcnt"]]
        final = {}
        for e in ENGS:
            comp = [o for o in self.q[e] if not o.is_dma]
            if comp:
                comp[-1].flag = True
            c = ebase[e]
            for o in self.q[e]:
                if not o.is_dma and o.flag:
                    c += 1
                    o.cnt = c
            final[e] = c
        dfinal = [16 * c for c in self.dma_cnt]
        SEMS["ebase"] = dict(final)
        SEMS["dcnt"] = list(self.dma_cnt)
        q = self.q

        def run(e, eng):
            waited_e = dict(ebase)
            waited_d = list(dbase)
            for o in q[e]:
                ws = list(o.waits)
                if o.pre is not None:
                    ws.append(o.pre)
                for d in ws:
                    if d.is_dma:
                        if waited_d[d.dsem] < d.dval:
                            eng.wait_ge(dsem[d.dsem], d.dval)
                            waited_d[d.dsem] = d.dval
                    else:
                        if waited_e[d.eng] < d.cnt:
                            eng.wait_ge(esem[d.eng], d.cnt)
                            waited_e[d.eng] = d.cnt
                ins = o.fn(eng)
                if o.is_dma:
                    ins.then_inc(dsem[o.dsem], 16)
                elif o.flag:
                    ins.then_inc(esem[e], 1)
            for x in ENGS:
                if final[x] > waited_e[x]:
                    eng.wait_ge(esem[x], final[x])
            for i in range(N_DMA_SEMS):
                if dfinal[i] > waited_d[i]:
                    eng.wait_ge(dsem[i], dfinal[i])

        with nc.Block() as block:
            @block.tensor
            def _(eng):
                run("pe", eng)

            @block.scalar
            def _(eng):
                run("act", eng)

            @block.vector
            def _(eng):
                run("dve", eng)

            @block.gpsimd
            def _(eng):
                run("pool", eng)

            @block.sync
            def _(eng):
                run("sp", eng)
        self.stack.close()


S = 2048
D = 1024
NT = 16
NE = 16
CAP = 256
FF = 2048
PLE = 256
NCORES = 8
import os as _os
CCUT = _os.environ.get('CCUT', '')


def _consts():
    half = 128
    inv = (10000.0 ** (-np.arange(half, dtype=np.float32) / np.float32(half))).astype(np.float32)
    pos = np.arange(S, dtype=np.float32)
    ang = (inv[:, None] * pos[None, :]).astype(np.float32)
    cs = np.stack([np.cos(ang.astype(np.float64)), np.sin(ang.astype(np.float64))], 1).astype(np.float32)
    j = np.arange(128, dtype=np.float32)[:, None]
    i = np.arange(128, dtype=np.float32)[None, :]
    dmat = np.stack([np.maximum(i - j, 0), (j <= i).astype(np.float32),
                     np.maximum(j - i, 0), (j > i).astype(np.float32)], 0).astype(np.float32)
    rows = np.stack([np.broadcast_to(i + 1, (128, 128)), np.broadcast_to(128 - i, (128, 128))], 0).astype(np.float32)
    cols = np.concatenate([127 - j, j, np.full((128, 1), 128.0, np.float32)], 1).astype(np.float32)
    seqoff = ((np.arange(64) // 16) * S).astype(np.float32)[:, None]
    return dict(ident=np.eye(128, dtype=np.float32), cs=np.ascontiguousarray(cs), dmat=dmat,
                rows=np.ascontiguousarray(rows), cols=np.ascontiguousarray(cols), seqoff=seqoff)


def build(nseq, stop_after=None, debug=False):
    nc = bass.Bass("TRN2", target_bir_lowering=False)
    T = nseq * S
    NTL = nseq * NT

    def din(name, shape, dt=F32):
        return nc.dram_tensor(name, list(shape), dt, kind="ExternalInput").ap()

    x = din("x", [T, D])
    p_in = din("p", [2, T, PLE])
    norm_mix = din("norm_mix", [2, D])
    norm_ffn = din("norm_ffn", [2, D])
    norm_ple = din("norm_ple", [2, D])
    final_norm = din("final_norm", [1, D])
    conv_w_in = din("conv_w_in", [D, 3 * D])
    conv_wb = din("conv_wb", [4, D])
    conv_w_out = din("conv_w_out", [D, D])
    ret_w_in = din("ret_w_in", [D, 6 * D])
    ret_ld = din("ret_ld", [1, 8])
    ret_w_out = din("ret_w_out", [2 * D, D])
    router_w = din("router_w", [2, D, NE])
    w_gate = din("exp_w_gate", [2, NE, D, FF])
    w_up = din("exp_w_up", [2, NE, D, FF])
    w_down = din("exp_w_down", [2, NE, FF, D])
    ple_proj = din("ple_w_proj", [2, PLE, D])
    ple_gate = din("ple_w_gate", [2, D, D])
    c_ident = din("ident", [128, 128])
    c_cs = din("cs", [128, 2, S])
    c_dmat = din("dmat", [4, 128, 128])
    c_rows = din("rows", [2, 128, 128])
    c_cols = din("cols", [128, 3])
    c_seqoff = din("seqoff", [64, 1])
    out = nc.dram_tensor("out", [T, D], F32, kind="ExternalOutput").ap()
    H = nc.dram_tensor("Hres", [T, D], F32, kind="ExternalOutput" if debug else "Internal").ap()
    XN = nc.dram_tensor("XNs", [T, D], BF16, kind="Internal").ap()
    SBD = nc.dram_tensor("SBD", [16, 128, 1024], BF16, kind="Internal").ap()

    gst = ExitStack()
    SEMS["esem"] = {e: gst.enter_context(nc.semaphore(f"s_{e}")) for e in ENGS}
    SEMS["dsem"] = [gst.enter_context(nc.semaphore(f"d{i}")) for i in range(N_DMA_SEMS)]
    SEMS["ebase"] = {e: 0 for e in ENGS}
    SEMS["dcnt"] = [0] * N_DMA_SEMS
    identf = gst.enter_context(nc.sbuf_tensor("g_identf", [128, 128], F32))
    identb = gst.enter_context(nc.sbuf_tensor("g_identb", [128, 128], BF16))
    aff_tok = gst.enter_context(nc.sbuf_tensor("g_aff", [128, NT, 64], F32))
    epsn = gst.enter_context(nc.sbuf_tensor("g_eps", [128, 2], F32))

    ph = Phase(nc, "I")
    ph.op("sp", lambda e: e.dma_start(out=identf[:], in_=c_ident), w=["idf"], dma=True)
    ph.op("dve", lambda e: e.tensor_copy(out=identb[:], in_=identf[:]), r=["idf"], w=["idb"])
    ph.op("dve", lambda e: e.memset(aff_tok[:], 0.0), w=["aff"])
    ph.op("dve", lambda e: e.memset(epsn[:, 0:1], 1e-6), w=["eps0"])
    ph.op("dve", lambda e: e.memset(epsn[:, 1:2], 1e-5), w=["eps1"])
    ph.emit()

    def newton(ph, src, skey, tile, c0, tag, scale, eps, it_eng="pool"):
        v = tile[:, c0:c0 + 1]
        y = tile[:, c0 + 1:c0 + 2]
        t = tile[:, c0 + 2:c0 + 3]
        vi = v.bitcast(I32)
        yi = y.bitcast(I32)
        kv, ky, kt = ("nv", tag), ("ny", tag), ("nt", tag)
        ph.op("dve", lambda e: e.tensor_scalar(out=v, in0=src, scalar1=scale, scalar2=eps, op0=ALU.mult, op1=ALU.add), r=[skey], w=[kv])
        ph.op("dve", lambda e: e.tensor_single_scalar(out=yi, in_=vi, scalar=1, op=ALU.logical_shift_right), r=[kv], w=[ky])
        ph.op("dve", lambda e: e.tensor_scalar(out=yi, in0=yi, scalar1=-1, scalar2=0x5f3759df, op0=ALU.mult, op1=ALU.add), r=[ky], w=[ky])
        for _ in range(2):
            if it_eng == "dve":
                ph.op("dve", lambda e: e.scalar_tensor_tensor(out=t, in0=v, scalar=y, in1=y, op0=ALU.mult, op1=ALU.mult), r=[kv, ky], w=[kt])
            else:
                ph.op(it_eng, lambda e: e.tensor_tensor(out=t, in0=v, in1=y, op=ALU.mult), r=[kv, ky], w=[kt])
                ph.op(it_eng, lambda e: e.tensor_tensor(out=t, in0=t, in1=y, op=ALU.mult), r=[kt, ky], w=[kt])
            ph.op(it_eng, lambda e: e.tensor_scalar(out=t, in0=t, scalar1=-0.5, scalar2=1.5, op0=ALU.mult, op1=ALU.add), r=[kt], w=[kt])
            ph.op(it_eng, lambda e: e.tensor_tensor(out=y, in0=y, in1=t, op=ALU.mult), r=[kt, ky], w=[ky])
        return y, ky

    def rmsnorm(ph, hin, hkey, gb, gkey, outt, okey, junk, sst, tag, it_eng="pool"):
        hkeys = hkey if isinstance(hkey, list) else [hkey]
        ph.op("act", lambda e: e.activation(out=junk[:], in_=hin, func=AF.Square, accum_out=sst[:, 3:4]),
              r=hkeys, w=[("ss", tag)])
        y, ky = newton(ph, sst[:, 3:4], ("ss", tag), sst, 0, tag, 1.0 / D, 1e-6, it_eng)
        ph.op("dve", lambda e: e.scalar_tensor_tensor(out=outt, in0=hin, scalar=y, in1=gb[:], op0=ALU.mult, op1=ALU.mult),
              r=hkeys + [ky, gkey], w=[okey])

    def transpose8(ph, src, skey, ptb, pkey, dst, dkey, eng, n=8):
        for k in range(n):
            ph.op("pe", lambda e, k=k: e.transpose(out=ptb[:, k, :], in_=src[:, k * 128:(k + 1) * 128], identity=identb[:]),
                  r=[skey], w=[pkey])
        if eng == "act":
            ph.op("act", lambda e: e.activation(out=dst, in_=ptb[:, 0:n, :], func=AF.Copy), r=[pkey], w=[dkey])
        else:
            ph.op("dve", lambda e: e.tensor_copy(out=dst, in_=ptb[:, 0:n, :]), r=[pkey], w=[dkey])

    def phase_A(s):
        ph = Phase(nc, f"A{s}")
        xnT = ph.sb("xnT", [128, 8, S], BF16)
        zT = ph.sb("zT", [128, 8, S], BF16)
        wout = ph.sb("wout", [128, 8, D], BF16)
        win = [ph.sb(f"win{i}", [128, 3, 8, 128], BF16) for i in range(2)]
        wst = [ph.sb(f"wst{i}", [128, 3, 8, 128], F32) for i in range(2)]
        gmix = ph.sb("gmix", [128, D], F32)
        cw4 = ph.sb("cw4", [4, D], F32)
        cwb = ph.sb("cwb", [128, 8, 4], F32)
        xt = [ph.sb(f"xt{i}", [128, D], F32) for i in range(2)]
        junk = ph.sb("junk", [128, D], BF16)
        sst = [ph.sb(f"sst{i}", [128, 4], F32) for i in range(2)]
        xn = [ph.sb(f"xn{i}", [128, D], BF16) for i in range(2)]
        u = [ph.sb(f"u{i}", [128, S + 2], F32) for i in range(2)]
        bsb = [ph.sb(f"bsb{i}", [128, S], F32) for i in range(2)]
        csb = [ph.sb(f"csb{i}", [128, 512], F32) for i in range(2)]
        yc = [ph.sb(f"yc{i}", [128, S], F32) for i in range(2)]
        hn = [ph.sb(f"hn{i}", [128, D], F32) for i in range(2)]
        ptb = [ph.ps(f"ptb{i}", [128, 8, 128], BF16) for i in range(2)]
        PP = [ph.ps(f"pp{i}", [128, 1024], F32) for i in range(3)]

        ph.op("sp", lambda e: e.dma_start(out=gmix[:], in_=norm_mix[0:1, :].partition_broadcast(128)), w=["gmix"], dma=True)
        ph.op("sp", lambda e: e.dma_start(out=cw4[:], in_=conv_wb), w=["cw4"], dma=True)
        ph.op("pool", lambda e: e.dma_start(out=wout[:], in_=conv_w_out.rearrange("(k p) n -> p k n", p=128)), w=["wout"], dma=True)
        cps = PP[0][:, 0:32].rearrange("p (j w) -> p j w", w=4)
        for j in range(8):
            ph.op("pe", lambda e, j=j: e.transpose(out=cps[:, j, :], in_=cw4[:, j * 128:(j + 1) * 128], identity=identf[0:4, 0:4]),
                  r=["cw4"], w=[("pp", 0, 0)])
        ph.op("dve", lambda e: e.tensor_copy(out=cwb[:], in_=cps), r=[("pp", 0, 0)], w=["cwb"])
        for i in range(2):
            ph.op("dve", lambda e, i=i: e.memset(u[i][:, 0:1], 0.0), w=[("u", i)])
            ph.op("dve", lambda e, i=i: e.memset(u[i][:, S + 1:S + 2], 0.0), w=[("u", i)])
        def A1a(i):
            b = i % 2
            r0 = s * S + i * 128
            ph.op("sp", lambda e, b=b, r0=r0: e.dma_start(out=xt[b][:], in_=x[r0:r0 + 128, :]), w=[("xt", b)], dma=True)
            rmsnorm(ph, xt[b][:], ("xt", b), gmix, "gmix", xn[b][:], ("xn", b), junk, sst[b], b)

        def A1b(i):
            b = i % 2
            transpose8(ph, xn[b], ("xn", b), ptb[b], ("ptb", b), xnT[:, :, i * 128:(i + 1) * 128], ("xnT", i), "act")

        for step in range(NT + 1):
            if step < NT:
                A1a(step)
            if step >= 1:
                A1b(step - 1)
        xnT_keys = [("xnT", i) for i in range(NT)]
        w_in_v = conv_w_in.rearrange("(k p) (t n) -> p t k n", p=128, t=3)
        def load_w(j):
            jb = j % 2
            ph.op("sp", lambda e, j=j, jb=jb: e.dma_start(out=wst[jb][:], in_=w_in_v[:, :, :, j * 128:(j + 1) * 128]),
                  w=[("wst", jb)], dma=True)
            ph.op("pool", lambda e, jb=jb: e.tensor_copy(out=win[jb][:, 0:2], in_=wst[jb][:, 0:2]), r=[("wst", jb)], w=[("win", jb)])
            ph.op("act", lambda e, jb=jb: e.activation(out=win[jb][:, 2], in_=wst[jb][:, 2], func=AF.Copy), r=[("wst", jb)], w=[("win", jb, 2)])

        for j in range(8):
            jb = j % 2
            if j == 0:
                load_w(0)
            for tb in range(4):
                q = (j * 4 + tb) % 2
                bps = PP[q][:, 0:512]
                cps_ = PP[q][:, 512:1024]
                vps = PP[2][:, q * 512:(q + 1) * 512]
                tks = [("xnT", i) for i in range(tb * 4, tb * 4 + 4)]
                for t, (dst, key) in enumerate([(bps, ("pp", q, 0)), (cps_, ("pp", q, 1)), (vps, ("pp", 2, q))]):
                    for k in range(8):
                        ph.op("pe", lambda e, dst=dst, t=t, k=k, jb=jb, tb=tb: e.matmul(
                            dst, lhsT=win[jb][:, t, k, :], rhs=xnT[:, k, tb * 512:(tb + 1) * 512], start=(k == 0), stop=(k == 7)),
                            r=[("win", jb), ("win", jb, 2)] + tks, w=[key])
                ph.op("act", lambda e, q=q, cps_=cps_: e.activation(out=csb[q][:], in_=cps_, func=AF.Copy), r=[("pp", q, 1)], w=[("csb", q)])
                ph.op("act", lambda e, jb=jb, tb=tb, bps=bps: e.activation(out=bsb[jb][:, tb * 512:(tb + 1) * 512], in_=bps, func=AF.Copy),
                      r=[("pp", q, 0)], w=[("bsb", jb)])
                ph.op("dve", lambda e, jb=jb, tb=tb, q=q, vps=vps: e.tensor_tensor(
                    out=u[jb][:, 1 + tb * 512:1 + (tb + 1) * 512], in0=csb[q][:], in1=vps, op=ALU.mult),
                    r=[("csb", q), ("pp", 2, q)], w=[("u", jb)])
            if j + 1 < 8:
                load_w(j + 1)
            ph.op("act", lambda e, j=j, jb=jb: e.activation(out=yc[jb][:], in_=u[jb][:, 1:S + 1], func=AF.Identity,
                                                            bias=cwb[:, j, 3:4], scale=cwb[:, j, 1:2]),
                  r=[("u", jb), "cwb"], w=[("yc", jb)])
            ph.op("dve", lambda e, j=j, jb=jb: e.scalar_tensor_tensor(out=yc[jb][:], in0=u[jb][:, 0:S], scalar=cwb[:, j, 0:1], in1=yc[jb][:],
                                                                     op0=ALU.mult, op1=ALU.add),
                  r=[("u", jb), "cwb", ("yc", jb)], w=[("yc", jb)])
            ph.op("dve", lambda e, j=j, jb=jb: e.scalar_tensor_tensor(out=yc[jb][:], in0=u[jb][:, 2:S + 2], scalar=cwb[:, j, 2:3], in1=yc[jb][:],
                                                                      op0=ALU.mult, op1=ALU.add),
                  r=[("u", jb), "cwb", ("yc", jb)], w=[("yc", jb)])
            ph.op("pool", lambda e, j=j, jb=jb: e.tensor_tensor(out=zT[:, j, :], in0=bsb[jb][:], in1=yc[jb][:], op=ALU.mult),
                  r=[("bsb", jb), ("yc", jb)], w=[("zT", j)])
        zkeys = [("zT", j) for j in range(8)]
        def loads_A3(i):
            b = i % 2
            r0 = s * S + i * 128
            ph.op("sp", lambda e, b=b, r0=r0: e.dma_start(out=xt[b][:], in_=x[r0:r0 + 128, :]), w=[("xt", b)], dma=True)

        loads_A3(0)
        for i in range(NT):
            b = i % 2
            r0 = s * S + i * 128
            if i + 1 < NT:
                loads_A3(i + 1)
            for nh in range(2):
                for k in range(8):
                    ph.op("pe", lambda e, b=b, nh=nh, k=k, i=i: e.matmul(
                        PP[b][:, nh * 512:(nh + 1) * 512], lhsT=zT[:, k, i * 128:(i + 1) * 128], rhs=wout[:, k, nh * 512:(nh + 1) * 512],
                        start=(k == 0), stop=(k == 7)), r=zkeys + ["wout"], w=[("pp", b, nh)])
                ph.op("dve", lambda e, b=b, nh=nh: e.tensor_tensor(out=hn[b][:, nh * 512:(nh + 1) * 512], in0=xt[b][:, nh * 512:(nh + 1) * 512],
                                                                  in1=PP[b][:, nh * 512:(nh + 1) * 512], op=ALU.add),
                      r=[("xt", b), ("pp", b, nh)], w=[("hn", b, nh)])
            ph.op("sp", lambda e, b=b, r0=r0: e.dma_start(out=H[r0:r0 + 128, :], in_=hn[b][:]), r=[("hn", b, 0), ("hn", b, 1)], w=[("H", r0)], dma=True)
        ph.emit()

    def phase_R(l):
        ph = Phase(nc, f"R{l}")
        NB = 6
        gffn = ph.sb("gffn", [128, D], F32)
        wr = ph.sb("wr", [128, 8, NE], BF16)
        hn = [ph.sb(f"hn{i}", [128, D], F32) for i in range(NB)]
        junk = ph.sb("junk", [128, D], BF16)
        sst = [ph.sb(f"sst{i}", [128, 4], F32) for i in range(NB)]
        xn = [ph.sb(f"xn{i}", [128, D], BF16) for i in range(NB)]
        xT = [ph.sb(f"xT{i}", [128, 8, 128], BF16) for i in range(NB)]
        sm = [ph.sb(f"sm{i}", [128, 4], F32) for i in range(NB)]
        ex = [ph.sb(f"ex{i}", [128, NE], F32) for i in range(NB)]
        ptb = [ph.ps(f"ptb{i}", [128, 8, 128], BF16) for i in range(2)]
        PL = [ph.ps(f"pl{i}", [128, NE], F32) for i in range(2)]
        ph.op("sp", lambda e: e.dma_start(out=gffn[:], in_=norm_ffn[l:l + 1, :].partition_broadcast(128)), w=["gffn"], dma=True)
        ph.op("pool", lambda e: e.dma_start(out=wr[:], in_=router_w[l].rearrange("(k p) n -> p k n", p=128)), w=["wr"], dma=True)
        def loads_R(ti):
            b = ti % NB
            r0 = ti * 128
            ph.op("sp", lambda e, b=b, r0=r0: e.dma_start(out=hn[b][:], in_=H[r0:r0 + 128, :]), w=[("hn", b)], dma=True)

        def R_s1(ti):
            b = ti % NB
            pb2 = ti % 2
            s, i = divmod(ti, NT)
            r0 = ti * 128
            rmsnorm(ph, hn[b][:], ("hn", b), gffn, "gffn", xn[b][:], ("xn", b), junk, sst[b], b)

        def R_s1b(ti):
            b = ti % NB
            pb2 = ti % 2
            r0 = ti * 128
            ph.op("sp", lambda e, b=b, r0=r0: e.dma_start(out=XN[r0:r0 + 128, :], in_=xn[b][:]), r=[("xn", b)], w=[("XN", r0)], dma=True)
            transpose8(ph, xn[b], ("xn", b), ptb[pb2], ("ptb", pb2), xT[b][:], ("xT", b), "act")

        def R_s2(ti):
            b = ti % NB
            pb2 = ti % 2
            s, i = divmod(ti, NT)
            r0 = ti * 128
            for k in range(8):
                ph.op("pe", lambda e, b=b, k=k, pb2=pb2: e.matmul(PL[pb2][:], lhsT=xT[b][:, k, :], rhs=wr[:, k, :], start=(k == 0), stop=(k == 7)),
                      r=[("xT", b), "wr"], w=[("pl", pb2)])
            ph.op("dve", lambda e, b=b, pb2=pb2: e.reduce_max(out=sm[b][:, 0:1], in_=PL[pb2][:], axis=AX.X), r=[("pl", pb2)], w=[("mx", b)])
            ph.op("dve", lambda e, b=b: e.tensor_scalar(out=sm[b][:, 1:2], in0=sm[b][:, 0:1], scalar1=-1.0, scalar2=None, op0=ALU.mult),
                  r=[("mx", b)], w=[("nmx", b)])
            ph.op("act", lambda e, b=b, pb2=pb2: e.activation(out=ex[b][:], in_=PL[pb2][:], func=AF.Exp, bias=sm[b][:, 1:2], scale=1.0, accum_out=sm[b][:, 2:3]),
                  r=[("pl", pb2), ("nmx", b)], w=[("ex", b), ("sum", b)])
            ph.op("dve", lambda e, b=b: e.reciprocal(out=sm[b][:, 3:4], in_=sm[b][:, 2:3]), r=[("sum", b)], w=[("rsum", b)])
            ph.op("dve", lambda e, b=b, s=s, i=i: e.tensor_scalar(out=aff_tok[:, i, s * 16:(s + 1) * 16], in0=ex[b][:], scalar1=sm[b][:, 3:4],
                                                                 scalar2=None, op0=ALU.mult),
                  r=[("ex", b), ("rsum", b)], w=[("aff", ti)])

        for step in range(NTL + 4):
            if step < NTL:
                loads_R(step)
            if 0 <= step - 2 < NTL:
                R_s1(step - 2)
            if 0 <= step - 3 < NTL:
                R_s1b(step - 3)
            if 0 <= step - 4 < NTL:
                R_s2(step - 4)
        ph.emit()

    def phase_P(l, final):
        ph = Phase(nc, f"P{l}")
        NB = 6
        wpg = ph.sb("wpg", [128, 8, D], BF16)
        wpp = ph.sb("wpp", [128, 2, D], BF16)
        gple = ph.sb("gple", [128, D], F32)
        gnx = ph.sb("gnx", [128, D], F32)
        hn = [ph.sb(f"hn{i}", [128, D], F32) for i in range(NB)]
        pt = [ph.sb(f"pt{i}", [128, PLE], F32) for i in range(NB)]
        pb = [ph.sb(f"pb{i}", [128, PLE], BF16) for i in range(NB)]
        pT = [ph.sb(f"pT{i}", [128, 2, 128], BF16) for i in range(NB)]
        junk = ph.sb("junk", [128, D], BF16)
        sst = [ph.sb(f"sst{i}", [128, 4], F32) for i in range(2 * NB)]
        xn = [ph.sb(f"xn{i}", [128, D], BF16) for i in range(NB)]
        xT = [ph.sb(f"xT{i}", [128, 8, 128], BF16) for i in range(NB)]
        sgm = [ph.sb(f"sgm{i}", [128, D], F32) for i in range(NB)]
        h2 = [ph.sb(f"h2{i}", [128, D], F32) for i in range(NB)]
        xo = [ph.sb(f"xo{i}", [128, D], F32 if final else BF16) for i in range(NB)]
        ptb = [ph.ps(f"ptb{i}", [128, 8, 128], BF16) for i in range(2)]
        ptp = ph.ps("ptp", [128, 8, 128], BF16)
        PG = ph.ps("pg", [128, 1024], F32)
        PQ = ph.ps("pq", [128, 1024], F32)
        ph.op("pool", lambda e: e.dma_start(out=wpg[:], in_=ple_gate[l].rearrange("(k p) n -> p k n", p=128)), w=["wpg"], dma=True)
        ph.op("pool", lambda e: e.dma_start(out=wpp[:], in_=ple_proj[l].rearrange("(k p) n -> p k n", p=128)), w=["wpp"], dma=True)
        ph.op("sp", lambda e: e.dma_start(out=gple[:], in_=norm_ple[l:l + 1, :].partition_broadcast(128)), w=["gple"], dma=True)
        gsrc = final_norm[0:1, :] if final else norm_mix[l + 1:l + 2, :]
        ph.op("sp", lambda e: e.dma_start(out=gnx[:], in_=gsrc.partition_broadcast(128)), w=["gnx"], dma=True)
        def loads_P(ti):
            b = ti % NB
            r0 = ti * 128
            ph.op("sp", lambda e, b=b, r0=r0: e.dma_start(out=hn[b][:], in_=H[r0:r0 + 128, :]), w=[("hn", b)], dma=True)
            ph.op("sp", lambda e, b=b, r0=r0: e.dma_start(out=pt[b][:], in_=p_in[l, r0:r0 + 128, :]), w=[("pt", b)], dma=True)

        def P_s1(ti):
            b = ti % NB
            pb2 = ti % 2
            r0 = ti * 128
            rmsnorm(ph, hn[b][:], ("hn", b), gple, "gple", xn[b][:], ("xn", b), junk, sst[b], b, "dve")
            ph.op("pool", lambda e, b=b: e.tensor_copy(out=pb[b][:], in_=pt[b][:]), r=[("pt", b)], w=[("pb", b)])

        def P_s1b(ti):
            b = ti % NB
            pb2 = ti % 2
            transpose8(ph, xn[b], ("xn", b), ptb[pb2], ("ptb", pb2), xT[b][:], ("xT", b), "act")
            transpose8(ph, pb[b], ("pb", b), ptp, "ptp", pT[b][:], ("pT", b), "dve", n=2)

        def P_s2(ti):
            b = ti % NB
            pb2 = ti % 2
            r0 = ti * 128
            for nh in range(2):
                for k in range(8):
                    ph.op("pe", lambda e, b=b, nh=nh, k=k: e.matmul(PG[:, nh * 512:(nh + 1) * 512], lhsT=xT[b][:, k, :],
                                                                   rhs=wpg[:, k, nh * 512:(nh + 1) * 512], start=(k == 0), stop=(k == 7)),
                          r=[("xT", b), "wpg"], w=[("pg", nh)])
                for k in range(2):
                    ph.op("pe", lambda e, b=b, nh=nh, k=k: e.matmul(PQ[:, nh * 512:(nh + 1) * 512], lhsT=pT[b][:, k, :],
                                                                   rhs=wpp[:, k, nh * 512:(nh + 1) * 512], start=(k == 0), stop=(k == 1)),
                          r=[("pT", b), "wpp"], w=[("pq", nh)])
                sl = slice(nh * 512, (nh + 1) * 512)
                ph.op("act", lambda e, b=b, sl=sl: e.activation(out=sgm[b][:, sl], in_=PG[:, sl], func=AF.Sigmoid), r=[("pg", nh)], w=[("sgm", b, nh)])
                ph.op("dve", lambda e, b=b, sl=sl: e.tensor_tensor(out=sgm[b][:, sl], in0=sgm[b][:, sl], in1=PQ[:, sl], op=ALU.mult),
                      r=[("sgm", b, nh), ("pq", nh)], w=[("sgm", b, nh)])
                ph.op("pool", lambda e, b=b, sl=sl: e.tensor_tensor(out=h2[b][:, sl], in0=sgm[b][:, sl], in1=hn[b][:, sl], op=ALU.add),
                      r=[("sgm", b, nh), ("hn", b)], w=[("h2", b, nh)])
            hk = [("h2", b, 0), ("h2", b, 1)]
            if not final:
                ph.op("sp", lambda e, b=b, r0=r0: e.dma_start(out=H[r0:r0 + 128, :], in_=h2[b][:]), r=hk, w=[("H", r0)], dma=True)

        def P_s3(ti):
            b = ti % NB
            pb2 = ti % 2
            r0 = ti * 128
            hk = [("h2", b, 0), ("h2", b, 1)]
            rmsnorm(ph, h2[b][:], hk, gnx, "gnx", xo[b][:], ("xo", b), junk, sst[NB + b], ("n2", b))
            dst = out if final else XN
            ph.op("sp", lambda e, b=b, r0=r0, dst=dst: e.dma_start(out=dst[r0:r0 + 128, :], in_=xo[b][:]), r=[("xo", b)], w=[("O", r0)], dma=True)

        for step in range(NTL + 5):
            if step < NTL:
                loads_P(step)
            if 0 <= step - 2 < NTL:
                P_s1(step - 2)
            if 0 <= step - 3 < NTL:
                P_s1b(step - 3)
            if 0 <= step - 4 < NTL:
                P_s2(step - 4)
            if 0 <= step - 5 < NTL:
                P_s3(step - 5)
        ph.emit()

    def phase_B(l):
        ph = Phase(nc, f"B{l}")
        NTOK = nseq * CAP
        ntile = nseq * 2
        TB = min(512, NTOK)
        nblk = NTOK // TB
        work = ph.sb("work", [64, S], F32)
        gates = ph.sb("gates", [64, CAP], F32)
        idxu = ph.sb("idxu", [64, CAP], U32)
        idxf = ph.sb("idxf", [64, CAP], F32)
        soff = ph.sb("soff", [64, 1], F32)
        gT = ph.sb("gT", [128, 2, 64], F32)
        iTi = ph.sb("iTi", [128, 2, 64], I32)
        ring = [ph.sb(f"ring{i}", [128, 8192], BF16) for i in range(4)]
        xg = [ph.sb(f"xg{i}", [128, ntile, D], BF16) for i in range(2)]
        xgT = ph.sb("xgT", [128, 8, NTOK], BF16)
        hT = ph.sb("hT", [128, 16, NTOK], BF16)
        sg = [ph.sb(f"sg{i}", [128, 512], F32) for i in range(2)]
        yt = [ph.sb(f"yt{i}", [128, D], F32) for i in range(2)]
        ptb = [ph.ps(f"ptb{i}", [128, 8, 128], BF16) for i in range(2)]
        PGU = [ph.ps(f"pgu{i}", [128, 1024], F32) for i in range(2)]
        PD = ph.ps("pd", [128, 1024], F32)

        ph.op("sp", lambda e: e.dma_start(out=soff[:], in_=c_seqoff), w=["soff"], dma=True)
        for g in range(4):
            for ii in range(4):
                i = g * 4 + ii
                ph.op("pe", lambda e, g=g, ii=ii, i=i: e.transpose(out=PGU[g % 2][0:64, ii * 128:(ii + 1) * 128], in_=aff_tok[:, i, :], identity=identf[:]),
                      r=["aff"], w=[("pgu", g % 2, 0)])
            ph.op("dve", lambda e, g=g: e.tensor_copy(out=work[:, g * 512:(g + 1) * 512], in_=PGU[g % 2][0:64, 0:512]),
                  r=[("pgu", g % 2, 0)], w=["work"])
        for r in range(CAP // 8):
            sl = slice(r * 8, (r + 1) * 8)
            ph.op("dve", lambda e, sl=sl: e.max(out=gates[:, sl], in_=work[:]), r=["work"], w=[("gt", r)])
            ph.op("dve", lambda e, sl=sl: e.max_index(out=idxu[:, sl], in_max=gates[:, sl], in_values=work[:]), r=["work", ("gt", r)], w=[("ix", r)])
            ph.op("dve", lambda e, sl=sl: e.match_replace(out=work[:], in_to_replace=gates[:, sl], in_values=work[:], imm_value=-1.0),
                  r=["work", ("gt", r)], w=["work"])
        gkeys = [("gt", r) for r in range(CAP // 8)]
        ikeys = [("ix", r) for r in range(CAP // 8)]
        ph.op("dve", lambda e: e.tensor_copy(out=idxf[:], in_=idxu[:]), r=ikeys, w=["idxf"])
        ph.op("dve", lambda e: e.tensor_scalar(out=idxf[:], in0=idxf[:], scalar1=soff[:, 0:1], scalar2=None, op0=ALU.add), r=["idxf", "soff"], w=["idxf"])
        tp = PGU[0][:, 0:256].rearrange("p (a c) -> p a c", c=64)
        for hf in range(2):
            ph.op("pe", lambda e, hf=hf: e.transpose(out=tp[:, hf, :], in_=gates[:, hf * 128:(hf + 1) * 128], identity=identf[0:64, 0:64]),
                  r=gkeys, w=[("pgu", 0, 0)])
            ph.op("pe", lambda e, hf=hf: e.transpose(out=tp[:, 2 + hf, :], in_=idxf[:, hf * 128:(hf + 1) * 128], identity=identf[0:64, 0:64]),
                  r=["idxf"], w=[("pgu", 0, 0)])
        ph.op("dve", lambda e: e.tensor_copy(out=gT[:], in_=tp[:, 0:2, :]), r=[("pgu", 0, 0)], w=["gT"])
        ph.op("act", lambda e: e.activation(out=iTi[:], in_=tp[:, 2:4, :], func=AF.Copy), r=[("pgu", 0, 0)], w=["iTi"])

        wg_v = w_gate[l].rearrange("e (k p) f -> e p k f", p=128)
        wu_v = w_up[l].rearrange("e (k p) f -> e p k f", p=128)
        wd_v = w_down[l].rearrange("e (k p) n -> e p k n", p=128)

        def load_slab(g):
            e_, j = divmod(g, 6)
            if e_ >= NE:
                return
            slot = g % 4
            if j < 4:
                dstg = ring[slot][:, 0:4096].rearrange("p (k n) -> p k n", n=512)
                dstu = ring[slot][:, 4096:8192].rearrange("p (k n) -> p k n", n=512)
                ph.op("pool", lambda e: e.dma_start(out=dstg, in_=wg_v[e_, :, :, j * 512:(j + 1) * 512]), w=[("ring", slot, 0)], dma=True)
                ph.op("pool", lambda e: e.dma_start(out=dstu, in_=wu_v[e_, :, :, j * 512:(j + 1) * 512]), w=[("ring", slot, 1)], dma=True)
            else:
                dh = j - 4
                dst = ring[slot][:].rearrange("p (k n) -> p k n", n=1024)
                ph.op("pool", lambda e: e.dma_start(out=dst, in_=wd_v[e_, :, dh * 8:(dh + 1) * 8, :]), w=[("ring", slot, 0), ("ring", slot, 1)], dma=True)

        def gather(e_):
            if e_ >= NE:
                return
            for t in range(ntile):
                s, hf = divmod(t, 2)
                col = s * 16 + e_
                ph.op("pool", lambda e, t=t, hf=hf, col=col: e.indirect_dma_start(
                    out=xg[e_ % 2][:, t, :], out_offset=None, in_=XN[:, :],
                    in_offset=bass.IndirectOffsetOnAxis(ap=iTi[:, hf, col:col + 1], axis=0)),
                    r=["iTi"], w=[("xg", e_ % 2, t)], dma=True)

        def transposes(e_):
            if e_ >= NE:
                return
            for t in range(ntile):
                transpose8(ph, xg[e_ % 2][:, t, :], ("xg", e_ % 2, t), ptb[t % 2], ("ptb", t % 2),
                           xgT[:, :, t * 128:(t + 1) * 128], ("xgT", t), "act" if t % 2 == 0 else "dve")

        for g in range(4):
            load_slab(g)
        gather(0)
        transposes(0)
        gather(1)
        prev_sc = {}
        cur_sc = {}
        cnt = 0
        for e_ in range(NE):
            for fg in range(4):
                g = e_ * 6 + fg
                slot = g % 4
                sv = ring[slot][:].rearrange("p (a k n) -> p a k n", a=2, k=8)
                for fc4 in range(4):
                    fc = fg * 4 + fc4
                    for hb in range(nblk):
                        q = cnt % 2
                        cnt += 1
                        tks = [("xgT", t) for t in range(hb * (TB // 128), (hb + 1) * (TB // 128))]
                        for a in range(2):
                            for k in range(8):
                                ph.op("pe", lambda e, q=q, a=a, k=k, sv=sv, fc4=fc4, hb=hb: e.matmul(
                                    PGU[q][:, a * 512:a * 512 + TB], lhsT=sv[:, a, k, fc4 * 128:(fc4 + 1) * 128],
                                    rhs=xgT[:, k, hb * TB:(hb + 1) * TB], start=(k == 0), stop=(k == 7)),
                                    r=[("ring", slot, a)] + tks, w=[("pgu", q, a)])
                        ph.op("act", lambda e, q=q: e.activation(out=sg[q][:, 0:TB], in_=PGU[q][:, 0:TB], func=AF.Silu), r=[("pgu", q, 0)], w=[("sg", q)])
                        ph.op("dve", lambda e, q=q, fc=fc, hb=hb: e.tensor_tensor(out=hT[:, fc, hb * TB:(hb + 1) * TB], in0=sg[q][:, 0:TB],
                                                                                 in1=PGU[q][:, 512:512 + TB], op=ALU.mult),
                              r=[("sg", q), ("pgu", q, 1)], w=[("hT", fc, hb)])
                load_slab(g + 4)
            transposes(e_ + 1)
            gather(e_ + 2)
            d0 = ring[(e_ * 6 + 4) % 4][:].rearrange("p (k n) -> p k n", n=1024)
            d1 = ring[(e_ * 6 + 5) % 4][:].rearrange("p (k n) -> p k n", n=1024)
            dkeys = [("ring", (e_ * 6 + 4) % 4, 0), ("ring", (e_ * 6 + 4) % 4, 1), ("ring", (e_ * 6 + 5) % 4, 0), ("ring", (e_ * 6 + 5) % 4, 1)]
            for t in range(ntile):
                s, hf = divmod(t, 2)
                col = s * 16 + e_
                hb = (t * 128) // TB
                b = t % 2
                for nh in range(2):
                    for fc in range(16):
                        dsl = d0 if fc < 8 else d1
                        ph.op("pe", lambda e, nh=nh, fc=fc, dsl=dsl, t=t: e.matmul(
                            PD[:, nh * 512:(nh + 1) * 512], lhsT=hT[:, fc, t * 128:(t + 1) * 128], rhs=dsl[:, fc % 8, nh * 512:(nh + 1) * 512],
                            start=(fc == 0), stop=(fc == 15)), r=dkeys + [("hT", fc_, hb) for fc_ in range(16)], w=[("pd", nh)])
                    sl = slice(nh * 512, (nh + 1) * 512)
                    if nh == 0:
                        ph.op("act", lambda e, b=b, sl=sl, hf=hf, col=col: e.activation(out=yt[b][:, sl], in_=PD[:, sl], func=AF.Copy, scale=gT[:, hf, col:col + 1]),
                              r=[("pd", nh), "gT"], w=[("yt", b, nh)])
                    else:
                        ph.op("dve", lambda e, b=b, sl=sl, hf=hf, col=col: e.tensor_scalar(out=yt[b][:, sl], in0=PD[:, sl], scalar1=gT[:, hf, col:col + 1],
                                                                                          scalar2=None, op0=ALU.mult),
                              r=[("pd", nh), "gT"], w=[("yt", b, nh)])
                o = ph.op("pool", lambda e, b=b, hf=hf, col=col: e.indirect_dma_start(
                    out=H[:, :], out_offset=bass.IndirectOffsetOnAxis(ap=iTi[:, hf, col:col + 1], axis=0),
                    in_=yt[b][:, :], in_offset=None, compute_op=ALU.add),
                    r=[("yt", b, 0), ("yt", b, 1), "iTi"], w=[], dma=True, after=prev_sc.get(s, []))
                cur_sc.setdefault(s, []).append(o)
            prev_sc = cur_sc
            cur_sc = {}
            load_slab(e_ * 6 + 4 + 4)
            load_slab(e_ * 6 + 5 + 4)
        ph.emit()

    def phase_C(s):
        ph = Phase(nc, f"C{s}")
        xnT = ph.sb("xnT", [128, 8, S], BF16)
        xl = [ph.sb(f"xl{i}", [128, D], BF16) for i in range(2)]
        ws = [ph.sb(f"ws{i}", [128, 4096], BF16) for i in range(4)]
        cs = [ph.sb(f"cs{i}", [128, 2, 512], F32) for i in range(1)]
        qT = ph.sb("qT", [128, 2, S], BF16)
        kT = ph.sb("kT", [128, 2, S], BF16)
        kf = ph.sb("kf", [128, NT, 256], BF16)
        kb = ph.sb("kb", [128, NT, 256], BF16)
        vt = ph.sb("vt", [128, NT, 512], BF16)
        rt = [ph.sb(f"rt{i}", [128, 512], F32) for i in range(4)]
        Sf32 = ph.sb("Sf32", [128, 1024], F32)
        Sb32 = [ph.sb(f"Sb32{i}", [128, 1024], F32) for i in range(2)]
        Sfb = [ph.sb(f"Sfb{i}", [128, 1024], BF16) for i in range(2)]
        Sbst = [ph.sb(f"Sbst{i}", [128, 1024], BF16) for i in range(4)]
        Sbin = [ph.sb(f"Sbin{i}", [128, 1024], BF16) for i in range(3)]
        ldb = ph.sb("ldb", [128, 8], F32)
        Mt = ph.sb("Mt", [128, 4, 128], F32)
        tmpM = [ph.sb(f"tmpM{i}", [128, 128], F32) for i in range(2)]
        qd = ph.sb("qd", [128, 4, 2, 2, 128], F32)
        kd = ph.sb("kd", [128, 4, 2], F32)
        cdc = ph.sb("cdc", [128, 4, 2], F32)
        dm = ph.sb("dm", [128, 4, 128], F32)
        rw = ph.sb("rw", [128, 2, 128], F32)
        cl = ph.sb("cl", [128, 3], F32)
        Pm = [ph.sb(f"Pm{i}", [128, 128], BF16) for i in range(2)]
        qfb = [ph.sb(f"qfb{i}", [128, 2, 2, 128], BF16) for i in range(2)]
        sgl = [ph.sb(f"sgl{i}", [128, 512], F32) for i in range(2)]
        on = [ph.sb(f"on{i}", [128, 512], F32) for i in range(2)]
        go = [ph.sb(f"go{i}", [128, 512], BF16) for i in range(2)]
        goT = [ph.sb(f"goT{i}", [128, 4, 128], BF16) for i in range(2)]
        mo = [ph.sb(f"mo{i}", [128, D], F32) for i in range(2)]
        bst = [ph.sb(f"bst{i}", [128, 6], F32) for i in range(2)]
        mv = [ph.sb(f"mv{i}", [128, 8], F32) for i in range(2)]
        ptb = ph.ps("ptb", [128, 8, 128], BF16)
        PG = ph.ps("pg", [128, 512], F32)
        PA = ph.ps("pa", [128, 1024], F32)
        PB = ph.ps("pb", [128, 1024], F32)
        PO = ph.ps("po", [128, 512], F32)
        PSC = ph.ps("psc", [128, 128], F32)
        ptk = ptb[:].rearrange("p a b -> p (a b)").rearrange("p (a b) -> p a b", b=256)

        ph.op("sp", lambda e: e.dma_start(out=ldb[:], in_=ret_ld.partition_broadcast(128)), w=["ldb"], dma=True)
        ph.op("sp", lambda e: e.dma_start(out=dm[:], in_=c_dmat.rearrange("a j i -> j a i")), w=["dm"], dma=True)
        ph.op("sp", lambda e: e.dma_start(out=rw[:], in_=c_rows.rearrange("a p i -> p a i")), w=["rw"], dma=True)
        ph.op("sp", lambda e: e.dma_start(out=cl[:], in_=c_cols), w=["cl"], dma=True)
        for h in range(4):
            ph.op("act", lambda e, h=h: e.activation(out=tmpM[0][:], in_=dm[:, 0, :], func=AF.Exp, scale=ldb[:, h:h + 1]), r=["dm", "ldb"], w=["tmA"])
            ph.op("dve", lambda e, h=h: e.tensor_tensor(out=tmpM[0][:], in0=tmpM[0][:], in1=dm[:, 1, :], op=ALU.mult), r=["tmA", "dm"], w=["tmA"])
            ph.op("act", lambda e, h=h: e.activation(out=tmpM[1][:], in_=dm[:, 2, :], func=AF.Exp, scale=ldb[:, 4 + h:5 + h]), r=["dm", "ldb"], w=["tmB"])
            ph.op("dve", lambda e, h=h: e.tensor_tensor(out=tmpM[1][:], in0=tmpM[1][:], in1=dm[:, 3, :], op=ALU.mult), r=["tmB", "dm"], w=["tmB"])
            ph.op("dve", lambda e, h=h: e.tensor_tensor(out=Mt[:, h, :], in0=tmpM[0][:], in1=tmpM[1][:], op=ALU.add), r=["tmA", "tmB"], w=[("Mt", h)])
            for d_ in range(2):
                lc = ldb[:, 4 * d_ + h:4 * d_ + h + 1]
                for dc in range(2):
                    ph.op("act", lambda e, h=h, d_=d_, lc=lc, dc=dc: e.activation(out=qd[:, h, d_, dc, :], in_=rw[:, d_, :], func=AF.Exp, scale=lc), r=["rw", "ldb"], w=[("qd", h)])
                ph.op("act", lambda e, h=h, d_=d_, lc=lc: e.activation(out=kd[:, h, d_:d_ + 1], in_=cl[:, d_:d_ + 1], func=AF.Exp, scale=lc), r=["cl", "ldb"], w=[("kd", h)])
                ph.op("act", lambda e, h=h, d_=d_, lc=lc: e.activation(out=cdc[:, h, d_:d_ + 1], in_=cl[:, 2:3], func=AF.Exp, scale=lc), r=["cl", "ldb"], w=[("cdc", h)])
        for i in range(NT):
            b = i % 2
            r0 = s * S + i * 128
            ph.op("sp", lambda e, b=b, r0=r0: e.dma_start(out=xl[b][:], in_=XN[r0:r0 + 128, :]), w=[("xl", b)], dma=True)
            transpose8(ph, xl[b], ("xl", b), ptb, "ptb", xnT[:, :, i * 128:(i + 1) * 128], ("xnT", i), "act" if b == 0 else "dve")
        allx = [("xnT", i) for i in range(NT)]
        if CCUT == "c1":
            ph.emit()
            return
        w_in_v = ret_w_in.rearrange("(k p) n -> p k n", p=128)
        w_out_v = ret_w_out.rearrange("(k p) n -> p k n", p=128)
        ws0v = ws[0][:].rearrange("p (k n) -> p k n", n=512)
        ws1v = ws[1][:].rearrange("p (k n) -> p k n", n=512)
        ws2v = ws[2][:].rearrange("p (k n) -> p k n", n=512)
        ws3v = ws[3][:].rearrange("p (k n) -> p k n", n=1024)
        prev_acc = {}
        cnt = 0
        for h in range(4):
            ph.op("pool", lambda e, h=h: e.dma_start(out=ws0v[:, :, 0:256], in_=w_in_v[:, :, h * 256:(h + 1) * 256]), w=[("ws0", 0)], dma=True)
            ph.op("pool", lambda e, h=h: e.dma_start(out=ws0v[:, :, 256:512], in_=w_in_v[:, :, 1024 + h * 256:1024 + (h + 1) * 256]), w=[("ws0", 1)], dma=True)
            ph.op("pool", lambda e, h=h: e.dma_start(out=ws1v, in_=w_in_v[:, :, 2048 + h * 512:2048 + (h + 1) * 512]), w=["ws1"], dma=True)
            ph.op("pool", lambda e, h=h: e.dma_start(out=ws2v, in_=w_in_v[:, :, 4096 + h * 512:4096 + (h + 1) * 512]), w=["ws2"], dma=True)
            ph.op("pool", lambda e, h=h: e.dma_start(out=ws3v, in_=w_out_v[:, h * 4:(h + 1) * 4, :]), w=["ws3"], dma=True)
            for tb in range(4):
                cb_ = 0
                ph.op("sp", lambda e, cb_=cb_, tb=tb: e.dma_start(out=cs[cb_][:], in_=c_cs[:, :, tb * 512:(tb + 1) * 512]), w=[("cs", cb_)], dma=True)
                tks = [("xnT", i) for i in range(tb * 4, tb * 4 + 4)]
                for which in range(2):
                    PX, pk = (PA, "pa") if cnt % 2 == 0 else (PB, "pb")
                    cnt += 1
                    off = which * 256
                    for dc in range(2):
                        for k in range(8):
                            ph.op("pe", lambda e, PX=PX, dc=dc, k=k, off=off, tb=tb: e.matmul(
                                PX[:, dc * 512:(dc + 1) * 512], lhsT=ws0v[:, k, off + dc * 128:off + (dc + 1) * 128],
                                rhs=xnT[:, k, tb * 512:(tb + 1) * 512], start=(k == 0), stop=(k == 7)),
                                r=[("ws0", which)] + tks, w=[(pk, dc)])
                    sc = 1.0 if which == 0 else 1.0 / 16.0
                    combos = [(0, 0), (1, 1), (0, 1), (1, 0)]
                    for ri, (xi, ci) in enumerate(combos):
                        ph.op("dve", lambda e, PX=PX, ri=ri, xi=xi, ci=ci, sc=sc, cb_=cb_: e.scalar_tensor_tensor(
                            out=rt[ri][:], in0=PX[:, xi * 512:(xi + 1) * 512], scalar=sc, in1=cs[cb_][:, ci, :], op0=ALU.mult, op1=ALU.mult),
                            r=[(pk, xi), ("cs", cb_)], w=[("rt", ri)])
                    dstT, dk = (qT, "qT") if which == 0 else (kT, "kT")
                    ph.op("pool", lambda e, dstT=dstT, tb=tb: e.tensor_tensor(out=dstT[:, 0, tb * 512:(tb + 1) * 512], in0=rt[0][:], in1=rt[1][:], op=ALU.subtract),
                          r=[("rt", 0), ("rt", 1)], w=[(dk, tb, 0)])
                    ph.op("pool", lambda e, dstT=dstT, tb=tb: e.tensor_tensor(out=dstT[:, 1, tb * 512:(tb + 1) * 512], in0=rt[2][:], in1=rt[3][:], op=ALU.add),
                          r=[("rt", 2), ("rt", 3)], w=[(dk, tb, 1)])
            if CCUT == "c2":
                break
            for i in range(NT):
                PV, pk = (PG, "pg") if i % 2 == 0 else (PO, "po")
                for k in range(8):
                    ph.op("pe", lambda e, PV=PV, i=i, k=k: e.matmul(PV[:], lhsT=xnT[:, k, i * 128:(i + 1) * 128], rhs=ws1v[:, k, :], start=(k == 0), stop=(k == 7)),
                          r=[("xnT", i), "ws1"], w=[pk])
                ph.op("act", lambda e, PV=PV, i=i: e.activation(out=vt[:, i, :], in_=PV[:], func=AF.Copy), r=[pk], w=[("vt", i)])
            if CCUT == "c3":
                break
            for g4 in range(4):
                for ii in range(4):
                    i = g4 * 4 + ii
                    for dc in range(2):
                        ph.op("pe", lambda e, ii=ii, dc=dc, i=i: e.transpose(out=ptk[:, ii, dc * 128:(dc + 1) * 128], in_=kT[:, dc, i * 128:(i + 1) * 128], identity=identb[:]),
                              r=[("kT", g4, dc)], w=["ptb"])
                ph.op("act", lambda e, g4=g4, h=h: e.activation(out=kf[:, g4 * 4:(g4 + 1) * 4, :], in_=ptk, func=AF.Copy, scale=kd[:, h, 0:1]),
                      r=["ptb", ("kd", h)], w=[("kf", g4)])
                ph.op("dve", lambda e, g4=g4, h=h: e.tensor_scalar(out=kb[:, g4 * 4:(g4 + 1) * 4, :], in0=ptk, scalar1=kd[:, h, 1:2], scalar2=None, op0=ALU.mult),
                      r=["ptb", ("kd", h), ("kf", g4)], w=[("kb", g4)])
            if CCUT == "proj":
                break
            ph.op("dve", lambda e: e.memset(Sb32[1][:], 0.0), w=[("Sb32", 1, 0), ("Sb32", 1, 1)])
            for c in range(NT - 1, 0, -1):
                PX, pk = (PA, "pa") if c % 2 == 0 else (PB, "pb")
                src_, dst_ = Sb32[c % 2], Sb32[(c + 1) % 2]
                for dc in range(2):
                    ph.op("pe", lambda e, PX=PX, dc=dc, c=c: e.matmul(PX[:, dc * 512:(dc + 1) * 512], lhsT=kb[:, c, dc * 128:(dc + 1) * 128], rhs=vt[:, c, :], start=True, stop=True),
                          r=[("kb", c // 4), ("vt", c)], w=[(pk, dc)])
                for dc in range(2):
                    ph.op("dve", lambda e, PX=PX, h=h, dc=dc, src_=src_, dst_=dst_: e.scalar_tensor_tensor(
                        out=dst_[:, dc * 512:(dc + 1) * 512], in0=src_[:, dc * 512:(dc + 1) * 512],
                        scalar=cdc[:, h, 1:2], in1=PX[:, dc * 512:(dc + 1) * 512], op0=ALU.mult, op1=ALU.add),
                        r=[("Sb32", c % 2, dc), (pk, dc), ("cdc", h)], w=[("Sb32", (c + 1) % 2, dc)])
                ph.op("act", lambda e, c=c, dst_=dst_: e.activation(out=Sbst[c % 4][:], in_=dst_[:], func=AF.Copy),
                      r=[("Sb32", (c + 1) % 2, 0), ("Sb32", (c + 1) % 2, 1)], w=[("Sbst", c % 4)])
                ph.op("sp", lambda e, c=c: e.dma_start(out=SBD[c - 1], in_=Sbst[c % 4][:]), r=[("Sbst", c % 4)], w=[("SBD", c - 1)], dma=True)
            if CCUT == "bwd":
                break
            ph.op("dve", lambda e: e.memset(Sf32[:], 0.0), w=["Sf32"])

            def prefetch(c):
                if c < NT - 1:
                    ph.op("sp", lambda e, c=c: e.dma_start(out=Sbin[c % 3][:], in_=SBD[c]), r=[("SBD", c)], w=[("Sbin", c % 3)], dma=True)

            def tail(c):
                b = c % 2
                r0 = s * S + c * 128
                for vc in range(4):
                    ph.op("pe", lambda e, b=b, vc=vc: e.transpose(out=ptb[:, vc, :], in_=go[b][:, vc * 128:(vc + 1) * 128], identity=identb[:]),
                          r=[("go", b)], w=["ptb"])
                ph.op("act", lambda e, b=b: e.activation(out=goT[b][:], in_=ptb[:, 0:4, :], func=AF.Copy), r=["ptb"], w=[("goT", b)])
                for nh in range(2):
                    for vc in range(4):
                        ph.op("pe", lambda e, b=b, nh=nh, vc=vc: e.matmul(PB[:, nh * 512:(nh + 1) * 512], lhsT=goT[b][:, vc, :], rhs=ws3v[:, vc, nh * 512:(nh + 1) * 512],
                                                                         start=(vc == 0), stop=(vc == 3)),
                              r=[("goT", b), "ws3"], w=[("pb", nh)])
                ph.op("act", lambda e, b=b: e.activation(out=mo[b][:, 0:512], in_=PB[:, 0:512], func=AF.Copy), r=[("pb", 0)], w=[("mo", b, 0)])
                ph.op("act", lambda e, b=b: e.activation(out=mo[b][:, 512:1024], in_=PB[:, 512:1024], func=AF.Copy), r=[("pb", 1)], w=[("mo", b, 1)])
                o = ph.op("pool", lambda e, b=b, r0=r0: e.dma_start(out=H[r0:r0 + 128, :], in_=mo[b][:], accum_op=ALU.add),
                          r=[("mo", b, 0), ("mo", b, 1)], w=[], dma=True, after=prev_acc.get(c, []))
                prev_acc[c] = [o]

            prefetch(0)
            prefetch(1)
            for c in range(NT):
                b = c % 2
                tbk = c // 4
                if c < NT - 1:
                    for dc in range(2):
                        ph.op("pe", lambda e, dc=dc, c=c: e.matmul(PA[:, dc * 512:(dc + 1) * 512], lhsT=kf[:, c, dc * 128:(dc + 1) * 128], rhs=vt[:, c, :], start=True, stop=True),
                              r=[("kf", c // 4), ("vt", c)], w=[("pa", dc)])
                for dc in range(2):
                    ph.op("pe", lambda e, dc=dc, c=c: e.matmul(PSC[:], lhsT=kT[:, dc, c * 128:(c + 1) * 128], rhs=qT[:, dc, c * 128:(c + 1) * 128], start=(dc == 0), stop=(dc == 1)),
                          r=[("kT", tbk, 0), ("kT", tbk, 1), ("qT", tbk, 0), ("qT", tbk, 1)], w=["psc"])
                ph.op("dve", lambda e, b=b, h=h: e.tensor_tensor(out=Pm[b][:], in0=PSC[:], in1=Mt[:, h, :], op=ALU.mult), r=["psc", ("Mt", h)], w=[("Pm", b)])
                for d_ in range(2):
                    ph.op("dve", lambda e, b=b, d_=d_, c=c, h=h: e.tensor_tensor(out=qfb[b][:, d_, :, :], in0=qT[:, :, c * 128:(c + 1) * 128],
                                                                                 in1=qd[:, h, d_, :, :], op=ALU.mult),
                          r=[("qT", tbk, 0), ("qT", tbk, 1), ("qd", h)], w=[("qfb", b, d_)])
                for k in range(8):
                    ph.op("pe", lambda e, c=c, k=k: e.matmul(PG[:], lhsT=xnT[:, k, c * 128:(c + 1) * 128], rhs=ws2v[:, k, :], start=(k == 0), stop=(k == 7)),
                          r=[("xnT", c), "ws2"], w=["pg"])
                ph.op("act", lambda e, b=b: e.activation(out=sgl[b][:], in_=PG[:], func=AF.Silu), r=["pg"], w=[("sgl", b)])
                mms = [(Pm[b][:], vt[:, c, :], [("Pm", b), ("vt", c)])]
                if c > 0:
                    for dc in range(2):
                        mms.append((qfb[b][:, 0, dc, :], Sfb[c % 2][:, dc * 512:(dc + 1) * 512], [("qfb", b, 0), ("Sfb", c % 2)]))
                if c < NT - 1:
                    for dc in range(2):
                        mms.append((qfb[b][:, 1, dc, :], Sbin[c % 3][:, dc * 512:(dc + 1) * 512], [("qfb", b, 1), ("Sbin", c % 3)]))
                for mi, (l_, r_, ks) in enumerate(mms):
                    ph.op("pe", lambda e, l_=l_, r_=r_, mi=mi, n=len(mms): e.matmul(PO[:], lhsT=l_, rhs=r_, start=(mi == 0), stop=(mi == n - 1)), r=ks, w=["po"])
                if c < NT - 1:
                    for dc in range(2):
                        ph.op("dve", lambda e, h=h, dc=dc: e.scalar_tensor_tensor(out=Sf32[:, dc * 512:(dc + 1) * 512], in0=Sf32[:, dc * 512:(dc + 1) * 512],
                                                                              scalar=cdc[:, h, 0:1], in1=PA[:, dc * 512:(dc + 1) * 512], op0=ALU.mult, op1=ALU.add),
                              r=["Sf32", ("pa", dc), ("cdc", h)], w=["Sf32"])
                    ph.op("act", lambda e, c=c: e.activation(out=Sfb[(c + 1) % 2][:], in_=Sf32[:], func=AF.Copy), r=["Sf32"], w=[("Sfb", (c + 1) % 2)])
                ph.op("dve", lambda e, b=b: e.bn_stats(out=bst[b][:], in_=PO[:]), r=["po"], w=[("bst", b)])
                ph.op("dve", lambda e, b=b: e.bn_aggr(out=mv[b][:, 0:2], in_=bst[b][:]), r=[("bst", b)], w=[("mv", b)])
                yy, kyy = newton(ph, mv[b][:, 1:2], ("mv", b), mv[b], 4, ("gn", b), 1.0, 1e-5)
                ph.op("dve", lambda e, b=b, yy=yy: e.tensor_scalar(out=on[b][:], in0=PO[:], scalar1=mv[b][:, 0:1], scalar2=yy, op0=ALU.subtract, op1=ALU.mult),
                      r=["po", ("mv", b), kyy], w=[("on", b)])
                ph.op("pool", lambda e, b=b: e.tensor_tensor(out=go[b][:], in0=on[b][:], in1=sgl[b][:], op=ALU.mult), r=[("on", b), ("sgl", b)], w=[("go", b)])
                prefetch(c + 2)
                if c > 0 and CCUT != "notail":
                    tail(c - 1)
            if CCUT != "notail":
                tail(NT - 1)
        ph.emit()

    sched = []
    for s in range(nseq):
        sched.append(("A", lambda s=s: phase_A(s)))
    sched.append(("R0", lambda: phase_R(0)))
    sched.append(("B0", lambda: phase_B(0)))
    sched.append(("P0", lambda: phase_P(0, False)))
    for s in range(nseq):
        sched.append(("C", lambda s=s: phase_C(s)))
    sched.append(("R1", lambda: phase_R(1)))
    sched.append(("B1", lambda: phase_B(1)))
    sched.append(("P1", lambda: phase_P(1, True)))
    only = _os.environ.get("SCHED_ONLY", "")
    for name, fn in sched:
        if only and name != only:
            continue
        fn()
        if stop_after is not None and name == stop_after:
            break
    gst.close()
    dbg = {"H": H}
    return nc, dbg


def make_in_maps(inputs, nseq, ncores):
    c = _consts()
    f32 = np.float32
    x = np.asarray(inputs["x"], f32)
    p = np.asarray(inputs["p"], f32)
    shared = dict(
        norm_mix=np.asarray(inputs["norm_mix"], f32), norm_ffn=np.asarray(inputs["norm_ffn"], f32),
        norm_ple=np.asarray(inputs["norm_ple"], f32), final_norm=np.asarray(inputs["final_norm"], f32).reshape(1, D),
        conv_w_in=np.asarray(inputs["conv_w_in"], f32)[0],
        conv_wb=np.ascontiguousarray(np.concatenate([np.asarray(inputs["conv_w"], f32)[0], np.asarray(inputs["conv_b"], f32)], 0)),
        conv_w_out=np.asarray(inputs["conv_w_out"], f32)[0],
        ret_w_in=np.asarray(inputs["ret_w_in"], f32)[0], ret_ld=np.asarray(inputs["ret_log_decay"], f32).reshape(1, 8),
        ret_w_out=np.asarray(inputs["ret_w_out"], f32)[0], router_w=np.asarray(inputs["router_w"], f32),
        exp_w_gate=np.asarray(inputs["exp_w_gate"], f32), exp_w_up=np.asarray(inputs["exp_w_up"], f32),
        exp_w_down=np.asarray(inputs["exp_w_down"], f32), ple_w_proj=np.asarray(inputs["ple_w_proj"], f32),
        ple_w_gate=np.asarray(inputs["ple_w_gate"], f32), **c)
    maps = []
    for ci in range(ncores):
        m = dict(shared)
        m["x"] = np.ascontiguousarray(x[ci * nseq:(ci + 1) * nseq].reshape(nseq * S, D))
        m["p"] = np.ascontiguousarray(p[:, ci * nseq:(ci + 1) * nseq].reshape(2, nseq * S, PLE))
        maps.append(m)
    return maps


def kernel(**inputs):
    B = np.asarray(inputs["x"]).shape[0]
    nseq = B // NCORES
    nc, _ = build(nseq)
    maps = make_in_maps(inputs, nseq, NCORES)
    res = run_bass_kernel_spmd(nc, maps, core_ids=list(range(NCORES)))
    outs = [np.asarray(r["out"], np.float32).reshape(nseq, S, D) for r in res.results]
    return np.concatenate(outs, 0)
```

```python
import numpy as np
from contextlib import ExitStack
import concourse.bass as bass
import concourse.mybir as mybir
from concourse.bass_utils import run_bass_kernel_spmd

F32 = mybir.dt.float32
BF16 = mybir.dt.bfloat16
I32 = mybir.dt.int32
U32 = mybir.dt.uint32
AF = mybir.ActivationFunctionType
ALU = mybir.AluOpType
AX = mybir.AxisListType

ENGS = ["pe", "act", "dve", "pool", "sp"]
N_DMA_SEMS = 20
SEMS = {}


class Op:
    __slots__ = ("eng", "fn", "waits", "idx", "flag", "cnt", "dsem", "dval", "is_dma", "pre")

    def __init__(self, eng, fn, idx, is_dma):
        self.eng = eng
        self.fn = fn
        self.idx = idx
        self.is_dma = is_dma
        self.flag = False
        self.cnt = None
        self.waits = []
        self.dsem = None
        self.dval = None
        self.pre = None


class Phase:
    def __init__(self, nc, name):
        self.nc = nc
        self.name = name
        self.q = {e: [] for e in ENGS}
        self.last_w = {}
        self.readers = {}
        self.stack = ExitStack()
        self.dma_rr = 0
        self.dma_last = [None] * N_DMA_SEMS
        self.dma_cnt = list(SEMS["dcnt"])
        self.n_ops = 0

    def sb(self, name, shape, dt):
        return self.stack.enter_context(self.nc.sbuf_tensor(f"{self.name}_{name}", list(shape), dt))

    def ps(self, name, shape, dt=F32):
        return self.stack.enter_context(self.nc.psum_tensor(f"{self.name}_{name}", list(shape), dt))

    def op(self, eng, fn, r=(), w=(), dma=False, after=(), pe_acc=False):
        o = Op(eng, fn, len(self.q[eng]), dma)
        deps = []
        for k in r:
            lw = self.last_w.get(k)
            if lw is not None:
                deps.append(lw)
        for k in w:
            lw = self.last_w.get(k)
            if lw is not None:
                deps.append(lw)
            deps.extend(self.readers.get(k, ()))
        deps.extend(after)
        seen = set()
        for d in deps:
            if d is o or id(d) in seen:
                continue
            seen.add(id(d))
            if d.eng == "pe" and eng == "pe" and not d.is_dma:
                continue
            o.waits.append(d)
            if not d.is_dma:
                d.flag = True
        if dma:
            s = self.dma_rr % N_DMA_SEMS
            self.dma_rr += 1
            o.pre = self.dma_last[s]
            self.dma_cnt[s] += 1
            o.dsem = s
            o.dval = 16 * self.dma_cnt[s]
            self.dma_last[s] = o
        for k in w:
            self.last_w[k] = o
            self.readers[k] = []
        for k in r:
            if k not in w:
                self.readers.setdefault(k, []).append(o)
        self.q[eng].append(o)
        self.n_ops += 1
        return o

    def emit(self):
        nc = self.nc
        st = self.stack
        esem = SEMS["esem"]
        dsem = SEMS["dsem"]
        ebase = dict(SEMS["ebase"])
        dbase = [16 * c for c in SEMS["dcnt"]]
        final = {}
        for e in ENGS:
            comp = [o for o in self.q[e] if not o.is_dma]
            if comp:
                comp[-1].flag = True
            c = ebase[e]
            for o in self.q[e]:
                if not o.is_dma and o.flag:
                    c += 1
                    o.cnt = c
            final[e] = c
        dfinal = [16 * c for c in self.dma_cnt]
        SEMS["ebase"] = dict(final)
        SEMS["dcnt"] = list(self.dma_cnt)
        q = self.q

        def run(e, eng):
            waited_e = dict(ebase)
            waited_d = list(dbase)
            for o in q[e]:
                ws = list(o.waits)
                if o.pre is not None:
                    ws.append(o.pre)
                for d in ws:
                    if d.is_dma:
                        if waited_d[d.dsem] < d.dval:
                            eng.wait_ge(dsem[d.dsem], d.dval)
                            waited_d[d.dsem] = d.dval
                    else:
                        if waited_e[d.eng] < d.cnt:
                            eng.wait_ge(esem[d.eng], d.cnt)
                            waited_e[d.eng] = d.cnt
                ins = o.fn(eng)
                if o.is_dma:
                    ins.then_inc(dsem[o.dsem], 16)
                elif o.flag:
                    ins.then_inc(esem[e], 1)
            for x in ENGS:
                if final[x] > waited_e[x]:
                    eng.wait_ge(esem[x], final[x])
            for i in range(N_DMA_SEMS):
                if dfinal[i] > waited_d[i]:
                    eng.wait_ge(dsem[i], dfinal[i])

        with nc.Block() as block:
            @block.tensor
            def _(eng):
                run("pe", eng)

            @block.scalar
            def _(eng):
                run("act", eng)

            @block.vector
            def _(eng):
                run("dve", eng)

            @block.gpsimd
            def _(eng):
                run("pool", eng)

            @block.sync
            def _(eng):
                run("sp", eng)
        self.stack.close()


S = 2048
D = 1024
NT = 16
NE = 16
CAP = 256
FF = 2048
PLE = 256
NCORES = 8
import os as _os
CCUT = _os.environ.get('CCUT', '')


def _consts():
    half = 128
    inv = (10000.0 ** (-np.arange(half, dtype=np.float32) / np.float32(half))).astype(np.float32)
    pos = np.arange(S, dtype=np.float32)
    ang = (inv[:, None] * pos[None, :]).astype(np.float32)
    cs = np.stack([np.cos(ang.astype(np.float64)), np.sin(ang.astype(np.float64))], 1).astype(np.float32)
    j = np.arange(128, dtype=np.float32)[:, None]
    i = np.arange(128, dtype=np.float32)[None, :]
    dmat = np.stack([np.maximum(i - j, 0), (j <= i).astype(np.float32),
                     np.maximum(j - i, 0), (j > i).astype(np.float32)], 0).astype(np.float32)
    rows = np.stack([np.broadcast_to(i + 1, (128, 128)), np.broadcast_to(128 - i, (128, 128))], 0).astype(np.float32)
    cols = np.concatenate([127 - j, j, np.full((128, 1), 128.0, np.float32)], 1).astype(np.float32)
    seqoff = ((np.arange(64) // 16) * S).astype(np.float32)[:, None]
    return dict(ident=np.eye(128, dtype=np.float32), cs=np.ascontiguousarray(cs), dmat=dmat,
                rows=np.ascontiguousarray(rows), cols=np.ascontiguousarray(cols), seqoff=seqoff)


def build(nseq, stop_after=None, debug=False):
    nc = bass.Bass("TRN2", target_bir_lowering=False)
    T = nseq * S
    NTL = nseq * NT

    def din(name, shape, dt=F32):
        return nc.dram_tensor(name, list(shape), dt, kind="ExternalInput").ap()

    x = din("x", [T, D])
    p_in = din("p", [2, T, PLE])
    norm_mix = din("norm_mix", [2, D])
    norm_ffn = din("norm_ffn", [2, D])
    norm_ple = din("norm_ple", [2, D])
    final_norm = din("final_norm", [1, D])
    conv_w_in = din("conv_w_in", [D, 3 * D])
    conv_wb = din("conv_wb", [4, D])
    conv_w_out = din("conv_w_out", [D, D])
    ret_w_in = din("ret_w_in", [D, 6 * D])
    ret_ld = din("ret_ld", [1, 8])
    ret_w_out = din("ret_w_out", [2 * D, D])
    router_w = din("router_w", [2, D, NE])
    w_gate = din("exp_w_gate", [2, NE, D, FF])
    w_up = din("exp_w_up", [2, NE, D, FF])
    w_down = din("exp_w_down", [2, NE, FF, D])
    ple_proj = din("ple_w_proj", [2, PLE, D])
    ple_gate = din("ple_w_gate", [2, D, D])
    c_ident = din("ident", [128, 128])
    c_cs = din("cs", [128, 2, S])
    c_dmat = din("dmat", [4, 128, 128])
    c_rows = din("rows", [2, 128, 128])
    c_cols = din("cols", [128, 3])
    c_seqoff = din("seqoff", [64, 1])
    out = nc.dram_tensor("out", [T, D], F32, kind="ExternalOutput").ap()
    H = nc.dram_tensor("Hres", [T, D], F32, kind="ExternalOutput" if debug else "Internal").ap()
    XN = nc.dram_tensor("XNs", [T, D], BF16, kind="Internal").ap()
    SBD = nc.dram_tensor("SBD", [16, 128, 1024], BF16, kind="Internal").ap()

    gst = ExitStack()
    SEMS["esem"] = {e: gst.enter_context(nc.semaphore(f"s_{e}")) for e in ENGS}
    SEMS["dsem"] = [gst.enter_context(nc.semaphore(f"d{i}")) for i in range(N_DMA_SEMS)]
    SEMS["ebase"] = {e: 0 for e in ENGS}
    SEMS["dcnt"] = [0] * N_DMA_SEMS
    identf = gst.enter_context(nc.sbuf_tensor("g_identf", [128, 128], F32))
    identb = gst.enter_context(nc.sbuf_tensor("g_identb", [128, 128], BF16))
    aff_tok = gst.enter_context(nc.sbuf_tensor("g_aff", [128, NT, 64], F32))
    epsn = gst.enter_context(nc.sbuf_tensor("g_eps", [128, 2], F32))

    ph = Phase(nc, "I")
    ph.op("sp", lambda e: e.dma_start(out=identf[:], in_=c_ident), w=["idf"], dma=True)
    ph.op("dve", lambda e: e.tensor_copy(out=identb[:], in_=identf[:]), r=["idf"], w=["idb"])
    ph.op("dve", lambda e: e.memset(aff_tok[:], 0.0), w=["aff"])
    ph.op("dve", lambda e: e.memset(epsn[:, 0:1], 1e-6), w=["eps0"])
    ph.op("dve", lambda e: e.memset(epsn[:, 1:2], 1e-5), w=["eps1"])
    ph.emit()

    def newton(ph, src, skey, tile, c0, tag, scale, eps, it_eng="pool"):
        v = tile[:, c0:c0 + 1]
        y = tile[:, c0 + 1:c0 + 2]
        t = tile[:, c0 + 2:c0 + 3]
        vi = v.bitcast(I32)
        yi = y.bitcast(I32)
        kv, ky, kt = ("nv", tag), ("ny", tag), ("nt", tag)
        ph.op("dve", lambda e: e.tensor_scalar(out=v, in0=src, scalar1=scale, scalar2=eps, op0=ALU.mult, op1=ALU.add), r=[skey], w=[kv])
        ph.op("dve", lambda e: e.tensor_single_scalar(out=yi, in_=vi, scalar=1, op=ALU.logical_shift_right), r=[kv], w=[ky])
        ph.op("dve", lambda e: e.tensor_scalar(out=yi, in0=yi, scalar1=-1, scalar2=0x5f3759df, op0=ALU.mult, op1=ALU.add), r=[ky], w=[ky])
        for _ in range(2):
            if it_eng == "dve":
                ph.op("dve", lambda e: e.scalar_tensor_tensor(out=t, in0=v, scalar=y, in1=y, op0=ALU.mult, op1=ALU.mult), r=[kv, ky], w=[kt])
            else:
                ph.op(it_eng, lambda e: e.tensor_tensor(out=t, in0=v, in1=y, op=ALU.mult), r=[kv, ky], w=[kt])
                ph.op(it_eng, lambda e: e.tensor_tensor(out=t, in0=t, in1=y, op=ALU.mult), r=[kt, ky], w=[kt])
            ph.op(it_eng, lambda e: e.tensor_scalar(out=t, in0=t, scalar1=-0.5, scalar2=1.5, op0=ALU.mult, op1=ALU.add), r=[kt], w=[kt])
            ph.op(it_eng, lambda e: e.tensor_tensor(out=y, in0=y, in1=t, op=ALU.mult), r=[kt, ky], w=[ky])
        return y, ky

    def rmsnorm(ph, hin, hkey, gb, gkey, outt, okey, junk, sst, tag, it_eng="pool"):
        hkeys = hkey if isinstance(hkey, list) else [hkey]
        ph.op("act", lambda e: e.activation(out=junk[:], in_=hin, func=AF.Square, accum_out=sst[:, 3:4]),
              r=hkeys, w=[("ss", tag)])
        y, ky = newton(ph, sst[:, 3:4], ("ss", tag), sst, 0, tag, 1.0 / D, 1e-6, it_eng)
        ph.op("dve", lambda e: e.scalar_tensor_tensor(out=outt, in0=hin, scalar=y, in1=gb[:], op0=ALU.mult, op1=ALU.mult),
              r=hkeys + [ky, gkey], w=[okey])

    def transpose8(ph, src, skey, ptb, pkey, dst, dkey, eng, n=8):
        for k in range(n):
            ph.op("pe", lambda e, k=k: e.transpose(out=ptb[:, k, :], in_=src[:, k * 128:(k + 1) * 128], identity=identb[:]),
                  r=[skey], w=[pkey])
        if eng == "act":
            ph.op("act", lambda e: e.activation(out=dst, in_=ptb[:, 0:n, :], func=AF.Copy), r=[pkey], w=[dkey])
        else:
            ph.op("dve", lambda e: e.tensor_copy(out=dst, in_=ptb[:, 0:n, :]), r=[pkey], w=[dkey])

    def phase_A(s):
        ph = Phase(nc, f"A{s}")
        xnT = ph.sb("xnT", [128, 8, S], BF16)
        zT = ph.sb("zT", [128, 8, S], BF16)
        wout = ph.sb("wout", [128, 8, D], BF16)
        win = [ph.sb(f"win{i}", [128, 3, 8, 128], BF16) for i in range(2)]
        wst = [ph.sb(f"wst{i}", [128, 3, 8, 128], F32) for i in range(2)]
        gmix = ph.sb("gmix", [128, D], F32)
        cw4 = ph.sb("cw4", [4, D], F32)
        cwb = ph.sb("cwb", [128, 8, 4], F32)
        xt = [ph.sb(f"xt{i}", [128, D], F32) for i in range(2)]
        junk = ph.sb("junk", [128, D], BF16)
        sst = [ph.sb(f"sst{i}", [128, 4], F32) for i in range(2)]
        xn = [ph.sb(f"xn{i}", [128, D], BF16) for i in range(2)]
        u = [ph.sb(f"u{i}", [128, S + 2], F32) for i in range(2)]
        bsb = [ph.sb(f"bsb{i}", [128, S], F32) for i in range(2)]
        csb = [ph.sb(f"csb{i}", [128, 512], F32) for i in range(2)]
        yc = [ph.sb(f"yc{i}", [128, S], F32) for i in range(2)]
        hn = [ph.sb(f"hn{i}", [128, D], F32) for i in range(2)]
        ptb = [ph.ps(f"ptb{i}", [128, 8, 128], BF16) for i in range(2)]
        PP = [ph.ps(f"pp{i}", [128, 1024], F32) for i in range(3)]

        ph.op("sp", lambda e: e.dma_start(out=gmix[:], in_=norm_mix[0:1, :].partition_broadcast(128)), w=["gmix"], dma=True)
        ph.op("sp", lambda e: e.dma_start(out=cw4[:], in_=conv_wb), w=["cw4"], dma=True)
        ph.op("pool", lambda e: e.dma_start(out=wout[:], in_=conv_w_out.rearrange("(k p) n -> p k n", p=128)), w=["wout"], dma=True)
        cps = PP[0][:, 0:32].rearrange("p (j w) -> p j w", w=4)
        for j in range(8):
            ph.op("pe", lambda e, j=j: e.transpose(out=cps[:, j, :], in_=cw4[:, j * 128:(j + 1) * 128], identity=identf[0:4, 0:4]),
                  r=["cw4"], w=[("pp", 0, 0)])
        ph.op("dve", lambda e: e.tensor_copy(out=cwb[:], in_=cps), r=[("pp", 0, 0)], w=["cwb"])
        for i in range(2):
            ph.op("dve", lambda e, i=i: e.memset(u[i][:, 0:1], 0.0), w=[("u", i)])
            ph.op("dve", lambda e, i=i: e.memset(u[i][:, S + 1:S + 2], 0.0), w=[("u", i)])
        def A1a(i):
            b = i % 2
            r0 = s * S + i * 128
            ph.op("sp", lambda e, b=b, r0=r0: e.dma_start(out=xt[b][:], in_=x[r0:r0 + 128, :]), w=[("xt", b)], dma=True)
            rmsnorm(ph, xt[b][:], ("xt", b), gmix, "gmix", xn[b][:], ("xn", b), junk, sst[b], b)

        def A1b(i):
            b = i % 2
            transpose8(ph, xn[b], ("xn", b), ptb[b], ("ptb", b), xnT[:, :, i * 128:(i + 1) * 128], ("xnT", i), "act")

        for step in range(NT + 1):
            if step < NT:
                A1a(step)
            if step >= 1:
                A1b(step - 1)
        xnT_keys = [("xnT", i) for i in range(NT)]
        w_in_v = conv_w_in.rearrange("(k p) (t n) -> p t k n", p=128, t=3)
        def load_w(j):
            jb = j % 2
            ph.op("sp", lambda e, j=j, jb=jb: e.dma_start(out=wst[jb][:], in_=w_in_v[:, :, :, j * 128:(j + 1) * 128]),
                  w=[("wst", jb)], dma=True)
            ph.op("pool", lambda e, jb=jb: e.tensor_copy(out=win[jb][:, 0:2], in_=wst[jb][:, 0:2]), r=[("wst", jb)], w=[("win", jb)])
            ph.op("act", lambda e, jb=jb: e.activation(out=win[jb][:, 2], in_=wst[jb][:, 2], func=AF.Copy), r=[("wst", jb)], w=[("win", jb, 2)])

        for j in range(8):
            jb = j % 2
            if j == 0:
                load_w(0)
            for tb in range(4):
                q = (j * 4 + tb) % 2
                bps = PP[q][:, 0:512]
                cps_ = PP[q][:, 512:1024]
                vps = PP[2][:, q * 512:(q + 1) * 512]
                tks = [("xnT", i) for i in range(tb * 4, tb * 4 + 4)]
                for t, (dst, key) in enumerate([(bps, ("pp", q, 0)), (cps_, ("pp", q, 1)), (vps, ("pp", 2, q))]):
                    for k in range(8):
                        ph.op("pe", lambda e, dst=dst, t=t, k=k, jb=jb, tb=tb: e.matmul(
                            dst, lhsT=win[jb][:, t, k, :], rhs=xnT[:, k, tb * 512:(tb + 1) * 512], start=(k == 0), stop=(k == 7)),
                            r=[("win", jb), ("win", jb, 2)] + tks, w=[key])
                ph.op("act", lambda e, q=q, cps_=cps_: e.activation(out=csb[q][:], in_=cps_, func=AF.Copy), r=[("pp", q, 1)], w=[("csb", q)])
                ph.op("act", lambda e, jb=jb, tb=tb, bps=bps: e.activation(out=bsb[jb][:, tb * 512:(tb + 1) * 512], in_=bps, func=AF.Copy),
                      r=[("pp", q, 0)], w=[("bsb", jb)])
                ph.op("dve", lambda e, jb=jb, tb=tb, q=q, vps=vps: e.tensor_tensor(
                    out=u[jb][:, 1 + tb * 512:1 + (tb + 1) * 512], in0=csb[q][:], in1=vps, op=ALU.mult),
                    r=[("csb", q), ("pp", 2, q)], w=[("u", jb)])
            if j + 1 < 8:
                load_w(j + 1)
            ph.op("act", lambda e, j=j, jb=jb: e.activation(out=yc[jb][:], in_=u[jb][:, 1:S + 1], func=AF.Identity,
                                                            bias=cwb[:, j, 3:4], scale=cwb[:, j, 1:2]),
                  r=[("u", jb), "cwb"], w=[("yc", jb)])
            ph.op("dve", lambda e, j=j, jb=jb: e.scalar_tensor_tensor(out=yc[jb][:], in0=u[jb][:, 0:S], scalar=cwb[:, j, 0:1], in1=yc[jb][:],
                                                                     op0=ALU.mult, op1=ALU.add),
                  r=[("u", jb), "cwb", ("yc", jb)], w=[("yc", jb)])
            ph.op("dve", lambda e, j=j, jb=jb: e.scalar_tensor_tensor(out=yc[jb][:], in0=u[jb][:, 2:S + 2], scalar=cwb[:, j, 2:3], in1=yc[jb][:],
                                                                      op0=ALU.mult, op1=ALU.add),
                  r=[("u", jb), "cwb", ("yc", jb)], w=[("yc", jb)])
            ph.op("pool", lambda e, j=j, jb=jb: e.tensor_tensor(out=zT[:, j, :], in0=bsb[jb][:], in1=yc[jb][:], op=ALU.mult),
                  r=[("bsb", jb), ("yc", jb)], w=[("zT", j)])
        zkeys = [("zT", j) for j in range(8)]
        def loads_A3(i):
            b = i % 2
            r0 = s * S + i * 128
            ph.op("sp", lambda e, b=b, r0=r0: e.dma_start(out=xt[b][:], in_=x[r0:r0 + 128, :]), w=[("xt", b)], dma=True)

        loads_A3(0)
        for i in range(NT):
            b = i % 2
            r0 = s * S + i * 128
            if i + 1 < NT:
                loads_A3(i + 1)
            for nh in range(2):
                for k in range(8):
                    ph.op("pe", lambda e, b=b, nh=nh, k=k, i=i: e.matmul(
                        PP[b][:, nh * 512:(nh + 1) * 512], lhsT=zT[:, k, i * 128:(i + 1) * 128], rhs=wout[:, k, nh * 512:(nh + 1) * 512],
                        start=(k == 0), stop=(k == 7)), r=zkeys + ["wout"], w=[("pp", b, nh)])
                ph.op("dve", lambda e, b=b, nh=nh: e.tensor_tensor(out=hn[b][:, nh * 512:(nh + 1) * 512], in0=xt[b][:, nh * 512:(nh + 1) * 512],
                                                                  in1=PP[b][:, nh * 512:(nh + 1) * 512], op=ALU.add),
                      r=[("xt", b), ("pp", b, nh)], w=[("hn", b, nh)])
            ph.op("sp", lambda e, b=b, r0=r0: e.dma_start(out=H[r0:r0 + 128, :], in_=hn[b][:]), r=[("hn", b, 0), ("hn", b, 1)], w=[("H", r0)], dma=True)
        ph.emit()

    def phase_R(l):
        ph = Phase(nc, f"R{l}")
        NB = 6
        gffn = ph.sb("gffn", [128, D], F32)
        wr = ph.sb("wr", [128, 8, NE], BF16)
        hn = [ph.sb(f"hn{i}", [128, D], F32) for i in range(NB)]
        junk = ph.sb("junk", [128, D], BF16)
        sst = [ph.sb(f"sst{i}", [128, 4], F32) for i in range(NB)]
        xn = [ph.sb(f"xn{i}", [128, D], BF16) for i in range(NB)]
        xT = [ph.sb(f"xT{i}", [128, 8, 128], BF16) for i in range(NB)]
        sm = [ph.sb(f"sm{i}", [128, 4], F32) for i in range(NB)]
        ex = [ph.sb(f"ex{i}", [128, NE], F32) for i in range(NB)]
        ptb = [ph.ps(f"ptb{i}", [128, 8, 128], BF16) for i in range(2)]
        PL = [ph.ps(f"pl{i}", [128, NE], F32) for i in range(2)]
        ph.op("sp", lambda e: e.dma_start(out=gffn[:], in_=norm_ffn[l:l + 1, :].partition_broadcast(128)), w=["gffn"], dma=True)
        ph.op("pool", lambda e: e.dma_start(out=wr[:], in_=router_w[l].rearrange("(k p) n -> p k n", p=128)), w=["wr"], dma=True)
        def loads_R(ti):
            b = ti % NB
            r0 = ti * 128
            ph.op("sp", lambda e, b=b, r0=r0: e.dma_start(out=hn[b][:], in_=H[r0:r0 + 128, :]), w=[("hn", b)], dma=True)

        def R_s1(ti):
            b = ti % NB
            pb2 = ti % 2
            s, i = divmod(ti, NT)
            r0 = ti * 128
            rmsnorm(ph, hn[b][:], ("hn", b), gffn, "gffn", xn[b][:], ("xn", b), junk, sst[b], b)

        def R_s1b(ti):
            b = ti % NB
            pb2 = ti % 2
            r0 = ti * 128
            ph.op("sp", lambda e, b=b, r0=r0: e.dma_start(out=XN[r0:r0 + 128, :], in_=xn[b][:]), r=[("xn", b)], w=[("XN", r0)], dma=True)
            transpose8(ph, xn[b], ("xn", b), ptb[pb2], ("ptb", pb2), xT[b][:], ("xT", b), "act")

        def R_s2(ti):
            b = ti % NB
            pb2 = ti % 2
            s, i = divmod(ti, NT)
            r0 = ti * 128
            for k in range(8):
                ph.op("pe", lambda e, b=b, k=k, pb2=pb2: e.matmul(PL[pb2][:], lhsT=xT[b][:, k, :], rhs=wr[:, k, :], start=(k == 0), stop=(k == 7)),
                      r=[("xT", b), "wr"], w=[("pl", pb2)])
            ph.op("dve", lambda e, b=b, pb2=pb2: e.reduce_max(out=sm[b][:, 0:1], in_=PL[pb2][:], axis=AX.X), r=[("pl", pb2)], w=[("mx", b)])
            ph.op("dve", lambda e, b=b: e.tensor_scalar(out=sm[b][:, 1:2], in0=sm[b][:, 0:1], scalar1=-1.0, scalar2=None, op0=ALU.mult),
                  r=[("mx", b)], w=[("nmx", b)])
            ph.op("act", lambda e, b=b, pb2=pb2: e.activation(out=ex[b][:], in_=PL[pb2][:], func=AF.Exp, bias=sm[b][:, 1:2], scale=1.0, accum_out=sm[b][:, 2:3]),
                  r=[("pl", pb2), ("nmx", b)], w=[("ex", b), ("sum", b)])
            ph.op("dve", lambda e, b=b: e.reciprocal(out=sm[b][:, 3:4], in_=sm[b][:, 2:3]), r=[("sum", b)], w=[("rsum", b)])
            ph.op("dve", lambda e, b=b, s=s, i=i: e.tensor_scalar(out=aff_tok[:, i, s * 16:(s + 1) * 16], in0=ex[b][:], scalar1=sm[b][:, 3:4],
                                                                 scalar2=None, op0=ALU.mult),
                  r=[("ex", b), ("rsum", b)], w=[("aff", ti)])

        for step in range(NTL + 4):
            if step < NTL:
                loads_R(step)
            if 0 <= step - 2 < NTL:
                R_s1(step - 2)
            if 0 <= step - 3 < NTL:
                R_s1b(step - 3)
            if 0 <= step - 4 < NTL:
                R_s2(step - 4)
        ph.emit()

    def phase_P(l, final):
        ph = Phase(nc, f"P{l}")
        NB = 6
        wpg = ph.sb("wpg", [128, 8, D], BF16)
        wpp = ph.sb("wpp", [128, 2, D], BF16)
        gple = ph.sb("gple", [128, D], F32)
        gnx = ph.sb("gnx", [128, D], F32)
        hn = [ph.sb(f"hn{i}", [128, D], F32) for i in range(NB)]
        pt = [ph.sb(f"pt{i}", [128, PLE], F32) for i in range(NB)]
        pb = [ph.sb(f"pb{i}", [128, PLE], BF16) for i in range(NB)]
        pT = [ph.sb(f"pT{i}", [128, 2, 128], BF16) for i in range(NB)]
        junk = ph.sb("junk", [128, D], BF16)
        sst = [ph.sb(f"sst{i}", [128, 4], F32) for i in range(2 * NB)]
        xn = [ph.sb(f"xn{i}", [128, D], BF16) for i in range(NB)]
        xT = [ph.sb(f"xT{i}", [128, 8, 128], BF16) for i in range(NB)]
        sgm = [ph.sb(f"sgm{i}", [128, D], F32) for i in range(NB)]
        h2 = [ph.sb(f"h2{i}", [128, D], F32) for i in range(NB)]
        xo = [ph.sb(f"xo{i}", [128, D], F32 if final else BF16) for i in range(NB)]
        ptb = [ph.ps(f"ptb{i}", [128, 8, 128], BF16) for i in range(2)]
        ptp = ph.ps("ptp", [128, 8, 128], BF16)
        PG = ph.ps("pg", [128, 1024], F32)
        PQ = ph.ps("pq", [128, 1024], F32)
        ph.op("pool", lambda e: e.dma_start(out=wpg[:], in_=ple_gate[l].rearrange("(k p) n -> p k n", p=128)), w=["wpg"], dma=True)
        ph.op("pool", lambda e: e.dma_start(out=wpp[:], in_=ple_proj[l].rearrange("(k p) n -> p k n", p=128)), w=["wpp"], dma=True)
        ph.op("sp", lambda e: e.dma_start(out=gple[:], in_=norm_ple[l:l + 1, :].partition_broadcast(128)), w=["gple"], dma=True)
        gsrc = final_norm[0:1, :] if final else norm_mix[l + 1:l + 2, :]
        ph.op("sp", lambda e: e.dma_start(out=gnx[:], in_=gsrc.partition_broadcast(128)), w=["gnx"], dma=True)
        def loads_P(ti):
            b = ti % NB
            r0 = ti * 128
            ph.op("sp", lambda e, b=b, r0=r0: e.dma_start(out=hn[b][:], in_=H[r0:r0 + 128, :]), w=[("hn", b)], dma=True)
            ph.op("sp", lambda e, b=b, r0=r0: e.dma_start(out=pt[b][:], in_=p_in[l, r0:r0 + 128, :]), w=[("pt", b)], dma=True)

        def P_s1(ti):
            b = ti % NB
            pb2 = ti % 2
            r0 = ti * 128
            rmsnorm(ph, hn[b][:], ("hn", b), gple, "gple", xn[b][:], ("xn", b), junk, sst[b], b, "dve")
            ph.op("pool", lambda e, b=b: e.tensor_copy(out=pb[b][:], in_=pt[b][:]), r=[("pt", b)], w=[("pb", b)])

        def P_s1b(ti):
            b = ti % NB
            pb2 = ti % 2
            transpose8(ph, xn[b], ("xn", b), ptb[pb2], ("ptb", pb2), xT[b][:], ("xT", b), "act")
            transpose8(ph, pb[b], ("pb", b), ptp, "ptp", pT[b][:], ("pT", b), "dve", n=2)

        def P_s2(ti):
            b = ti % NB
            pb2 = ti % 2
            r0 = ti * 128
            for nh in range(2):
                for k in range(8):
                    ph.op("pe", lambda e, b=b, nh=nh, k=k: e.matmul(PG[:, nh * 512:(nh + 1) * 512], lhsT=xT[b][:, k, :],
                                                                   rhs=wpg[:, k, nh * 512:(nh + 1) * 512], start=(k == 0), stop=(k == 7)),
                          r=[("xT", b), "wpg"], w=[("pg", nh)])
                for k in range(2):
                    ph.op("pe", lambda e, b=b, nh=nh, k=k: e.matmul(PQ[:, nh * 512:(nh + 1) * 512], lhsT=pT[b][:, k, :],
                                                                   rhs=wpp[:, k, nh * 512:(nh + 1) * 512], start=(k == 0), stop=(k == 1)),
                          r=[("pT", b), "wpp"], w=[("pq", nh)])
                sl = slice(nh * 512, (nh + 1) * 512)
                ph.op("act", lambda e, b=b, sl=sl: e.activation(out=sgm[b][:, sl], in_=PG[:, sl], func=AF.Sigmoid), r=[("pg", nh)], w=[("sgm", b, nh)])
                ph.op("dve", lambda e, b=b, sl=sl: e.tensor_tensor(out=sgm[b][:, sl], in0=sgm[b][:, sl], in1=PQ[:, sl], op=ALU.mult),
                      r=[("sgm", b, nh), ("pq", nh)], w=[("sgm", b, nh)])
                ph.op("pool", lambda e, b=b, sl=sl: e.tensor_tensor(out=h2[b][:, sl], in0=sgm[b][:, sl], in1=hn[b][:, sl], op=ALU.add),
                      r=[("sgm", b, nh), ("hn", b)], w=[("h2", b, nh)])
            hk = [("h2", b, 0), ("h2", b, 1)]
            if not final:
                ph.op("sp", lambda e, b=b, r0=r0: e.dma_start(out=H[r0:r0 + 128, :], in_=h2[b][:]), r=hk, w=[("H", r0)], dma=True)

        def P_s3(ti):
            b = ti % NB
            pb2 = ti % 2
            r0 = ti * 128
            hk = [("h2", b, 0), ("h2", b, 1)]
            rmsnorm(ph, h2[b][:], hk, gnx, "gnx", xo[b][:], ("xo", b), junk, sst[NB + b], ("n2", b))
            dst = out if final else XN
            ph.op("sp", lambda e, b=b, r0=r0, dst=dst: e.dma_start(out=dst[r0:r0 + 128, :], in_=xo[b][:]), r=[("xo", b)], w=[("O", r0)], dma=True)

        for step in range(NTL + 5):
            if step < NTL:
                loads_P(step)
            if 0 <= step - 2 < NTL:
                P_s1(step - 2)
            if 0 <= step - 3 < NTL:
                P_s1b(step - 3)
            if 0 <= step - 4 < NTL:
                P_s2(step - 4)
            if 0 <= step - 5 < NTL:
                P_s3(step - 5)
        ph.emit()

    def phase_B(l):
        ph = Phase(nc, f"B{l}")
        NTOK = nseq * CAP
        ntile = nseq * 2
        TB = min(512, NTOK)
        nblk = NTOK // TB
        work = ph.sb("work", [64, S], F32)
        gates = ph.sb("gates", [64, CAP], F32)
        idxu = ph.sb("idxu", [64, CAP], U32)
        idxf = ph.sb("idxf", [64, CAP], F32)
        soff = ph.sb("soff", [64, 1], F32)
        gT = ph.sb("gT", [128, 2, 64], F32)
        iTi = ph.sb("iTi", [128, 2, 64], I32)
        ring = [ph.sb(f"ring{i}", [128, 8192], BF16) for i in range(4)]
        xg = [ph.sb(f"xg{i}", [128, ntile, D], BF16) for i in range(2)]
        xgT = ph.sb("xgT", [128, 8, NTOK], BF16)
        hT = ph.sb("hT", [128, 16, NTOK], BF16)
        sg = [ph.sb(f"sg{i}", [128, 512], F32) for i in range(2)]
        yt = [ph.sb(f"yt{i}", [128, D], F32) for i in range(2)]
        ptb = [ph.ps(f"ptb{i}", [128, 8, 128], BF16) for i in range(2)]
        PGU = [ph.ps(f"pgu{i}", [128, 1024], F32) for i in range(2)]
        PD = ph.ps("pd", [128, 1024], F32)

        ph.op("sp", lambda e: e.dma_start(out=soff[:], in_=c_seqoff), w=["soff"], dma=True)
        for g in range(4):
            for ii in range(4):
                i = g * 4 + ii
                ph.op("pe", lambda e, g=g, ii=ii, i=i: e.transpose(out=PGU[g % 2][0:64, ii * 128:(ii + 1) * 128], in_=aff_tok[:, i, :], identity=identf[:]),
                      r=["aff"], w=[("pgu", g % 2, 0)])
            ph.op("dve", lambda e, g=g: e.tensor_copy(out=work[:, g * 512:(g + 1) * 512], in_=PGU[g % 2][0:64, 0:512]),
                  r=[("pgu", g % 2, 0)], w=["work"])
        for r in range(CAP // 8):
            sl = slice(r * 8, (r + 1) * 8)
            ph.op("dve", lambda e, sl=sl: e.max(out=gates[:, sl], in_=work[:]), r=["work"], w=[("gt", r)])
            ph.op("dve", lambda e, sl=sl: e.max_index(out=idxu[:, sl], in_max=gates[:, sl], in_values=work[:]), r=["work", ("gt", r)], w=[("ix", r)])
            ph.op("dve", lambda e, sl=sl: e.match_replace(out=work[:], in_to_replace=gates[:, sl], in_values=work[:], imm_value=-1.0),
                  r=["work", ("gt", r)], w=["work"])
        gkeys = [("gt", r) for r in range(CAP // 8)]
        ikeys = [("ix", r) for r in range(CAP // 8)]
        ph.op("dve", lambda e: e.tensor_copy(out=idxf[:], in_=idxu[:]), r=ikeys, w=["idxf"])
        ph.op("dve", lambda e: e.tensor_scalar(out=idxf[:], in0=idxf[:], scalar1=soff[:, 0:1], scalar2=None, op0=ALU.add), r=["idxf", "soff"], w=["idxf"])
        tp = PGU[0][:, 0:256].rearrange("p (a c) -> p a c", c=64)
        for hf in range(2):
            ph.op("pe", lambda e, hf=hf: e.transpose(out=tp[:, hf, :], in_=gates[:, hf * 128:(hf + 1) * 128], identity=identf[0:64, 0:64]),
                  r=gkeys, w=[("pgu", 0, 0)])
            ph.op("pe", lambda e, hf=hf: e.transpose(out=tp[:, 2 + hf, :], in_=idxf[:, hf * 128:(hf + 1) * 128], identity=identf[0:64, 0:64]),
                  r=["idxf"], w=[("pgu", 0, 0)])
        ph.op("dve", lambda e: e.tensor_copy(out=gT[:], in_=tp[:, 0:2, :]), r=[("pgu", 0, 0)], w=["gT"])
        ph.op("act", lambda e: e.activation(out=iTi[:], in_=tp[:, 2:4, :], func=AF.Copy), r=[("pgu", 0, 0)], w=["iTi"])

        wg_v = w_gate[l].rearrange("e (k p) f -> e p k f", p=128)
        wu_v = w_up[l].rearrange("e (k p) f -> e p k f", p=128)
        wd_v = w_down[l].rearrange("e (k p) n -> e p k n", p=128)

        def load_slab(g):
            e_, j = divmod(g, 6)
            if e_ >= NE:
                return
            slot = g % 4
            if j < 4:
                dstg = ring[slot][:, 0:4096].rearrange("p (k n) -> p k n", n=512)
                dstu = ring[slot][:, 4096:8192].rearrange("p (k n) -> p k n", n=512)
                ph.op("pool", lambda e: e.dma_start(out=dstg, in_=wg_v[e_, :, :, j * 512:(j + 1) * 512]), w=[("ring", slot, 0)], dma=True)
                ph.op("pool", lambda e: e.dma_start(out=dstu, in_=wu_v[e_, :, :, j * 512:(j + 1) * 512]), w=[("ring", slot, 1)], dma=True)
            else:
                dh = j - 4
                dst = ring[slot][:].rearrange("p (k n) -> p k n", n=1024)
                ph.op("pool", lambda e: e.dma_start(out=dst, in_=wd_v[e_, :, dh * 8:(dh + 1) * 8, :]), w=[("ring", slot, 0), ("ring", slot, 1)], dma=True)

        def gather(e_):
            if e_ >= NE:
                return
            for t in range(ntile):
                s, hf = divmod(t, 2)
                col = s * 16 + e_
                ph.op("pool", lambda e, t=t, hf=hf, col=col: e.indirect_dma_start(
                    out=xg[e_ % 2][:, t, :], out_offset=None, in_=XN[:, :],
                    in_offset=bass.IndirectOffsetOnAxis(ap=iTi[:, hf, col:col + 1], axis=0)),
                    r=["iTi"], w=[("xg", e_ % 2, t)], dma=True)

        def transposes(e_):
            if e_ >= NE:
                return
            for t in range(ntile):
                transpose8(ph, xg[e_ % 2][:, t, :], ("xg", e_ % 2, t), ptb[t % 2], ("ptb", t % 2),
                           xgT[:, :, t * 128:(t + 1) * 128], ("xgT", t), "act" if t % 2 == 0 else "dve")

        for g in range(4):
            load_slab(g)
        gather(0)
        transposes(0)
        gather(1)
        prev_sc = {}
        cur_sc = {}
        cnt = 0
        for e_ in range(NE):
            for fg in range(4):
                g = e_ * 6 + fg
                slot = g % 4
                sv = ring[slot][:].rearrange("p (a k n) -> p a k n", a=2, k=8)
                for fc4 in range(4):
                    fc = fg * 4 + fc4
                    for hb in range(nblk):
                        q = cnt % 2
                        cnt += 1
                        tks = [("xgT", t) for t in range(hb * (TB // 128), (hb + 1) * (TB // 128))]
                        for a in range(2):
                            for k in range(8):
                                ph.op("pe", lambda e, q=q, a=a, k=k, sv=sv, fc4=fc4, hb=hb: e.matmul(
                                    PGU[q][:, a * 512:a * 512 + TB], lhsT=sv[:, a, k, fc4 * 128:(fc4 + 1) * 128],
                                    rhs=xgT[:, k, hb * TB:(hb + 1) * TB], start=(k == 0), stop=(k == 7)),
                                    r=[("ring", slot, a)] + tks, w=[("pgu", q, a)])
                        ph.op("act", lambda e, q=q: e.activation(out=sg[q][:, 0:TB], in_=PGU[q][:, 0:TB], func=AF.Silu), r=[("pgu", q, 0)], w=[("sg", q)])
                        ph.op("dve", lambda e, q=q, fc=fc, hb=hb: e.tensor_tensor(out=hT[:, fc, hb * TB:(hb + 1) * TB], in0=sg[q][:, 0:TB],
                                                                                 in1=PGU[q][:, 512:512 + TB], op=ALU.mult),
                              r=[("sg", q), ("pgu", q, 1)], w=[("hT", fc, hb)])
                load_slab(g + 4)
            transposes(e_ + 1)
            gather(e_ + 2)
            d0 = ring[(e_ * 6 + 4) % 4][:].rearrange("p (k n) -> p k n", n=1024)
            d1 = ring[(e_ * 6 + 5) % 4][:].rearrange("p (k n) -> p k n", n=1024)
            dkeys = [("ring", (e_ * 6 + 4) % 4, 0), ("ring", (e_ * 6 + 4) % 4, 1), ("ring", (e_ * 6 + 5) % 4, 0), ("ring", (e_ * 6 + 5) % 4, 1)]
            for t in range(ntile):
                s, hf = divmod(t, 2)
                col = s * 16 + e_
                hb = (t * 128) // TB
                b = t % 2
                for nh in range(2):
                    for fc in range(16):
                        dsl = d0 if fc < 8 else d1
                        ph.op("pe", lambda e, nh=nh, fc=fc, dsl=dsl, t=t: e.matmul(
                            PD[:, nh * 512:(nh + 1) * 512], lhsT=hT[:, fc, t * 128:(t + 1) * 128], rhs=dsl[:, fc % 8, nh * 512:(nh + 1) * 512],
                            start=(fc == 0), stop=(fc == 15)), r=dkeys + [("hT", fc_, hb) for fc_ in range(16)], w=[("pd", nh)])
                    sl = slice(nh * 512, (nh + 1) * 512)
                    if nh == 0:
                        ph.op("act", lambda e, b=b, sl=sl, hf=hf, col=col: e.activation(out=yt[b][:, sl], in_=PD[:, sl], func=AF.Copy, scale=gT[:, hf, col:col + 1]),
                              r=[("pd", nh), "gT"], w=[("yt", b, nh)])
                    else:
                        ph.op("dve", lambda e, b=b, sl=sl, hf=hf, col=col: e.tensor_scalar(out=yt[b][:, sl], in0=PD[:, sl], scalar1=gT[:, hf, col:col + 1],
                                                                                          scalar2=None, op0=ALU.mult),
                              r=[("pd", nh), "gT"], w=[("yt", b, nh)])
                o = ph.op("pool", lambda e, b=b, hf=hf, col=col: e.indirect_dma_start(
                    out=H[:, :], out_offset=bass.IndirectOffsetOnAxis(ap=iTi[:, hf, col:col + 1], axis=0),
                    in_=yt[b][:, :], in_offset=None, compute_op=ALU.add),
                    r=[("yt", b, 0), ("yt", b, 1), "iTi"], w=[], dma=True, after=prev_sc.get(s, []))
                cur_sc.setdefault(s, []).append(o)
            prev_sc = cur_sc
            cur_sc = {}
            load_slab(e_ * 6 + 4 + 4)
            load_slab(e_ * 6 + 5 + 4)
        ph.emit()

    def phase_C(s):
        ph = Phase(nc, f"C{s}")
        xnT = ph.sb("xnT", [128, 8, S], BF16)
        xl = [ph.sb(f"xl{i}", [128, D], BF16) for i in range(2)]
        ws = [ph.sb(f"ws{i}", [128, 4096], BF16) for i in range(4)]
        cs = [ph.sb(f"cs{i}", [128, 2, 512], F32) for i in range(1)]
        qT = ph.sb("qT", [128, 2, S], BF16)
        kT = ph.sb("kT", [128, 2, S], BF16)
        kf = ph.sb("kf", [128, NT, 256], BF16)
        kb = ph.sb("kb", [128, NT, 256], BF16)
        vt = ph.sb("vt", [128, NT, 512], BF16)
        rt = [ph.sb(f"rt{i}", [128, 512], F32) for i in range(4)]
        Sf32 = ph.sb("Sf32", [128, 1024], F32)
        Sb32 = [ph.sb(f"Sb32{i}", [128, 1024], F32) for i in range(2)]
        Sfb = [ph.sb(f"Sfb{i}", [128, 1024], BF16) for i in range(2)]
        Sbst = [ph.sb(f"Sbst{i}", [128, 1024], BF16) for i in range(4)]
        Sbin = [ph.sb(f"Sbin{i}", [128, 1024], BF16) for i in range(3)]
        ldb = ph.sb("ldb", [128, 8], F32)
        Mt = ph.sb("Mt", [128, 4, 128], F32)
        tmpM = [ph.sb(f"tmpM{i}", [128, 128], F32) for i in range(2)]
        qd = ph.sb("qd", [128, 4, 2, 2, 128], F32)
        kd = ph.sb("kd", [128, 4, 2], F32)
        cdc = ph.sb("cdc", [128, 4, 2], F32)
        dm = ph.sb("dm", [128, 4, 128], F32)
        rw = ph.sb("rw", [128, 2, 128], F32)
        cl = ph.sb("cl", [128, 3], F32)
        Pm = [ph.sb(f"Pm{i}", [128, 128], BF16) for i in range(2)]
        qfb = [ph.sb(f"qfb{i}", [128, 2, 2, 128], BF16) for i in range(2)]
        sgl = [ph.sb(f"sgl{i}", [128, 512], F32) for i in range(2)]
        on = [ph.sb(f"on{i}", [128, 512], F32) for i in range(2)]
        go = [ph.sb(f"go{i}", [128, 512], BF16) for i in range(2)]
        goT = [ph.sb(f"goT{i}", [128, 4, 128], BF16) for i in range(2)]
        mo = [ph.sb(f"mo{i}", [128, D], F32) for i in range(2)]
        bst = [ph.sb(f"bst{i}", [128, 6], F32) for i in range(2)]
        mv = [ph.sb(f"mv{i}", [128, 8], F32) for i in range(2)]
        ptb = ph.ps("ptb", [128, 8, 128], BF16)
        PG = ph.ps("pg", [128, 512], F32)
        PA = ph.ps("pa", [128, 1024], F32)
        PB = ph.ps("pb", [128, 1024], F32)
        PO = ph.ps("po", [128, 512], F32)
        PSC = ph.ps("psc", [128, 128], F32)
        ptk = ptb[:].rearrange("p a b -> p (a b)").rearrange("p (a b) -> p a b", b=256)

        ph.op("sp", lambda e: e.dma_start(out=ldb[:], in_=ret_ld.partition_broadcast(128)), w=["ldb"], dma=True)
        ph.op("sp", lambda e: e.dma_start(out=dm[:], in_=c_dmat.rearrange("a j i -> j a i")), w=["dm"], dma=True)
        ph.op("sp", lambda e: e.dma_start(out=rw[:], in_=c_rows.rearrange("a p i -> p a i")), w=["rw"], dma=True)
        ph.op("sp", lambda e: e.dma_start(out=cl[:], in_=c_cols), w=["cl"], dma=True)
        for h in range(4):
            ph.op("act", lambda e, h=h: e.activation(out=tmpM[0][:], in_=dm[:, 0, :], func=AF.Exp, scale=ldb[:, h:h + 1]), r=["dm", "ldb"], w=["tmA"])
            ph.op("dve", lambda e, h=h: e.tensor_tensor(out=tmpM[0][:], in0=tmpM[0][:], in1=dm[:, 1, :], op=ALU.mult), r=["tmA", "dm"], w=["tmA"])
            ph.op("act", lambda e, h=h: e.activation(out=tmpM[1][:], in_=dm[:, 2, :], func=AF.Exp, scale=ldb[:, 4 + h:5 + h]), r=["dm", "ldb"], w=["tmB"])
            ph.op("dve", lambda e, h=h: e.tensor_tensor(out=tmpM[1][:], in0=tmpM[1][:], in1=dm[:, 3, :], op=ALU.mult), r=["tmB", "dm"], w=["tmB"])
            ph.op("dve", lambda e, h=h: e.tensor_tensor(out=Mt[:, h, :], in0=tmpM[0][:], in1=tmpM[1][:], op=ALU.add), r=["tmA", "tmB"], w=[("Mt", h)])
            for d_ in range(2):
                lc = ldb[:, 4 * d_ + h:4 * d_ + h + 1]
                for dc in range(2):
                    ph.op("act", lambda e, h=h, d_=d_, lc=lc, dc=dc: e.activation(out=qd[:, h, d_, dc, :], in_=rw[:, d_, :], func=AF.Exp, scale=lc), r=["rw", "ldb"], w=[("qd", h)])
                ph.op("act", lambda e, h=h, d_=d_, lc=lc: e.activation(out=kd[:, h, d_:d_ + 1], in_=cl[:, d_:d_ + 1], func=AF.Exp, scale=lc), r=["cl", "ldb"], w=[("kd", h)])
                ph.op("act", lambda e, h=h, d_=d_, lc=lc: e.activation(out=cdc[:, h, d_:d_ + 1], in_=cl[:, 2:3], func=AF.Exp, scale=lc), r=["cl", "ldb"], w=[("cdc", h)])
        for i in range(NT):
            b = i % 2
            r0 = s * S + i * 128
            ph.op("sp", lambda e, b=b, r0=r0: e.dma_start(out=xl[b][:], in_=XN[r0:r0 + 128, :]), w=[("xl", b)], dma=True)
            transpose8(ph, xl[b], ("xl", b), ptb, "ptb", xnT[:, :, i * 128:(i + 1) * 128], ("xnT", i), "act" if b == 0 else "dve")
        allx = [("xnT", i) for i in range(NT)]
        if CCUT == "c1":
            ph.emit()
            return
        w_in_v = ret_w_in.rearrange("(k p) n -> p k n", p=128)
        w_out_v = ret_w_out.rearrange("(k p) n -> p k n", p=128)
        ws0v = ws[0][:].rearrange("p (k n) -> p k n", n=512)
        ws1v = ws[1][:].rearrange("p (k n) -> p k n", n=512)
        ws2v = ws[2][:].rearrange("p (k n) -> p k n", n=512)
        ws3v = ws[3][:].rearrange("p (k n) -> p k n", n=1024)
        prev_acc = {}
        cnt = 0
        for h in range(4):
            ph.op("pool", lambda e, h=h: e.dma_start(out=ws0v[:, :, 0:256], in_=w_in_v[:, :, h * 256:(h + 1) * 256]), w=[("ws0", 0)], dma=True)
            ph.op("pool", lambda e, h=h: e.dma_start(out=ws0v[:, :, 256:512], in_=w_in_v[:, :, 1024 + h * 256:1024 + (h + 1) * 256]), w=[("ws0", 1)], dma=True)
            ph.op("pool", lambda e, h=h: e.dma_start(out=ws1v, in_=w_in_v[:, :, 2048 + h * 512:2048 + (h + 1) * 512]), w=["ws1"], dma=True)
            ph.op("pool", lambda e, h=h: e.dma_start(out=ws2v, in_=w_in_v[:, :, 4096 + h * 512:4096 + (h + 1) * 512]), w=["ws2"], dma=True)
            ph.op("pool", lambda e, h=h: e.dma_start(out=ws3v, in_=w_out_v[:, h * 4:(h + 1) * 4, :]), w=["ws3"], dma=True)
            for tb in range(4):
                cb_ = 0
                ph.op("sp", lambda e, cb_=cb_, tb=tb: e.dma_start(out=cs[cb_][:], in_=c_cs[:, :, tb * 512:(tb + 1) * 512]), w=[("cs", cb_)], dma=True)
                tks = [("xnT", i) for i in range(tb * 4, tb * 4 + 4)]
                for which in range(2):
                    PX, pk = (PA, "pa") if cnt % 2 == 0 else (PB, "pb")
                    cnt += 1
                    off = which * 256
                    for dc in range(2):
                        for k in range(8):
                            ph.op("pe", lambda e, PX=PX, dc=dc, k=k, off=off, tb=tb: e.matmul(
                                PX[:, dc * 512:(dc + 1) * 512], lhsT=ws0v[:, k, off + dc * 128:off + (dc + 1) * 128],
                                rhs=xnT[:, k, tb * 512:(tb + 1) * 512], start=(k == 0), stop=(k == 7)),
                                r=[("ws0", which)] + tks, w=[(pk, dc)])
                    sc = 1.0 if which == 0 else 1.0 / 16.0
                    combos = [(0, 0), (1, 1), (0, 1), (1, 0)]
                    for ri, (xi, ci) in enumerate(combos):
                        ph.op("dve", lambda e, PX=PX, ri=ri, xi=xi, ci=ci, sc=sc, cb_=cb_: e.scalar_tensor_tensor(
                            out=rt[ri][:], in0=PX[:, xi * 512:(xi + 1) * 512], scalar=sc, in1=cs[cb_][:, ci, :], op0=ALU.mult, op1=ALU.mult),
                            r=[(pk, xi), ("cs", cb_)], w=[("rt", ri)])
                    dstT, dk = (qT, "qT") if which == 0 else (kT, "kT")
                    ph.op("pool", lambda e, dstT=dstT, tb=tb: e.tensor_tensor(out=dstT[:, 0, tb * 512:(tb + 1) * 512], in0=rt[0][:], in1=rt[1][:], op=ALU.subtract),
                          r=[("rt", 0), ("rt", 1)], w=[(dk, tb, 0)])
                    ph.op("pool", lambda e, dstT=dstT, tb=tb: e.tensor_tensor(out=dstT[:, 1, tb * 512:(tb + 1) * 512], in0=rt[2][:], in1=rt[3][:], op=ALU.add),
                          r=[("rt", 2), ("rt", 3)], w=[(dk, tb, 1)])
            if CCUT == "c2":
                break
            for i in range(NT):
                PV, pk = (PG, "pg") if i % 2 == 0 else (PO, "po")
                for k in range(8):
                    ph.op("pe", lambda e, PV=PV, i=i, k=k: e.matmul(PV[:], lhsT=xnT[:, k, i * 128:(i + 1) * 128], rhs=ws1v[:, k, :], start=(k == 0), stop=(k == 7)),
                          r=[("xnT", i), "ws1"], w=[pk])
                ph.op("act", lambda e, PV=PV, i=i: e.activation(out=vt[:, i, :], in_=PV[:], func=AF.Copy), r=[pk], w=[("vt", i)])
            if CCUT == "c3":
                break
            for g4 in range(4):
                for ii in range(4):
                    i = g4 * 4 + ii
                    for dc in range(2):
                        ph.op("pe", lambda e, ii=ii, dc=dc, i=i: e.transpose(out=ptk[:, ii, dc * 128:(dc + 1) * 128], in_=kT[:, dc, i * 128:(i + 1) * 128], identity=identb[:]),
                              r=[("kT", g4, dc)], w=["ptb"])
                ph.op("act", lambda e, g4=g4, h=h: e.activation(out=kf[:, g4 * 4:(g4 + 1) * 4, :], in_=ptk, func=AF.Copy, scale=kd[:, h, 0:1]),
                      r=["ptb", ("kd", h)], w=[("kf", g4)])
                ph.op("dve", lambda e, g4=g4, h=h: e.tensor_scalar(out=kb[:, g4 * 4:(g4 + 1) * 4, :], in0=ptk, scalar1=kd[:, h, 1:2], scalar2=None, op0=ALU.mult),
                      r=["ptb", ("kd", h), ("kf", g4)], w=[("kb", g4)])
            if CCUT == "proj":
                break
            ph.op("dve", lambda e: e.memset(Sb32[1][:], 0.0), w=[("Sb32", 1, 0), ("Sb32", 1, 1)])
            for c in range(NT - 1, 0, -1):
                PX, pk = (PA, "pa") if c % 2 == 0 else (PB, "pb")
                src_, dst_ = Sb32[c % 2], Sb32[(c + 1) % 2]
                for dc in range(2):
                    ph.op("pe", lambda e, PX=PX, dc=dc, c=c: e.matmul(PX[:, dc * 512:(dc + 1) * 512], lhsT=kb[:, c, dc * 128:(dc + 1) * 128], rhs=vt[:, c, :], start=True, stop=True),
                          r=[("kb", c // 4), ("vt", c)], w=[(pk, dc)])
                for dc in range(2):
                    ph.op("dve", lambda e, PX=PX, h=h, dc=dc, src_=src_, dst_=dst_: e.scalar_tensor_tensor(
                        out=dst_[:, dc * 512:(dc + 1) * 512], in0=src_[:, dc * 512:(dc + 1) * 512],
                        scalar=cdc[:, h, 1:2], in1=PX[:, dc * 512:(dc + 1) * 512], op0=ALU.mult, op1=ALU.add),
                        r=[("Sb32", c % 2, dc), (pk, dc), ("cdc", h)], w=[("Sb32", (c + 1) % 2, dc)])
                ph.op("act", lambda e, c=c, dst_=dst_: e.activation(out=Sbst[c % 4][:], in_=dst_[:], func=AF.Copy),
                      r=[("Sb32", (c + 1) % 2, 0), ("Sb32", (c + 1) % 2, 1)], w=[("Sbst", c % 4)])
                ph.op("sp", lambda e, c=c: e.dma_start(out=SBD[c - 1], in_=Sbst[c % 4][:]), r=[("Sbst", c % 4)], w=[("SBD", c - 1)], dma=True)
            if CCUT == "bwd":
                break
            ph.op("dve", lambda e: e.memset(Sf32[:], 0.0), w=["Sf32"])

            def prefetch(c):
                if c < NT - 1:
                    ph.op("sp", lambda e, c=c: e.dma_start(out=Sbin[c % 3][:], in_=SBD[c]), r=[("SBD", c)], w=[("Sbin", c % 3)], dma=True)

            def tail(c):
                b = c % 2
                r0 = s * S + c * 128
                for vc in range(4):
                    ph.op("pe", lambda e, b=b, vc=vc: e.transpose(out=ptb[:, vc, :], in_=go[b][:, vc * 128:(vc + 1) * 128], identity=identb[:]),
                          r=[("go", b)], w=["ptb"])
                ph.op("act", lambda e, b=b: e.activation(out=goT[b][:], in_=ptb[:, 0:4, :], func=AF.Copy), r=["ptb"], w=[("goT", b)])
                for nh in range(2):
                    for vc in range(4):
                        ph.op("pe", lambda e, b=b, nh=nh, vc=vc: e.matmul(PB[:, nh * 512:(nh + 1) * 512], lhsT=goT[b][:, vc, :], rhs=ws3v[:, vc, nh * 512:(nh + 1) * 512],
                                                                         start=(vc == 0), stop=(vc == 3)),
                              r=[("goT", b), "ws3"], w=[("pb", nh)])
                ph.op("act", lambda e, b=b: e.activation(out=mo[b][:, 0:512], in_=PB[:, 0:512], func=AF.Copy), r=[("pb", 0)], w=[("mo", b, 0)])
                ph.op("act", lambda e, b=b: e.activation(out=mo[b][:, 512:1024], in_=PB[:, 512:1024], func=AF.Copy), r=[("pb", 1)], w=[("mo", b, 1)])
                o = ph.op("pool", lambda e, b=b, r0=r0: e.dma_start(out=H[r0:r0 + 128, :], in_=mo[b][:], accum_op=ALU.add),
                          r=[("mo", b, 0), ("mo", b, 1)], w=[], dma=True, after=prev_acc.get(c, []))
                prev_acc[c] = [o]

            nstate = {}

            def epi1(c):
                b = c % 2
                ph.op("dve", lambda e, b=b: e.bn_stats(out=bst[b][:], in_=PO[:]), r=["po"], w=[("bst", b)])
                ph.op("dve", lambda e, b=b: e.bn_aggr(out=mv[b][:, 0:2], in_=bst[b][:]), r=[("bst", b)], w=[("mv", b)])
                yy, kyy = newton(ph, mv[b][:, 1:2], ("mv", b), mv[b], 4, ("gn", b), 1.0, 1e-5)
                nstate[c] = (yy, kyy)

            def epi2(c):
                b = c % 2
                yy, kyy = nstate[c]
                ph.op("dve", lambda e, b=b, yy=yy: e.tensor_scalar(out=on[b][:], in0=PO[:], scalar1=mv[b][:, 0:1], scalar2=yy, op0=ALU.subtract, op1=ALU.mult),
                      r=["po", ("mv", b), kyy], w=[("on", b)])
                ph.op("pool", lambda e, b=b: e.tensor_tensor(out=go[b][:], in0=on[b][:], in1=sgl[b][:], op=ALU.mult), r=[("on", b), ("sgl", b)], w=[("go", b)])

            prefetch(0)
            prefetch(1)
            for c in range(NT):
                b = c % 2
                tbk = c // 4
                if c < NT - 1:
                    for dc in range(2):
                        ph.op("pe", lambda e, dc=dc, c=c: e.matmul(PA[:, dc * 512:(dc + 1) * 512], lhsT=kf[:, c, dc * 128:(dc + 1) * 128], rhs=vt[:, c, :], start=True, stop=True),
                              r=[("kf", c // 4), ("vt", c)], w=[("pa", dc)])
                for dc in range(2):
                    ph.op("pe", lambda e, dc=dc, c=c: e.matmul(PSC[:], lhsT=kT[:, dc, c * 128:(c + 1) * 128], rhs=qT[:, dc, c * 128:(c + 1) * 128], start=(dc == 0), stop=(dc == 1)),
                          r=[("kT", tbk, 0), ("kT", tbk, 1), ("qT", tbk, 0), ("qT", tbk, 1)], w=["psc"])
                ph.op("dve", lambda e, b=b, h=h: e.tensor_tensor(out=Pm[b][:], in0=PSC[:], in1=Mt[:, h, :], op=ALU.mult), r=["psc", ("Mt", h)], w=[("Pm", b)])
                for d_ in range(2):
                    ph.op("dve", lambda e, b=b, d_=d_, c=c, h=h: e.tensor_tensor(out=qfb[b][:, d_, :, :], in0=qT[:, :, c * 128:(c + 1) * 128],
                                                                                 in1=qd[:, h, d_, :, :], op=ALU.mult),
                          r=[("qT", tbk, 0), ("qT", tbk, 1), ("qd", h)], w=[("qfb", b, d_)])
                if c > 0:
                    epi2(c - 1)
                for k in range(8):
                    ph.op("pe", lambda e, c=c, k=k: e.matmul(PG[:], lhsT=xnT[:, k, c * 128:(c + 1) * 128], rhs=ws2v[:, k, :], start=(k == 0), stop=(k == 7)),
                          r=[("xnT", c), "ws2"], w=["pg"])
                ph.op("act", lambda e, b=b: e.activation(out=sgl[b][:], in_=PG[:], func=AF.Silu), r=["pg"], w=[("sgl", b)])
                mms = [(Pm[b][:], vt[:, c, :], [("Pm", b), ("vt", c)])]
                if c > 0:
                    for dc in range(2):
                        mms.append((qfb[b][:, 0, dc, :], Sfb[c % 2][:, dc * 512:(dc + 1) * 512], [("qfb", b, 0), ("Sfb", c % 2)]))
                if c < NT - 1:
                    for dc in range(2):
                        mms.append((qfb[b][:, 1, dc, :], Sbin[c % 3][:, dc * 512:(dc + 1) * 512], [("qfb", b, 1), ("Sbin", c % 3)]))
                for mi, (l_, r_, ks) in enumerate(mms):
                    ph.op("pe", lambda e, l_=l_, r_=r_, mi=mi, n=len(mms): e.matmul(PO[:], lhsT=l_, rhs=r_, start=(mi == 0), stop=(mi == n - 1)), r=ks, w=["po"])
                if c < NT - 1:
                    for dc in range(2):
                        ph.op("dve", lambda e, h=h, dc=dc: e.scalar_tensor_tensor(out=Sf32[:, dc * 512:(dc + 1) * 512], in0=Sf32[:, dc * 512:(dc + 1) * 512],
                                                                              scalar=cdc[:, h, 0:1], in1=PA[:, dc * 512:(dc + 1) * 512], op0=ALU.mult, op1=ALU.add),
                              r=["Sf32", ("pa", dc), ("cdc", h)], w=["Sf32"])
                    ph.op("act", lambda e, c=c: e.activation(out=Sfb[(c + 1) % 2][:], in_=Sf32[:], func=AF.Copy), r=["Sf32"], w=[("Sfb", (c + 1) % 2)])
                epi1(c)
                prefetch(c + 2)
                if c > 0 and CCUT != "notail":
                    tail(c - 1)
            epi2(NT - 1)
            if CCUT != "notail":
                tail(NT - 1)
        ph.emit()

    sched = []
    for s in range(nseq):
        sched.append(("A", lambda s=s: phase_A(s)))
    sched.append(("R0", lambda: phase_R(0)))
    sched.append(("B0", lambda: phase_B(0)))
    sched.append(("P0", lambda: phase_P(0, False)))
    for s in range(nseq):
        sched.append(("C", lambda s=s: phase_C(s)))
    sched.append(("R1", lambda: phase_R(1)))
    sched.append(("B1", lambda: phase_B(1)))
    sched.append(("P1", lambda: phase_P(1, True)))
    only = _os.environ.get("SCHED_ONLY", "")
    for name, fn in sched:
        if only and name != only:
            continue
        fn()
        if stop_after is not None and name == stop_after:
            break
    gst.close()
    dbg = {"H": H}
    return nc, dbg


def make_in_maps(inputs, nseq, ncores):
    c = _consts()
    f32 = np.float32
    x = np.asarray(inputs["x"], f32)
    p = np.asarray(inputs["p"], f32)
    shared = dict(
        norm_mix=np.asarray(inputs["norm_mix"], f32), norm_ffn=np.asarray(inputs["norm_ffn"], f32),
        norm_ple=np.asarray(inputs["norm_ple"], f32), final_norm=np.asarray(inputs["final_norm"], f32).reshape(1, D),
        conv_w_in=np.asarray(inputs["conv_w_in"], f32)[0],
        conv_wb=np.ascontiguousarray(np.concatenate([np.asarray(inputs["conv_w"], f32)[0], np.asarray(inputs["conv_b"], f32)], 0)),
        conv_w_out=np.asarray(inputs["conv_w_out"], f32)[0],
        ret_w_in=np.asarray(inputs["ret_w_in"], f32)[0], ret_ld=np.asarray(inputs["ret_log_decay"], f32).reshape(1, 8),
        ret_w_out=np.asarray(inputs["ret_w_out"], f32)[0], router_w=np.asarray(inputs["router_w"], f32),
        exp_w_gate=np.asarray(inputs["exp_w_gate"], f32), exp_w_up=np.asarray(inputs["exp_w_up"], f32),
        exp_w_down=np.asarray(inputs["exp_w_down"], f32), ple_w_proj=np.asarray(inputs["ple_w_proj"], f32),
        ple_w_gate=np.asarray(inputs["ple_w_gate"], f32), **c)
    maps = []
    for ci in range(ncores):
        m = dict(shared)
        m["x"] = np.ascontiguousarray(x[ci * nseq:(ci + 1) * nseq].reshape(nseq * S, D))
        m["p"] = np.ascontiguousarray(p[:, ci * nseq:(ci + 1) * nseq].reshape(2, nseq * S, PLE))
        maps.append(m)
    return maps


def kernel(**inputs):
    B = np.asarray(inputs["x"]).shape[0]
    nseq = B // NCORES
    nc, _ = build(nseq)
    maps = make_in_maps(inputs, nseq, NCORES)
    res = run_bass_kernel_spmd(nc, maps, core_ids=list(range(NCORES)))
    outs = [np.asarray(r["out"], np.float32).reshape(nseq, S, D) for r in res.results]
    return np.concatenate(outs, 0)
```
